# Optimizing a Trainium2 kernel written in Bass

```python
import jax
import jax.numpy as jnp
from jax import lax
import numpy as np

D_MODEL = 2048
BATCH = 4
SEQ = 2048
DEPTH = 2

GRID_W = 64
CTX_LEN = 256
Q_BLOCK = 128
SCAN_CHUNK = 16
ROPE_THETA = 10000.0
NORM_EPS = 1e-6
A_HEADS = 8
A_DK = 128
A_DV = 128
B_HEADS = 8
B_Q_LORA = 512
B_KV_LORA = 256
B_NOPE = 128
B_ROPE = 64
B_DV = 128
C_HEADS = 8
C_KV_HEADS = 2
C_DH = 128
D_HEADS = 4
D_DK = 128
D_DV = 256
D_GATE_RANK = 16
GLA_TAU = 16.0
N_EXPERTS = 16
N_GROUPS = 4
TOP_K = 2
EXPERT_DIM = 512
N_AB = (DEPTH + 1) // 2
N_CD = DEPTH // 2

kernel_name = 'hybrid_hgrn2_mla_gqa_gla_moe_dit'


def rms_norm(x, g):
    xf = x.astype(jnp.float32)
    y = xf * lax.rsqrt(jnp.mean(xf * xf, axis=-1, keepdims=True) + NORM_EPS)
    return (y * g.astype(jnp.float32)).astype(x.dtype)


def modulate(h, shift, scale):
    return h * (1 + scale) + shift


def split_cols(p, sizes):
    cuts = np.cumsum(np.array(sizes))[:-1].tolist()
    return jnp.split(p, cuts, axis=-1)


def heads(x, h):
    b, t, w = x.shape
    return x.reshape(b, t, h, w // h).transpose(0, 2, 1, 3)


def merge(x):
    b, h, t, d = x.shape
    return x.transpose(0, 2, 1, 3).reshape(b, t, h * d)


def axial_angles(t_len, d_rope):
    rows = t_len // GRID_W
    quarter = d_rope // 4
    freqs = ROPE_THETA ** (-jnp.arange(quarter, dtype=jnp.float32) / quarter)
    row = jnp.repeat(jnp.arange(rows, dtype=jnp.float32), GRID_W)
    col = jnp.tile(jnp.arange(GRID_W, dtype=jnp.float32), rows)
    return jnp.concatenate([row[:, None] * freqs, col[:, None] * freqs], axis=-1)


def apply_rope(x, ang):
    half = x.shape[-1] // 2
    x1, x2 = x[..., :half], x[..., half:]
    cos, sin = jnp.cos(ang), jnp.sin(ang)
    return jnp.concatenate([x1 * cos - x2 * sin, x2 * cos + x1 * sin], axis=-1).astype(x.dtype)


def block_attention(q, k, v, scale):
    b, hk, g, t, dq = q.shape
    dv = v.shape[-1]
    nb = t // Q_BLOCK
    qb = jnp.moveaxis(q.reshape(b, hk, g, nb, Q_BLOCK, dq), 3, 0)

    def one_block(qi):
        s = jnp.einsum('bhgtd,bhsd->bhgts', qi, k, preferred_element_type=jnp.float32) * scale
        p = jax.nn.softmax(s, axis=-1)
        return jnp.einsum('bhgts,bhsv->bhgtv', p.astype(v.dtype), v)

    o = lax.map(one_block, qb)
    return jnp.moveaxis(o, 0, 3).reshape(b, hk * g, t, dv)


def chunked_gated_scan(q, k, v, g, s0):
    b, h, t, dk = q.shape
    n = t // SCAN_CHUNK

    def to_chunks(a):
        return a.astype(jnp.float32).reshape(b, h, n, SCAN_CHUNK, a.shape[-1]).transpose(2, 0, 1, 3, 4)

    mask = jnp.tril(jnp.ones((SCAN_CHUNK, SCAN_CHUNK), dtype=bool))[:, :, None]

    def step(state, inp):
        qc, kc, vc, gc = inp
        cum = jnp.cumsum(gc, axis=-2)
        rel = jnp.where(mask, cum[..., :, None, :] - cum[..., None, :, :], -jnp.inf)
        scores = jnp.einsum('bhtd,bhsd,bhtsd->bhts', qc, kc, jnp.exp(rel))
        o = jnp.einsum('bhts,bhsv->bhtv', scores, vc) + jnp.einsum('bhtd,bhdv->bhtv', qc * jnp.exp(cum), state)
        last = cum[..., -1:, :]
        state = state * jnp.exp(last)[..., 0, :, None] + jnp.einsum('bhsd,bhsv->bhdv', kc * jnp.exp(last - cum), vc)
        return state, o

    s_final, o = lax.scan(step, s0.astype(jnp.float32), (to_chunks(q), to_chunks(k), to_chunks(v), to_chunks(g)))
    o = o.transpose(1, 2, 0, 3, 4).reshape(b, h, t, v.shape[-1])
    return o.astype(v.dtype), s_final


def scan_ctx_then_lat(c_args, x_args, reverse):
    if reverse:
        c_args = tuple(jnp.flip(a, axis=2) for a in c_args)
        x_args = tuple(jnp.flip(a, axis=2) for a in x_args)
    b, h, _, dk = c_args[0].shape
    dv = c_args[2].shape[-1]
    oc, s_ctx = chunked_gated_scan(*c_args, jnp.zeros((b, h, dk, dv), jnp.float32))
    ox, _ = chunked_gated_scan(*x_args, s_ctx)
    if reverse:
        oc, ox = jnp.flip(oc, axis=2), jnp.flip(ox, axis=2)
    return oc, ox


def mixer_ab(hc, hx, w_in, w_out, lb_f, lb_b, hg_norm_g, mq_norm_g, w_uq, mkv_norm_g, w_ukv, ang, need_ctx):
    sizes = (A_HEADS * A_DK,) * 3 + (A_HEADS * A_DV,) * 2 + (B_Q_LORA, B_KV_LORA, B_ROPE)
    pc = split_cols(hc @ w_in, sizes)
    px = split_cols(hx @ w_in, sizes)

    def hgrn_args(p):
        q = heads(jax.nn.silu(p[0]), A_HEADS) * A_DK ** -0.5
        v = heads(p[3], A_HEADS)

        def direction(f_logit, lb):
            f = lb + (1 - lb) * jax.nn.sigmoid(f_logit.astype(jnp.float32))
            return heads(1 - f, A_HEADS), heads(jnp.log(f), A_HEADS)

        kf, gf = direction(p[1], lb_f)
        kb, gb = direction(p[2], lb_b)
        return (q, kf, v, gf), (q, kb, v, gb)

    c_fwd, c_bwd = hgrn_args(pc)
    x_fwd, x_bwd = hgrn_args(px)
    oc_f, ox_f = scan_ctx_then_lat(c_fwd, x_fwd, False)
    oc_b, ox_b = scan_ctx_then_lat(c_bwd, x_bwd, True)

    def hgrn_out(o, gate):
        return merge(rms_norm(o, hg_norm_g) * jax.nn.sigmoid(heads(gate, A_HEADS)))

    def mla_qkv(p, rotate):
        q = heads(rms_norm(p[5], mq_norm_g) @ w_uq, B_HEADS)
        kv = heads(rms_norm(p[6], mkv_norm_g) @ w_ukv, B_HEADS)
        q_nope, q_rope = q[..., :B_NOPE], q[..., B_NOPE:]
        k_nope, v = kv[..., :B_NOPE], kv[..., B_NOPE:]
        k_rope = p[7][:, None]
        if rotate:
            q_rope, k_rope = apply_rope(q_rope, ang), apply_rope(k_rope, ang)
        k = jnp.concatenate([k_nope, jnp.broadcast_to(k_rope, k_nope.shape[:-1] + (B_ROPE,))], axis=-1)
        return jnp.concatenate([q_nope, q_rope], axis=-1), k, v

    qc, kc, vc = mla_qkv(pc, False)
    qx, kx, vx = mla_qkv(px, True)
    scale = (B_NOPE + B_ROPE) ** -0.5
    ox_mla = block_attention(qx[:, :, None], jnp.concatenate([kc, kx], axis=2), jnp.concatenate([vc, vx], axis=2), scale)
    out_x = jnp.concatenate([hgrn_out(ox_f + ox_b, px[4]), merge(ox_mla)], axis=-1) @ w_out
    if not need_ctx:
        return None, out_x
    oc_mla = block_attention(qc[:, :, None], kc, vc, scale)
    out_c = jnp.concatenate([hgrn_out(oc_f + oc_b, pc[4]), merge(oc_mla)], axis=-1) @ w_out
    return out_c, out_x


def mixer_cd(hc, hx, w_in, w_out, q_norm_g, k_norm_g, w_a2, b_a, gla_norm_g, ang, need_ctx):
    sizes = (C_HEADS * C_DH, C_KV_HEADS * C_DH, C_KV_HEADS * C_DH, D_HEADS * D_DK, D_HEADS * D_DK,
             D_HEADS * D_DV, D_HEADS * D_DV, D_GATE_RANK, D_GATE_RANK)
    pc = split_cols(hc @ w_in, sizes)
    px = split_cols(hx @ w_in, sizes)

    def gqa_qkv(p, rotate):
        q = rms_norm(heads(p[0], C_HEADS), q_norm_g)
        k = rms_norm(heads(p[1], C_KV_HEADS), k_norm_g)
        v = heads(p[2], C_KV_HEADS)
        if rotate:
            q, k = apply_rope(q, ang), apply_rope(k, ang)
        b, _, t, _ = q.shape
        return q.reshape(b, C_KV_HEADS, C_HEADS // C_KV_HEADS, t, C_DH), k, v

    qc, kc, vc = gqa_qkv(pc, False)
    qx, kx, vx = gqa_qkv(px, True)
    scale = C_DH ** -0.5
    ox_att = block_attention(qx, jnp.concatenate([kc, kx], axis=2), jnp.concatenate([vc, vx], axis=2), scale)

    def gla_args(p):
        q = heads(p[3], D_HEADS) * D_DK ** -0.5
        k = heads(p[4], D_HEADS)
        v = heads(p[5], D_HEADS)

        def direction(a, d):
            return heads(jax.nn.log_sigmoid((a @ w_a2[d] + b_a[d]).astype(jnp.float32)) / GLA_TAU, D_HEADS)

        return (q, k, v, direction(p[7], 0)), (q, k, v, direction(p[8], 1))

    c_fwd, c_bwd = gla_args(pc)
    x_fwd, x_bwd = gla_args(px)
    oc_f, ox_f = scan_ctx_then_lat(c_fwd, x_fwd, False)
    oc_b, ox_b = scan_ctx_then_lat(c_bwd, x_bwd, True)

    def gla_out(o, gate):
        return merge(rms_norm(o, gla_norm_g) * jax.nn.silu(heads(gate, D_HEADS)))

    out_x = jnp.concatenate([merge(ox_att), gla_out(ox_f + ox_b, px[6])], axis=-1) @ w_out
    if not need_ctx:
        return None, out_x
    oc_att = block_attention(qc, kc, vc, scale)
    out_c = jnp.concatenate([merge(oc_att), gla_out(oc_f + oc_b, pc[6])], axis=-1) @ w_out
    return out_c, out_x


def moe(h, router_w, router_b, w1, w3, w2):
    shape = h.shape
    hf = h.reshape(-1, shape[-1])
    scores = jax.nn.sigmoid((hf @ router_w).astype(jnp.float32))
    sel = scores + router_b.astype(jnp.float32)
    per_group = N_EXPERTS // N_GROUPS
    grp_top, _ = lax.top_k(sel.reshape(-1, N_GROUPS, per_group), 2)
    g_idx = jnp.argmax(grp_top.sum(-1), axis=-1)
    g_mask = jnp.repeat(jnp.arange(N_GROUPS)[None, :] == g_idx[:, None], per_group, axis=1)
    _, e_idx = lax.top_k(jnp.where(g_mask, sel, -jnp.inf), TOP_K)
    w = jnp.take_along_axis(scores, e_idx, axis=-1)
    w = w / jnp.sum(w, axis=-1, keepdims=True)
    combine = jnp.sum(jax.nn.one_hot(e_idx, N_EXPERTS, dtype=jnp.float32) * w[..., None], axis=1)
    hidden = jax.nn.silu(jnp.einsum('nd,edf->nef', hf, w1)) * jnp.einsum('nd,edf->nef', hf, w3)
    y = jnp.einsum('nef,efd->nd', hidden * combine[..., None].astype(hidden.dtype), w2)
    return y.reshape(shape)


def setup_inputs(seed: int = 0) -> dict:
    key = jax.random.key(seed)
    ks = jax.random.split(key, 29)
    D = D_MODEL
    ab_in = 3 * A_HEADS * A_DK + 2 * A_HEADS * A_DV + B_Q_LORA + B_KV_LORA + B_ROPE
    ab_mix = A_HEADS * A_DV + B_HEADS * B_DV
    cd_in = (C_HEADS + 2 * C_KV_HEADS) * C_DH + 2 * D_HEADS * D_DK + 2 * D_HEADS * D_DV + 2 * D_GATE_RANK
    cd_mix = C_HEADS * C_DH + D_HEADS * D_DV
    mod_init = 0.5

    def nrm(i, shape, scale):
        return jax.random.normal(ks[i], shape, jnp.float32) * scale

    def gain(i, shape):
        return 1.0 + nrm(i, shape, 0.02)

    return {
        'x': nrm(0, (BATCH, SEQ, D), 1.0),
        'c': nrm(1, (BATCH, D), 1.0),
        'ctx': nrm(2, (BATCH, CTX_LEN, D), 1.0),
        'c_ctx': nrm(3, (D,), 1.0),
        'mod_w': nrm(4, (DEPTH, D, 6 * D), mod_init * D ** -0.5),
        'mod_b': nrm(5, (DEPTH, 6 * D), 0.02),
        'norm_attn_g': gain(6, (DEPTH, D)),
        'norm_ffn_g': gain(7, (DEPTH, D)),
        'final_norm_g': gain(8, (D,)),
        'ab_w_in': nrm(9, (N_AB, D, ab_in), D ** -0.5),
        'ab_w_out': nrm(10, (N_AB, ab_mix, D), ab_mix ** -0.5),
        'hgrn_lb_logits': nrm(11, (2, DEPTH + 1, A_HEADS * A_DK), 0.1),
        'hgrn_norm_g': gain(12, (N_AB, A_DV)),
        'mla_q_norm_g': gain(13, (N_AB, B_Q_LORA)),
        'mla_w_uq': nrm(14, (N_AB, B_Q_LORA, B_HEADS * (B_NOPE + B_ROPE)), B_Q_LORA ** -0.5),
        'mla_kv_norm_g': gain(15, (N_AB, B_KV_LORA)),
        'mla_w_ukv': nrm(16, (N_AB, B_KV_LORA, B_HEADS * (B_NOPE + B_DV)), B_KV_LORA ** -0.5),
        'cd_w_in': nrm(17, (N_CD, D, cd_in), D ** -0.5),
        'cd_w_out': nrm(18, (N_CD, cd_mix, D), cd_mix ** -0.5),
        'gqa_q_norm_g': gain(19, (N_CD, C_DH)),
        'gqa_k_norm_g': gain(20, (N_CD, C_DH)),
        'gla_w_a2': nrm(21, (N_CD, 2, D_GATE_RANK, D_HEADS * D_DK), D_GATE_RANK ** -0.5),
        'gla_b_a': nrm(22, (N_CD, 2, D_HEADS * D_DK), 0.1),
        'gla_norm_g': gain(23, (N_CD, D_DV)),
        'router_w': nrm(24, (D, N_EXPERTS), D ** -0.5),
        'router_b': nrm(25, (N_EXPERTS,), 0.01),
        'moe_w1': nrm(26, (DEPTH, N_EXPERTS, D, EXPERT_DIM), D ** -0.5),
        'moe_w3': nrm(27, (DEPTH, N_EXPERTS, D, EXPERT_DIM), D ** -0.5),
        'moe_w2': nrm(28, (DEPTH, N_EXPERTS, EXPERT_DIM, D), EXPERT_DIM ** -0.5),
    }


def reference(x, c, ctx, c_ctx, mod_w, mod_b, norm_attn_g, norm_ffn_g, final_norm_g,
              ab_w_in, ab_w_out, hgrn_lb_logits, hgrn_norm_g, mla_q_norm_g, mla_w_uq, mla_kv_norm_g, mla_w_ukv,
              cd_w_in, cd_w_out, gqa_q_norm_g, gqa_k_norm_g, gla_w_a2, gla_b_a, gla_norm_g,
              router_w, router_b, moe_w1, moe_w3, moe_w2):
    t_len = x.shape[1]
    ang_b = axial_angles(t_len, B_ROPE)
    ang_c = axial_angles(t_len, C_DH)
    lb = jnp.cumsum(jax.nn.softmax(hgrn_lb_logits.astype(jnp.float32), axis=1), axis=1)
    xc = ctx
    for l in range(DEPTH):
        need_ctx = l < DEPTH - 1
        j = l // 2
        mx = (jax.nn.silu(c) @ mod_w[l] + mod_b[l])[:, None, :]
        mc = jax.nn.silu(c_ctx) @ mod_w[l] + mod_b[l]
        sx1, ax1, gx1, sx2, ax2, gx2 = jnp.split(mx, 6, axis=-1)
        sc1, ac1, gc1, sc2, ac2, gc2 = jnp.split(mc, 6, axis=-1)
        hx = modulate(rms_norm(x, norm_attn_g[l]), sx1, ax1)
        hc = modulate(rms_norm(xc, norm_attn_g[l]), sc1, ac1)
        if l % 2 == 0:
            oc, ox = mixer_ab(hc, hx, ab_w_in[j], ab_w_out[j], lb[0, l], lb[1, l], hgrn_norm_g[j],
                              mla_q_norm_g[j], mla_w_uq[j], mla_kv_norm_g[j], mla_w_ukv[j], ang_b, need_ctx)
        else:
            oc, ox = mixer_cd(hc, hx, cd_w_in[j], cd_w_out[j], gqa_q_norm_g[j], gqa_k_norm_g[j],
                              gla_w_a2[j], gla_b_a[j], gla_norm_g[j], ang_c, need_ctx)
        x = x + gx1 * ox
        hx = modulate(rms_norm(x, norm_ffn_g[l]), sx2, ax2)
        x = x + gx2 * moe(hx, router_w, router_b, moe_w1[l], moe_w3[l], moe_w2[l])
        if need_ctx:
            xc = xc + gc1 * oc
            hc = modulate(rms_norm(xc, norm_ffn_g[l]), sc2, ac2)
            xc = xc + gc2 * moe(hc, router_w, router_b, moe_w1[l], moe_w3[l], moe_w2[l])
    return rms_norm(x, final_norm_g)
```

```python
import contextlib
import numpy as np
import concourse.bass as bass
import concourse.mybir as mybir
from concourse.bass_utils import run_bass_kernel_spmd

F32 = mybir.dt.float32
BF16 = mybir.dt.bfloat16
AF = mybir.ActivationFunctionType
ALU = mybir.AluOpType
DT_SIZE = {F32: 4, BF16: 2}

D = 2048
CTX = 256
LAT = 2048
T = CTX + LAT
NT = T // 128
EPS = 1e-6
NE = 16
EDIM = 512


def _box(ap):
    esz = DT_SIZE[ap.dtype]
    pat = ap.ap
    off = ap.offset
    name = ap.tensor.name
    if str(ap.space) == 'DRAM':
        hi = off + sum((c - 1) * s for s, c in pat) + 1
        return (name, 0, 1, off * esz, hi * esz)
    pstep, pcnt = pat[0]
    if pstep == 0:
        p0 = 0
        f0 = off
        pcnt = 128
    else:
        p0 = off // pstep
        f0 = off - p0 * pstep
    f1 = f0 + sum((c - 1) * s for s, c in pat[1:]) + 1
    return (name, p0, p0 + pcnt, f0 * esz, f1 * esz)


def _ov(a, b):
    return a[1] < b[2] and b[1] < a[2] and a[3] < b[4] and b[3] < a[4]


class Sch:
    ENG = ('pe', 'act', 'dve', 'pool', 'sp')

    def __init__(s, nc, es, sbuf_bytes=192 * 1024):
        s.nc = nc
        s.es = es
        s.eng = {'pe': nc.tensor, 'act': nc.scalar, 'dve': nc.vector, 'pool': nc.gpsimd, 'sp': nc.sync}
        s.sem = {e: es.enter_context(nc.semaphore('sem_' + e)) for e in s.ENG}
        s.cnt = {e: 0 for e in s.ENG}
        s.waited = {}
        s.regions = {}
        s.dma_sems = {}
        s.big = es.enter_context(nc.sbuf_tensor('SB', [128, sbuf_bytes // 4], F32))
        s.sb_top = 0
        s.sb_cap = sbuf_bytes
        s.ps = es.enter_context(nc.psum_tensor('PS', [128, 4096], F32))
        s.n_inst = 0
        s.dom_map = {}
        import os
        s.npool = int(os.environ.get('NPOOL', '1000'))
        s.waitall = os.environ.get('WAITALL', '1') == '1'
        s.serial = os.environ.get('SERIAL', '1') == '1'

    def mark(s):
        return s.sb_top

    def release(s, m):
        s.sb_top = m

    @staticmethod
    def _shape(v, shape):
        if len(shape) > 2:
            names = ' '.join('d%d' % i for i in range(1, len(shape)))
            v = v.rearrange('p (%s) -> p %s' % (names, names), **{'d%d' % i: shape[i] for i in range(2, len(shape))})
        return v

    def sb(s, shape, dt):
        n = int(np.prod(shape[1:]))
        nb = (n * DT_SIZE[dt] + 63) // 64 * 64
        assert s.sb_top + nb <= s.sb_cap, ('SBUF OOM', s.sb_top, nb)
        v = s.big[0:shape[0], s.sb_top // 4:(s.sb_top + nb) // 4]
        s.sb_top += nb
        if dt != F32:
            v = v.bitcast(dt)
        return s._shape(v[:, 0:n], shape)

    def psum(s, bank, shape, dt=F32, off=0):
        n = int(np.prod(shape[1:]))
        nb = n * DT_SIZE[dt]
        assert off % 4 == 0 and off + nb <= 2048 * (8 - bank)
        v = s.ps[0:shape[0], bank * 512 + off // 4: bank * 512 + (off + nb + 3) // 4]
        if dt != F32:
            v = v.bitcast(dt)
        return s._shape(v[:, 0:n], shape)

    def _deps(s, reads, writes, me):
        deps = {}
        rb = [_box(a) for a in reads]
        wb = [_box(a) for a in writes]
        for b in rb:
            for r in s.regions.get(b[0], ()):
                if _ov(r[0], b):
                    for dom, val in r[1].items():
                        if deps.get(dom, 0) < val:
                            deps[dom] = val
        for b in wb:
            for r in s.regions.get(b[0], ()):
                if _ov(r[0], b):
                    for dom, val in r[1].items():
                        if deps.get(dom, 0) < val:
                            deps[dom] = val
                    for dom, val in r[2].items():
                        if dom != me and deps.get(dom, 0) < val:
                            deps[dom] = val
        return deps, rb, wb

    def _record(s, rb, wb, dom, val):
        for b in rb:
            lst = s.regions.setdefault(b[0], [])
            found = None
            for r in lst:
                if r[0] == b:
                    found = r
                elif _ov(r[0], b):
                    r[2][dom] = val
            if found is None:
                w = {}
                for r in lst:
                    if _ov(r[0], b):
                        for d, v in r[1].items():
                            if w.get(d, 0) < v:
                                w[d] = v
                found = [b, w, {}]
                lst.append(found)
            found[2][dom] = val
        for b in wb:
            lst = s.regions.setdefault(b[0], [])
            found = None
            for r in lst:
                if r[0] == b:
                    found = r
                elif _ov(r[0], b):
                    q = r[0]
                    if b[1] <= q[1] and q[2] <= b[2] and b[3] <= q[3] and q[4] <= b[4]:
                        r[1] = {dom: val}
                        r[2] = {}
                    else:
                        r[1][dom] = val
            if found is None:
                found = [b, {}, {}]
                lst.append(found)
            found[1] = {dom: val}
            found[2] = {}
        for b in rb + wb:
            lst = s.regions[b[0]]
            if len(lst) > 400:
                s._compact(b[0])

    def _compact(s, name):
        lst = s.regions[name]
        half = len(lst) // 2
        old = lst[:half]
        p0 = min(r[0][1] for r in old)
        p1 = max(r[0][2] for r in old)
        f0 = min(r[0][3] for r in old)
        f1 = max(r[0][4] for r in old)
        w = {}
        rd = {}
        for r in old:
            for d, v in r[1].items():
                if w.get(d, 0) < v:
                    w[d] = v
            for d, v in r[2].items():
                if rd.get(d, 0) < v:
                    rd[d] = v
        s.regions[name] = [[(name + '#old', p0, p1, f0, f1), w, rd]] + lst[half:]

    def _emit_waits(s, e, deps):
        eng = s.eng[e]
        if s.serial:
            deps = {d: v for d, v in s.cnt.items() if v > 0}
        for dom, val in deps.items():
            if dom == e and e == 'pe':
                continue
            if dom in s.dma_sems and s.waitall:
                val = s.cnt[dom]
            if s.waited.get((e, dom), 0) >= val:
                continue
            s.waited[(e, dom)] = val
            sem = s.sem[dom] if dom in s.sem else s.dma_sems[dom]
            eng.wait_ge(sem, val)
            s.n_inst += 1

    def op(s, e, fn, reads, writes):
        deps, rb, wb = s._deps(reads, writes, e)
        s._emit_waits(e, deps)
        ins = fn(s.eng[e])
        s.n_inst += 1
        s.cnt[e] += 1
        ins.then_inc(s.sem[e], 1)
        s._record(rb, wb, e, s.cnt[e])
        return ins

    def dma(s, e, out, in_, dom):
        if dom not in s.dom_map:
            s.dom_map[dom] = 'dq%d' % (len(s.dom_map) % s.npool)
        dom = s.dom_map[dom]
        if dom not in s.dma_sems:
            s.dma_sems[dom] = s.es.enter_context(s.nc.semaphore(dom))
            s.cnt[dom] = 0
        deps, rb, wb = s._deps([in_], [out], dom)
        s._emit_waits(e, deps)
        s.cnt[dom] += 16
        s.eng[e].dma_start(out=out, in_=in_).then_inc(s.dma_sems[dom], 16)
        s.n_inst += 1
        s._record(rb, wb, dom, s.cnt[dom])

    def finish(s):
        for dom in s.dma_sems:
            if s.cnt[dom] > 0:
                s.eng['sp'].wait_ge(s.dma_sems[dom], s.cnt[dom])

    def act(s, out, in_, func, bias=None, scale=None, accum_out=None, eng='act'):
        kw = {}
        rd = [in_]
        wr = [out]
        if bias is not None:
            kw['bias'] = bias
            if not isinstance(bias, (int, float)):
                rd.append(bias)
        if scale is not None:
            kw['scale'] = scale
            if not isinstance(scale, (int, float)):
                rd.append(scale)
        if accum_out is not None:
            kw['accum_out'] = accum_out
            wr.append(accum_out)
        return s.op('act', lambda e: e.activation(out=out, in_=in_, func=func, **kw), rd, wr)

    def tt(s, eng, out, in0, in1, op):
        return s.op(eng, lambda e: e.tensor_tensor(out=out, in0=in0, in1=in1, op=op), [in0, in1], [out])

    def ts(s, eng, out, in0, s1, op0, s2=None, op1=None):
        rd = [in0] + [x for x in (s1, s2) if x is not None and not isinstance(x, (int, float))]
        if op1 is None:
            return s.op(eng, lambda e: e.tensor_scalar(out=out, in0=in0, scalar1=s1, scalar2=None, op0=op0), rd, [out])
        return s.op(eng, lambda e: e.tensor_scalar(out=out, in0=in0, scalar1=s1, scalar2=s2, op0=op0, op1=op1), rd, [out])

    def stt(s, out, in0, sc, in1, op0, op1):
        rd = [in0, in1] + ([] if isinstance(sc, (int, float)) else [sc])
        return s.op('dve', lambda e: e.scalar_tensor_tensor(out=out, in0=in0, scalar=sc, in1=in1, op0=op0, op1=op1), rd, [out])

    def cp(s, eng, out, in_):
        if eng == 'act':
            return s.op('act', lambda e: e.copy(out=out, in_=in_), [in_], [out])
        return s.op(eng, lambda e: e.tensor_copy(out=out, in_=in_), [in_], [out])

    def recip(s, out, in_):
        return s.op('dve', lambda e: e.reciprocal(out=out, in_=in_), [in_], [out])

    def memset(s, eng, out, val):
        return s.op(eng, lambda e: e.memset(out, val), [], [out])

    def mm(s, out, pairs):
        n = len(pairs)

        def fn(e):
            ins = None
            for i, (l, r) in enumerate(pairs):
                ins = e.matmul(out, l, r, start=(i == 0), stop=(i == n - 1))
            return ins
        rd = []
        for l, r in pairs:
            rd.append(l)
            rd.append(r)
        s.n_inst += n - 1
        return s.op('pe', fn, rd, [out])

    def mm1(s, out, l, r, start, stop):
        return s.op('pe', lambda e: e.matmul(out, l, r, start=start, stop=stop), [l, r], [out])

    def tr(s, out, in_, ident):
        return s.op('pe', lambda e: e.transpose(out, in_, ident), [in_, ident], [out])


class Prog:
    def __init__(p, dbg=()):
        p.dbg = set(dbg)
        p.nc = bass.Bass("TRN2", target_bir_lowering=False)
        p.inputs = {}
        p.outputs = {}

    def din(p, name, shape, dt=F32):
        a = p.nc.dram_tensor(name, list(shape), dt, kind="ExternalInput").ap()
        p.inputs[name] = a
        return a

    def dscr(p, name, shape, dt=F32):
        kind = "ExternalOutput" if name in p.dbg else "Internal"
        a = p.nc.dram_tensor(name, list(shape), dt, kind=kind).ap()
        if name in p.dbg:
            p.outputs[name] = a
        return a

    def dout(p, name, shape, dt=F32):
        a = p.nc.dram_tensor(name, list(shape), dt, kind="ExternalOutput").ap()
        p.outputs[name] = a
        return a


def kc_view(w, cols=None):
    v = w.rearrange("(c p) f -> p c f", p=128)
    if cols is not None:
        v = v[:, :, cols[0]:cols[1]]
    return v


def stage_mod(P, S, I, modv):
    m = S.mark()
    cs32 = S.sb([128, 16, 2], F32)
    cs = S.sb([128, 16, 2], BF16)
    S.dma('sp', cs32, I['cT'], 'ld_small')
    S.act(cs, cs32, AF.Silu)
    msb = S.sb([2, 6 * D], F32)
    mb = S.sb([2, 6 * D], F32)
    gA = S.sb([2, D], F32)
    gF = S.sb([2, D], F32)
    wb = [S.sb([128, 16, 512], BF16) for _ in range(3)]
    k = 0
    for l in range(2):
        S.dma('sp', mb, I['mod_b'][l:l + 1, :].to_broadcast([2, 6 * D]), 'ld_small')
        S.dma('sp', gA, I['norm_attn_g'][l:l + 1, :].to_broadcast([2, D]), 'ld_small')
        S.dma('sp', gF, I['norm_ffn_g'][l:l + 1, :].to_broadcast([2, D]), 'ld_small')
        for j in range(24):
            w = wb[k % 3]
            S.dma('pool', w, kc_view(I['mod_w'][l], (j * 512, (j + 1) * 512)), 'ld_w%d' % (k % 3))
            ps = S.psum(k % 2, [2, 512])
            S.mm(ps, [(cs[:, c, :], w[:, c, :]) for c in range(16)])
            S.tt('dve', msb[:, j * 512:(j + 1) * 512], ps, mb[:, j * 512:(j + 1) * 512], ALU.add)
            k += 1
        S.stt(msb[:, D:2 * D], msb[:, D:2 * D], 1.0, gA, ALU.add, ALU.mult)
        S.stt(msb[:, 4 * D:5 * D], msb[:, 4 * D:5 * D], 1.0, gF, ALU.add, ALU.mult)
        S.dma('sp', modv[l], msb, 'st_small')
    S.release(m)


def load_bc(S, dst, modv_l, row, k, dom):
    S.dma('sp', dst, modv_l[row:row + 1, k * D:(k + 1) * D].to_broadcast([128, D]), dom)


def norm_mod_transpose(S, xt, G, Sf, hT_dst, ident_bf, junk, hb, small, ps_banks):
    ssq = small[:, 0:1]
    S.act(junk, xt, AF.Square)
    S.op('dve', lambda e, ssq=ssq, junk=junk: e.tensor_reduce(out=ssq, in_=junk, axis=mybir.AxisListType.X, op=ALU.add), [junk], [ssq])
    S.ts('dve', ssq, ssq, 1.0 / D, ALU.mult, EPS, ALU.add)
    S.act(ssq, ssq, AF.Sqrt)
    S.recip(ssq, ssq)
    S.stt(junk, xt, ssq, G, ALU.mult, ALU.mult)
    S.tt('pool', hb, junk, Sf, ALU.add)
    for half in range(2):
        ps = S.psum(ps_banks[half], [128, 8, 128], BF16)
        for c in range(8):
            S.tr(ps[:, c, :], hb[:, (half * 8 + c) * 128:(half * 8 + c + 1) * 128], ident_bf)
        if half == 0:
            S.cp('act', hT_dst[:, 0:8, :], ps)
        else:
            S.cp('dve', hT_dst[:, 8:16, :], ps)


def stage_A(P, S, I, xres, modv_l, kS, kG, hT, ident_bf, tiles, ps_banks=(6, 7)):
    m = S.mark()
    Gl = S.sb([128, D], F32)
    Sl = S.sb([128, D], F32)
    Gc = S.sb([128, D], F32)
    Sc = S.sb([128, D], F32)
    load_bc(S, Gl, modv_l, 0, kG, 'ld_bc')
    load_bc(S, Sl, modv_l, 0, kS, 'ld_bc')
    load_bc(S, Gc, modv_l, 1, kG, 'ld_bc')
    load_bc(S, Sc, modv_l, 1, kS, 'ld_bc')
    xts = [S.sb([128, D], F32) for _ in range(2)]
    junk = S.sb([128, D], F32)
    hb = S.sb([128, D], BF16)
    small = S.sb([128, 4], F32)
    for n, (ti, dc) in enumerate(tiles):
        xt = xts[n % 2]
        S.dma('sp', xt, xres[ti * 128:(ti + 1) * 128, :], 'ld_x%d' % (n % 2))
        isctx = ti < CTX // 128
        norm_mod_transpose(S, xt, Gc if isctx else Gl, Sc if isctx else Sl, hT[:, :, dc:dc + 128], ident_bf,
                           junk, hb, small, ps_banks)
    S.release(m)


def stage_inproj(P, S, w_in, F, hT, segs, PF, PV, wdom='ld_w'):
    m = S.mark()
    wb = [S.sb([128, 16, 512], BF16) for _ in range(3)]
    stg = [S.sb([128, 512], F32) for _ in range(3)]
    stgb = [S.sb([128, 512], BF16) for _ in range(2)]
    nblk = (F + 511) // 512
    k = 0
    kk = 0
    ttiles = [(t0, min(512, T - t0)) for t0 in range(0, T, 512)]
    for bi in range(nblk):
        c0 = bi * 512
        c1 = min(F, c0 + 512)
        w = wb[bi % 3]
        S.dma('pool', w[:, :, 0:c1 - c0], kc_view(w_in, (c0, c1)), '%s%d' % (wdom, bi % 3))
        for (lo, hi, kind, func, base) in segs:
            a = max(lo, c0)
            b = min(hi, c1)
            if a >= b:
                continue
            if kind == 'fm':
                for cc in range(a, b, 128):
                    ncol = min(128, b - cc)
                    for (t0, tn) in ttiles:
                        ps = S.psum(k % 4, [ncol, tn])
                        S.mm(ps, [(w[:, c, cc - c0:cc - c0 + ncol], hT[:, c, t0:t0 + tn]) for c in range(16)])
                        st = stg[k % 3]
                        if k % 2 == 0 or func != AF.Copy:
                            S.act(st[0:ncol, 0:tn], ps, func)
                        else:
                            S.cp('dve', st[0:ncol, 0:tn], ps)
                        S.dma('sp', PF[base + cc - lo: base + cc - lo + ncol, t0:t0 + tn], st[0:ncol, 0:tn], 'st_pf%d' % (k % 3))
                        k += 1
            else:
                wd = b - a
                for ti in range(NT):
                    ps = S.psum(4 + kk % 2, [128, wd])
                    S.mm(ps, [(hT[:, c, ti * 128:(ti + 1) * 128], w[:, c, a - c0:b - c0]) for c in range(16)])
                    st = stgb[kk % 2][:, 0:wd]
                    if kk % 2 == 0:
                        S.cp('act', st, ps)
                    else:
                        S.cp('dve', st, ps)
                    S.dma('sp', PV[ti * 128:(ti + 1) * 128, base + a - lo: base + b - lo], st, 'st_pv%d' % (kk % 2))
                    kk += 1
    S.release(m)


LT_TILES = [(0, 256)] + [(256 + 512 * i, 512) for i in range(4)]


def bc_mid(ap, n):
    return ap.to_broadcast([ap.shape[0], ap.shape[1], n])


def ssq_rstd(S, chunks, n, dim, ones_bf, sqb, rstd_out, bank):
    ps = S.psum(bank, [128, n])
    for i, ch in enumerate(chunks):
        R = ch.shape[0]
        sq = sqb[i % len(sqb)][0:R, 0:n]
        S.act(sq, ch, AF.Square)
        S.mm1(ps, ones_bf[0:R, :], sq, i == 0, i == len(chunks) - 1)
    S.ts('dve', rstd_out, ps, 1.0 / dim, ALU.mult, EPS, ALU.add)
    S.act(rstd_out, rstd_out, AF.Sqrt)
    S.recip(rstd_out, rstd_out)


def rope_fm(S, x32, R, n, cos, sin, perm_bf, out_bf, xb, tb, bank):
    S.cp('pool', xb[0:R, 0:n], x32)
    ps = S.psum(bank, [R, n])
    S.mm(ps, [(perm_bf[0:R, 0:R], xb[0:R, 0:n])])
    S.tt('pool', tb[0:R, 0:n], x32, cos, ALU.mult)
    S.tt('dve', x32, ps, sin, ALU.mult)
    S.tt('dve', out_bf, x32, tb[0:R, 0:n], ALU.add)


def attn_core(S, qparts, kparts, v, q0, nq, stiles, scale, dst, ones_bf, PTs, rden, par, cnt):
    oT = S.psum(2 + 2 * par, [128, nq])
    den = S.psum(3 + 2 * par, [128, nq])
    ns = len(stiles)
    for i, si in enumerate(stiles):
        sc = S.psum(cnt[0] % 2, [128, nq])
        S.mm(sc, [(kp[:, si * 128:(si + 1) * 128], qp[:, q0:q0 + nq]) for kp, qp in zip(kparts, qparts)])
        pt = PTs[cnt[0] % 2][:, 0:nq]
        cnt[0] += 1
        S.act(pt, sc, AF.Exp, scale=scale)
        S.mm1(oT, v[:, si, :], pt, i == 0, i == ns - 1)
        S.mm1(den, ones_bf, pt, i == 0, i == ns - 1)
    S.recip(rden[:, 0:nq], den)
    S.tt('dve', dst, oT, rden[:, 0:nq], ALU.mult)


def stage_mla(P, S, I, PF, mixT, C, need_ctx=True):
    import os
    ROT = 0 if os.environ.get('MLA_NOROPE') == '1' else CTX
    ROT = ROT if ROT else 10 ** 9
    m = S.mark()
    ones_bf = C['ones_bf']
    wuq = S.sb([128, 4, 1536], BF16)
    wukv = S.sb([128, 2, 2048], BF16)
    S.dma('pool', wuq, kc_view(I['mla_w_uq']), 'ld_w0')
    S.dma('pool', wukv, kc_view(I['mla_w_ukv']), 'ld_w1')
    gq = S.sb([128, 4], F32)
    gkv = S.sb([128, 2], F32)
    S.dma('sp', gq, I['mla_q_gT'], 'ld_small')
    S.dma('sp', gkv, I['mla_kv_gT'], 'ld_small')
    cosB = S.sb([64, LAT], F32)
    sinB = S.sb([64, LAT], F32)
    S.dma('sp', cosB, I['cosB'], 'ld_small')
    S.dma('sp', sinB, I['sinB'], 'ld_small')
    nqT = S.sb([128, 4, T], BF16)
    nkvT = S.sb([128, 2, T], BF16)
    krT = S.sb([64, T], BF16)
    sqb = [S.sb([128, 512], BF16) for _ in range(2)]
    rstd = S.sb([128, 512], F32)
    xb = S.sb([128, 512], BF16)
    tb = S.sb([128, 512], F32)
    x32 = S.sb([64, 512], F32)
    m1 = S.mark()
    p5 = S.sb([128, 4, 512], F32)
    p6 = S.sb([128, 2, 512], F32)
    p7 = S.sb([64, 512], F32)
    for (t0, n) in LT_TILES:
        S.dma('sp', p5[:, :, 0:n], PF[5120:5632, t0:t0 + n].rearrange('(c p) t -> p c t', p=128), 'ld_p5')
        S.dma('sp', p6[:, :, 0:n], PF[5632:5888, t0:t0 + n].rearrange('(c p) t -> p c t', p=128), 'ld_p6')
        S.dma('sp', p7[:, 0:n], PF[5888:5952, t0:t0 + n], 'ld_p7')
        ssq_rstd(S, [p5[:, c, 0:n] for c in range(4)], n, 512, ones_bf, sqb, rstd[:, 0:n], 6)
        for c in range(4):
            S.stt(nqT[:, c, t0:t0 + n], p5[:, c, 0:n], gq[:, c:c + 1], rstd[:, 0:n], ALU.mult, ALU.mult)
        ssq_rstd(S, [p6[:, c, 0:n] for c in range(2)], n, 256, ones_bf, sqb, rstd[:, 0:n], 7)
        for c in range(2):
            S.stt(nkvT[:, c, t0:t0 + n], p6[:, c, 0:n], gkv[:, c:c + 1], rstd[:, 0:n], ALU.mult, ALU.mult)
        if t0 >= ROT:
            l0 = t0 - CTX
            rope_fm(S, p7[:, 0:n], 64, n, cosB[:, l0:l0 + n], sinB[:, l0:l0 + n], C['perm64_bf'], krT[:, t0:t0 + n], xb, tb, 6)
        else:
            S.cp('pool', krT[:, t0:t0 + n], p7[:, 0:n])
    S.release(m1)
    hb = []
    for _ in range(1):
        hb.append((S.sb([128, T], BF16), S.sb([64, T], BF16), S.sb([128, T], BF16), S.sb([128, NT, 128], BF16)))
    PTs = [S.sb([128, 512], BF16) for _ in range(2)]
    rden = S.sb([128, 512], F32)
    cnt = [0]
    scale = float((128 + 64) ** -0.5)
    par = 0
    for h in range(8):
        qn, qr, kn, vh = hb[0]
        for (t0, n) in LT_TILES:
            ps = S.psum(6, [128, n])
            S.mm(ps, [(wuq[:, c, h * 192:h * 192 + 128], nqT[:, c, t0:t0 + n]) for c in range(4)])
            S.cp('act', qn[:, t0:t0 + n], ps)
            ps2 = S.psum(7, [64, n])
            S.mm(ps2, [(wuq[:, c, h * 192 + 128:h * 192 + 192], nqT[:, c, t0:t0 + n]) for c in range(4)])
            if t0 >= ROT:
                l0 = t0 - CTX
                S.cp('act', x32[:, 0:n], ps2)
                rope_fm(S, x32[:, 0:n], 64, n, cosB[:, l0:l0 + n], sinB[:, l0:l0 + n], C['perm64_bf'], qr[:, t0:t0 + n], xb, tb, 7)
            else:
                S.cp('act', qr[:, t0:t0 + n], ps2)
            ps = S.psum(6, [128, n])
            S.mm(ps, [(wukv[:, c, h * 256:h * 256 + 128], nkvT[:, c, t0:t0 + n]) for c in range(2)])
            S.cp('dve', kn[:, t0:t0 + n], ps)
            nb = n // 128
            psv = S.psum(7, [128, nb, 128])
            for j in range(nb):
                tk = t0 + j * 128
                S.mm(psv[:, j, :], [(nkvT[:, c, tk:tk + 128], wukv[:, c, h * 256 + 128:(h + 1) * 256]) for c in range(2)])
            S.cp('dve', vh[:, t0 // 128:t0 // 128 + nb, :], psv)
        if need_ctx:
            attn_core(S, [qn, qr], [kn, krT], vh, 0, 256, [0, 1], scale, mixT[:, 8 + h, 0:256], ones_bf, PTs, rden, par, cnt)
            par ^= 1
        for qi in range(4):
            q0 = CTX + 512 * qi
            attn_core(S, [qn, qr], [kn, krT], vh, q0, 512, list(range(NT)), scale, mixT[:, 8 + h, q0:q0 + 512], ones_bf, PTs, rden, par, cnt)
            par ^= 1
    S.release(m)


def stage_gqa(P, S, I, PF, PV, mixT, C):
    m = S.mark()
    ones_bf = C['ones_bf']
    gq = S.sb([128, 1], F32)
    gk = S.sb([128, 1], F32)
    S.dma('sp', gq, I['gqa_q_gT'], 'ld_small')
    S.dma('sp', gk, I['gqa_k_gT'], 'ld_small')
    cosC = S.sb([128, LAT], F32)
    sinC = S.sb([128, LAT], F32)
    S.dma('sp', cosC, I['cosC'], 'ld_small')
    S.dma('sp', sinC, I['sinC'], 'ld_small')
    kT = S.sb([128, T], BF16)
    qT = S.sb([128, T], BF16)
    vh = S.sb([128, NT, 128], BF16)
    sqb = [S.sb([128, 512], BF16) for _ in range(2)]
    rstd = S.sb([128, 512], F32)
    xb = S.sb([128, 512], BF16)
    tb = S.sb([128, 512], F32)
    x32 = S.sb([128, 512], F32)
    p = S.sb([128, 512], F32)
    PTs = [S.sb([128, 512], BF16) for _ in range(2)]
    rden = S.sb([128, 512], F32)
    cnt = [0]
    par = 0
    scale = float(128 ** -0.5)

    def prep(row0, gcol, dst, tiles):
        for (t0, n) in tiles:
            S.dma('sp', p[:, 0:n], PF[row0:row0 + 128, t0:t0 + n], 'ld_p5')
            ssq_rstd(S, [p[:, 0:n]], n, 128, ones_bf, sqb, rstd[:, 0:n], 6)
            if t0 >= CTX:
                l0 = t0 - CTX
                S.stt(x32[:, 0:n], p[:, 0:n], gcol, rstd[:, 0:n], ALU.mult, ALU.mult)
                rope_fm(S, x32[:, 0:n], 128, n, cosC[:, l0:l0 + n], sinC[:, l0:l0 + n], C['perm128_bf'], dst[:, t0:t0 + n], xb, tb, 7)
            else:
                S.stt(dst[:, t0:t0 + n], p[:, 0:n], gcol, rstd[:, 0:n], ALU.mult, ALU.mult)

    for g in range(2):
        prep(1024 + g * 128, gk[:, 0:1], kT, LT_TILES)
        S.dma('sp', vh, PV[:, g * 128:(g + 1) * 128].rearrange('(j p) v -> p j v', p=128), 'ld_v0')
        for i in range(4):
            h = g * 4 + i
            prep(h * 128, gq[:, 0:1], qT, LT_TILES[1:])
            for qi in range(4):
                q0 = CTX + 512 * qi
                attn_core(S, [qT], [kT], vh, q0, 512, list(range(NT)), scale, mixT[:, h, q0:q0 + 512], ones_bf, PTs, rden, par, cnt)
                par ^= 1
    S.release(m)


def stage_scan(P, S, I, cfg, PF, PV, OF, mixT, C):
    H = cfg['H']
    DVH = cfg['DVH']
    dv = 128 * DVH
    X = H * DVH
    kind = cfg['kind']
    vbase = cfg.get('vbase', 0)
    m = S.mark()
    ident_bf = C['ident_bf']
    ones_bf = C['ones_bf']
    NB = 1
    qt = [S.sb([128, H, 512], BF16) for _ in range(NB)]
    kt = [S.sb([128, H, 512], BF16) for _ in range(NB)]
    kh = [S.sb([64, H, 8, 128], BF16) for _ in range(NB)]
    vv = [S.sb([64, H, 8, dv], BF16) for _ in range(NB)]
    dec = [S.sb([128, H, 8], F32) for _ in range(NB)]
    tq = [S.sb([128, 512], F32) for _ in range(2)]
    tk = [S.sb([128, 512], F32) for _ in range(2)]
    tg = [S.sb([128, 512], F32) for _ in range(2)]
    tc_ = [S.sb([128, 512], F32) for _ in range(2)]
    ta = [S.sb([128, 512], F32) for _ in range(2)]
    te = [S.sb([128, 512], F32) for _ in range(2)]
    kht = [S.sb([128, 512], BF16) for _ in range(2)]
    Sst = S.sb([128, H, dv], F32)
    Sbf = S.sb([128, H, dv], BF16)
    otile = S.sb([128, X, 512], F32)
    scmb = [S.sb([64, H, 64], BF16) for _ in range(2)]
    gcol = S.sb([128, DVH], F32)
    S.dma('sp', gcol, cfg['gcol'], 'ld_small')
    if kind == 'hgrn':
        lbt = S.sb([128, 2, 3, 8], F32)
        S.dma('sp', lbt, I['lbT'], 'ld_small')
        S.act(lbt, lbt, AF.Exp)
        lsum = S.sb([128, 2, 8], F32)
        S.tt('dve', lsum, lbt[:, :, 0, :], lbt[:, :, 1, :], ALU.add)
        S.tt('dve', lsum, lsum, lbt[:, :, 2, :], ALU.add)
        S.recip(lsum, lsum)
        lb = S.sb([128, 2, 8], F32)
        oml = S.sb([128, 2, 8], F32)
        S.tt('dve', lb, lbt[:, :, 0, :], lsum, ALU.mult)
        S.ts('dve', oml, lb, -1.0, ALU.mult, 1.0, ALU.add)
    else:
        wa2 = S.sb([16, 2, 512], BF16)
        S.dma('pool', wa2, I['gla_w_a2'].rearrange('d r f -> r d f'), 'ld_w0')
        negb = S.sb([128, 2, 4], F32)
        S.dma('sp', negb, I['gla_bT'], 'ld_small')
        S.ts('dve', negb, negb, -1.0, ALU.mult)
        a32 = S.sb([16, 512], F32)
        abf = S.sb([16, 512], BF16)
    import os
    _dirs = [int(x) for x in os.environ.get('SCAN_DIRS', '0,1').split(',')]
    _nt = int(os.environ.get('SCAN_TILES', '5'))
    _noch = os.environ.get('SCAN_NOCHUNK') == '1'
    _nopost = os.environ.get('SCAN_NOPOST') == '1'
    k = 0
    for d in _dirs:
        order = LT_TILES if d == 0 else [LT_TILES[0]] + LT_TILES[:0:-1]
        mask = C['maskf'] if d == 0 else C['maskb']
        mask_b = mask.unsqueeze(1).to_broadcast([64, H, 64])
        S.memset('dve', Sst, 0.0)
        S.memset('pool', Sbf, 0.0)
        for ti_, (t0, n) in enumerate(order[:_nt]):
            nch = n // 64
            b = ti_ % NB
            if kind == 'gla':
                S.dma('sp', a32[:, 0:n], PF[cfg['abase'] + 16 * d: cfg['abase'] + 16 * d + 16, t0:t0 + n], 'ld_a')
                S.cp('act', abf[:, 0:n], a32[:, 0:n])
            for h in range(H):
                r = k % 2
                k += 1
                q = tq[r][:, 0:n]
                kk = tk[r][:, 0:n]
                g = tg[r][:, 0:n]
                c = tc_[r][:, 0:n]
                a = ta[r][:, 0:n]
                e_ = te[r][:, 0:n]
                S.dma('sp', q, PF[cfg['qbase'] + h * 128: cfg['qbase'] + (h + 1) * 128, t0:t0 + n], 'ld_q%d' % r)
                if kind == 'hgrn':
                    lo = 1024 * (1 + d) + h * 128
                    S.dma('sp', kk, PF[lo:lo + 128, t0:t0 + n], 'ld_k%d' % r)
                    S.act(kk, kk, AF.Sigmoid)
                    S.ts('dve', kk, kk, oml[:, d, h:h + 1], ALU.mult, lb[:, d, h:h + 1], ALU.add)
                    S.act(g, kk, AF.Ln)
                    S.ts('pool', kk, kk, -1.0, ALU.mult, 1.0, ALU.add)
                else:
                    S.dma('sp', kk, PF[cfg['kbase'] + h * 128: cfg['kbase'] + (h + 1) * 128, t0:t0 + n], 'ld_k%d' % r)
                    zp = S.psum(7, [128, n])
                    S.mm(zp, [(wa2[:, d, h * 128:(h + 1) * 128], abf[:, 0:n])])
                    S.act(e_, zp, AF.Exp, scale=-1.0, bias=negb[:, d, h:h + 1])
                    S.act(g, e_, AF.Ln, bias=1.0)
                    S.ts('dve', g, g, -1.0 / 16.0, ALU.mult)
                S.op('dve', lambda e, c=c, g=g, n=n: e.tensor_tensor_scan(out=c, data0=C['rmask'][:, 0:n], data1=g, initial=0.0,
                                                                            op0=ALU.mult, op1=ALU.add), [C['rmask'][:, 0:n], g], [c])
                c3 = c.rearrange('p (j s) -> p j s', s=64)
                tot = c3[:, :, 63:64]
                if d == 1:
                    S.tt('dve', a, g, c, ALU.subtract)
                    S.tt('dve', a.rearrange('p (j s) -> p j s', s=64), a.rearrange('p (j s) -> p j s', s=64), bc_mid(tot, 64), ALU.add)
                else:
                    a = c
                S.act(e_, a, AF.Exp)
                S.stt(qt[b][:, h, 0:n], q, float(cfg['qscale']), e_, ALU.mult, ALU.mult)
                S.act(e_, a, AF.Exp, scale=-1.0)
                S.tt('dve', kt[b][:, h, 0:n], kk, e_, ALU.mult)
                S.tt('dve', g.rearrange('p (j s) -> p j s', s=64), bc_mid(tot, 64), a.rearrange('p (j s) -> p j s', s=64), ALU.subtract)
                S.act(g, g, AF.Exp)
                S.tt('pool', kht[r][:, 0:n], kk, g, ALU.mult)
                S.act(dec[b][:, h, 0:nch], c3[:, :, 63], AF.Exp)
                psT = S.psum(6, [64, 8, 128], BF16)
                for j in range(nch):
                    S.tr(psT[:, j, :], kht[r][:, j * 64:(j + 1) * 64], ident_bf)
                S.cp('act', kh[b][:, h, 0:nch, :], psT[:, 0:nch, :])
                S.dma('sp', vv[b][:, h, 0:nch, :], PV[t0:t0 + n, vbase + h * dv:vbase + (h + 1) * dv].rearrange('(j p) v -> p j v', p=64), 'ld_v%d' % b)
            chs = list(range(nch)) if d == 0 else list(range(nch - 1, -1, -1))
            if _noch:
                chs = []
            for ji, j in enumerate(chs):
                sc = S.psum(ji % 2, [64, H, 64])
                for h in range(H):
                    S.mm(sc[:, h, :], [(kt[b][:, h, j * 64:(j + 1) * 64], qt[b][:, h, j * 64:(j + 1) * 64])])
                scm = scmb[ji % 2]
                S.tt('dve', scm, sc, mask_b, ALU.mult)
                ops = S.psum(2 + ji % 2, [128, X, 64])
                kv = S.psum(4, [128, H, dv])
                for h in range(H):
                    for hf in range(DVH):
                        S.mm(ops[:, h * DVH + hf, :], [(vv[b][:, h, j, hf * 128:(hf + 1) * 128], scm[:, h, :]),
                                                       (Sbf[:, h, hf * 128:(hf + 1) * 128], qt[b][:, h, j * 64:(j + 1) * 64])])
                    S.mm(kv[:, h, :], [(kh[b][:, h, j, :], vv[b][:, h, j, :])])
                    S.stt(Sst[:, h, :], Sst[:, h, :], dec[b][:, h, j:j + 1], kv[:, h, :], ALU.mult, ALU.add)
                    S.cp('act', Sbf[:, h, :], Sst[:, h, :])
                S.cp('act', otile[:, :, j * 64:(j + 1) * 64], ops)
            OFv = OF.rearrange('(x p) t -> p x t', p=128)[:, :, t0:t0 + n]
            if _nopost:
                continue
            if d == 0:
                S.dma('sp', OFv, otile[:, :, 0:n], 'st_of')
            else:
                m2 = S.mark()
                sqb = [kht[0], kht[1]]
                for h in range(H):
                    r = k % 2
                    k += 1
                    for hf in range(DVH):
                        x = h * DVH + hf
                        oft = tc_[(r + hf) % 2][:, 0:n]
                        S.dma('sp', oft, OF[x * 128:(x + 1) * 128, t0:t0 + n], 'ld_of%d' % ((r + hf) % 2))
                        S.tt('pool', otile[:, x, 0:n], otile[:, x, 0:n], oft, ALU.add)
                    rstd = tq[r][:, 0:n]
                    ssq_rstd(S, [otile[:, h * DVH + hf, 0:n] for hf in range(DVH)], n, dv, ones_bf, sqb, rstd, 7)
                    for hf in range(DVH):
                        x = h * DVH + hf
                        gt = tk[(r + hf) % 2][:, 0:n]
                        gl = cfg['gbase'] + x * 128
                        S.dma('sp', gt, PF[gl:gl + 128, t0:t0 + n], 'ld_g%d' % ((r + hf) % 2))
                        S.stt(otile[:, x, 0:n], otile[:, x, 0:n], gcol[:, hf:hf + 1], rstd, ALU.mult, ALU.mult)
                        S.tt('dve', mixT[:, cfg['mixbase'] + x, t0:t0 + n], otile[:, x, 0:n], gt, ALU.mult)
                S.release(m2)
    S.release(m)


def stage_outproj(P, S, I, w_out, mixT, modv_l, xsrc, XR, tiles):
    m = S.mark()
    wb = [S.sb([128, 16, 512], BF16) for _ in range(2)]
    gl = [S.sb([128, 512], F32) for _ in range(2)]
    gc = [S.sb([128, 512], F32) for _ in range(2)]
    xt = [S.sb([128, 512], F32) for _ in range(3)]
    yt = [S.sb([128, 512], F32) for _ in range(3)]
    k = 0
    for cb in range(4):
        w = wb[cb % 2]
        S.dma('pool', w, kc_view(w_out, (cb * 512, (cb + 1) * 512)), 'ld_w%d' % (cb % 2))
        S.dma('sp', gl[cb % 2], modv_l[0:1, 2 * D + cb * 512: 2 * D + (cb + 1) * 512].to_broadcast([128, 512]), 'ld_bc')
        S.dma('sp', gc[cb % 2], modv_l[1:2, 2 * D + cb * 512: 2 * D + (cb + 1) * 512].to_broadcast([128, 512]), 'ld_bc')
        for ti in tiles:
            ps = S.psum(k % 4, [128, 512])
            S.mm(ps, [(mixT[:, c, ti * 128:(ti + 1) * 128], w[:, c, :]) for c in range(16)])
            x = xt[k % 3]
            y = yt[k % 3]
            S.dma('sp', x, xsrc[ti * 128:(ti + 1) * 128, cb * 512:(cb + 1) * 512], 'ld_xo%d' % (k % 3))
            gate = gc[cb % 2] if ti < CTX // 128 else gl[cb % 2]
            S.tt('dve', y, ps, gate, ALU.mult)
            S.tt('pool', y, y, x, ALU.add)
            S.dma('sp', XR[ti * 128:(ti + 1) * 128, cb * 512:(cb + 1) * 512], y, 'st_xo%d' % (k % 3))
            k += 1
    S.release(m)


def stage_moe(P, S, I, l, modv_l, XR, C, supers, final_out=None):
    ident = C['ident']
    ident_bf = C['ident_bf']
    w1 = I['moe_w1'][l]
    w3 = I['moe_w3'][l]
    w2 = I['moe_w2'][l]
    for tiles in supers:
        nsub = len(tiles)
        TS = nsub * 128
        m = S.mark()
        h2T = S.sb([128, 16, TS], BF16)
        yacc = S.sb([128, nsub, D], F32)
        comb = S.sb([128, nsub, 16], F32)
        m1 = S.mark()
        Gl = S.sb([128, D], F32)
        Sl = S.sb([128, D], F32)
        Gc = S.sb([128, D], F32)
        Sc = S.sb([128, D], F32)
        load_bc(S, Gl, modv_l, 0, 4, 'ld_bc')
        load_bc(S, Sl, modv_l, 0, 3, 'ld_bc')
        if any(t < CTX // 128 for t in tiles):
            load_bc(S, Gc, modv_l, 1, 4, 'ld_bc')
            load_bc(S, Sc, modv_l, 1, 3, 'ld_bc')
        xts = [S.sb([128, D], F32) for _ in range(2)]
        junk = S.sb([128, D], F32)
        hf = S.sb([128, D], F32)
        hb = S.sb([128, D], BF16)
        h32T = S.sb([128, 16, 128], F32)
        rw = S.sb([128, 16, 16], F32)
        S.dma('sp', rw, kc_view(I['router_w']), 'ld_small')
        rb = S.sb([128, 16], F32)
        S.dma('sp', rb, I['router_b'].to_broadcast([128, 16]), 'ld_small')
        sm = S.sb([128, 4], F32)
        sc_ = S.sb([128, 16], F32)
        sel = S.sb([128, 16], F32)
        t4 = [S.sb([128, 4], F32) for _ in range(8)]
        em = S.sb([128, 16], F32)
        for n, ti in enumerate(tiles):
            xt = xts[n % 2]
            S.dma('sp', xt, XR[ti * 128:(ti + 1) * 128, :], 'ld_x%d' % (n % 2))
            isctx = ti < CTX // 128
            G = Gc if isctx else Gl
            Sf = Sc if isctx else Sl
            ssq = sm[:, 0:1]
            S.act(junk, xt, AF.Square)
            S.op('dve', lambda e, ssq=ssq, junk=junk: e.tensor_reduce(out=ssq, in_=junk, axis=mybir.AxisListType.X, op=ALU.add), [junk], [ssq])
            S.ts('dve', ssq, ssq, 1.0 / D, ALU.mult, EPS, ALU.add)
            S.act(ssq, ssq, AF.Sqrt)
            S.recip(ssq, ssq)
            S.stt(junk, xt, ssq, G, ALU.mult, ALU.mult)
            S.tt('pool', hf, junk, Sf, ALU.add)
            S.cp('act', hb, hf)
            for half in range(2):
                ps = S.psum(6 + half, [128, 8, 128], BF16)
                for c in range(8):
                    S.tr(ps[:, c, :], hb[:, (half * 8 + c) * 128:(half * 8 + c + 1) * 128], ident_bf)
                S.cp('act' if half == 0 else 'dve', h2T[:, half * 8:half * 8 + 8, n * 128:(n + 1) * 128], ps)
            for q4 in range(4):
                ps = S.psum(q4 % 2, [128, 4, 128])
                for c in range(4):
                    cc = q4 * 4 + c
                    S.tr(ps[:, c, :], hf[:, cc * 128:(cc + 1) * 128], ident)
                S.cp('act' if q4 % 2 == 0 else 'dve', h32T[:, q4 * 4:q4 * 4 + 4, :], ps)
            lg = S.psum(2, [128, 16])
            S.mm(lg, [(h32T[:, c, :], rw[:, c, :]) for c in range(16)])
            S.act(sc_, lg, AF.Sigmoid)
            S.tt('dve', sel, sc_, rb, ALU.add)
            s4 = sel.rearrange('p (g e) -> p g e', e=4)
            a_, b_, c_, d_ = s4[:, :, 0], s4[:, :, 1], s4[:, :, 2], s4[:, :, 3]
            m1_, n1_, m2_, n2_, top1, xx, yy, sec = t4
            S.tt('dve', m1_, a_, b_, ALU.max)
            S.tt('dve', n1_, a_, b_, ALU.min)
            S.tt('dve', m2_, c_, d_, ALU.max)
            S.tt('dve', n2_, c_, d_, ALU.min)
            S.tt('dve', top1, m1_, m2_, ALU.max)
            S.tt('dve', xx, m1_, m2_, ALU.min)
            S.tt('dve', yy, n1_, n2_, ALU.max)
            S.tt('dve', sec, xx, yy, ALU.max)
            S.tt('dve', top1, top1, sec, ALU.add)
            gmax = sm[:, 1:2]
            S.op('dve', lambda e, gmax=gmax, top1=top1: e.tensor_reduce(out=gmax, in_=top1, axis=mybir.AxisListType.X, op=ALU.max), [top1], [gmax])
            S.ts('dve', xx, top1, gmax, ALU.is_ge)
            e4 = em.rearrange('p (g e) -> p g e', e=4)
            S.tt('dve', e4, s4, bc_mid(sec.unsqueeze(2), 4), ALU.is_ge)
            S.tt('dve', e4, e4, bc_mid(xx.unsqueeze(2), 4), ALU.mult)
            S.tt('dve', em, em, sc_, ALU.mult)
            den = sm[:, 2:3]
            S.op('dve', lambda e, den=den: e.tensor_reduce(out=den, in_=em, axis=mybir.AxisListType.X, op=ALU.add), [em], [den])
            S.recip(den, den)
            S.ts('dve', comb[:, n, :], em, den, ALU.mult)
        S.release(m1)
        m1 = S.mark()
        NBUF = 2
        w1b = [S.sb([128, 16, 256], BF16) for _ in range(NBUF)]
        w3b = [S.sb([128, 16, 256], BF16) for _ in range(NBUF)]
        w2b = [S.sb([128, 2, D], BF16) for _ in range(NBUF)]
        s1 = [S.sb([128, 512], F32) for _ in range(2)]
        hid = [S.sb([128, 2, 512], BF16) for _ in range(2)]
        ntiles = [(t0, min(512, TS - t0)) for t0 in range(0, TS, 512)]
        k = 0
        kq = 0
        for e in range(NE):
            for hfx in range(2):
                bsel = k % NBUF
                f0 = hfx * 256
                S.dma('pool', w1b[bsel], kc_view(w1[e], (f0, f0 + 256)), 'ld_w1_%d' % bsel)
                S.dma('pool', w3b[bsel], kc_view(w3[e], (f0, f0 + 256)), 'ld_w3_%d' % bsel)
                S.dma('pool', w2b[bsel], w2[e][f0:f0 + 256, :].rearrange('(c p) d -> p c d', p=128), 'ld_w2_%d' % bsel)
                first = (k == 0)
                k += 1
                for (t0, tn) in ntiles:
                    hd = hid[kq % 2]
                    for fc in range(2):
                        p1 = S.psum(0 + fc, [128, tn])
                        p3 = S.psum(2 + fc, [128, tn])
                        S.mm(p1, [(w1b[bsel][:, c, fc * 128:(fc + 1) * 128], h2T[:, c, t0:t0 + tn]) for c in range(16)])
                        S.mm(p3, [(w3b[bsel][:, c, fc * 128:(fc + 1) * 128], h2T[:, c, t0:t0 + tn]) for c in range(16)])
                        st = s1[fc][:, 0:tn]
                        S.act(st, p1, AF.Silu)
                        S.tt('dve', hd[:, fc, 0:tn], st, p3, ALU.mult)
                    kq += 1
                    for ts_ in range(tn // 128):
                        sub = t0 // 128 + ts_
                        for dc in range(4):
                            yp = S.psum(4 + (ts_ * 4 + dc) % 4, [128, 512])
                            S.mm(yp, [(hd[:, fc, ts_ * 128:(ts_ + 1) * 128], w2b[bsel][:, fc, dc * 512:(dc + 1) * 512]) for fc in range(2)])
                            ya = yacc[:, sub, dc * 512:(dc + 1) * 512]
                            if first:
                                S.ts('dve', ya, yp, comb[:, sub, e:e + 1], ALU.mult)
                            else:
                                S.stt(ya, yp, comb[:, sub, e:e + 1], ya, ALU.mult, ALU.add)
        S.release(m1)
        m1 = S.mark()
        g2l = S.sb([128, D], F32)
        g2c = S.sb([128, D], F32)
        load_bc(S, g2l, modv_l, 0, 5, 'ld_bc')
        if any(t < CTX // 128 for t in tiles):
            load_bc(S, g2c, modv_l, 1, 5, 'ld_bc')
        xts = [S.sb([128, D], F32) for _ in range(2)]
        if final_out is not None:
            fg = S.sb([128, D], F32)
            S.dma('sp', fg, I['final_norm_g'].unsqueeze(0).to_broadcast([128, D]), 'ld_bc')
            junk = S.sb([128, D], F32)
            sm = S.sb([128, 4], F32)
        for n, ti in enumerate(tiles):
            xt = xts[n % 2]
            S.dma('sp', xt, XR[ti * 128:(ti + 1) * 128, :], 'ld_x%d' % (n % 2))
            g2 = g2c if ti < CTX // 128 else g2l
            S.tt('dve', yacc[:, n, :], yacc[:, n, :], g2, ALU.mult)
            S.tt('pool', xt, xt, yacc[:, n, :], ALU.add)
            if final_out is None:
                S.dma('sp', XR[ti * 128:(ti + 1) * 128, :], xt, 'st_x%d' % (n % 2))
            else:
                ssq = sm[:, 0:1]
                S.act(junk, xt, AF.Square)
                S.op('dve', lambda e, ssq=ssq, junk=junk: e.tensor_reduce(out=ssq, in_=junk, axis=mybir.AxisListType.X, op=ALU.add), [junk], [ssq])
                S.ts('dve', ssq, ssq, 1.0 / D, ALU.mult, EPS, ALU.add)
                S.act(ssq, ssq, AF.Sqrt)
                S.recip(ssq, ssq)
                S.stt(xt, xt, ssq, fg, ALU.mult, ALU.mult)
                r0 = ti * 128 - CTX
                S.dma('sp', final_out[r0:r0 + 128, :], xt, 'st_x%d' % (n % 2))
        S.release(m1)
        S.release(m)


def build(dbg=(), stop_after=None, sbuf_kb=192):
    P = Prog(dbg)
    nc = P.nc
    I = {}
    I['xin'] = P.din('xin', [T, D])
    I['cT'] = P.din('cT', [128, 16, 2])
    I['mod_w'] = P.din('mod_w', [2, D, 6 * D])
    I['mod_b'] = P.din('mod_b', [2, 6 * D])
    I['norm_attn_g'] = P.din('norm_attn_g', [2, D])
    I['norm_ffn_g'] = P.din('norm_ffn_g', [2, D])
    I['final_norm_g'] = P.din('final_norm_g', [D])
    I['ab_w_in'] = P.din('ab_w_in', [D, 5952])
    I['ab_w_out'] = P.din('ab_w_out', [D, D])
    I['mla_w_uq'] = P.din('mla_w_uq', [512, 1536])
    I['mla_w_ukv'] = P.din('mla_w_ukv', [256, 2048])
    I['mla_q_gT'] = P.din('mla_q_gT', [128, 4])
    I['mla_kv_gT'] = P.din('mla_kv_gT', [128, 2])
    I['hgrn_gT'] = P.din('hgrn_gT', [128, 1])
    I['lbT'] = P.din('lbT', [128, 2, 3, 8])
    I['cosB'] = P.din('cosB', [64, LAT])
    I['sinB'] = P.din('sinB', [64, LAT])
    I['cd_w_in'] = P.din('cd_w_in', [D, 4640])
    I['cd_w_out'] = P.din('cd_w_out', [D, D])
    I['gqa_q_gT'] = P.din('gqa_q_gT', [128, 1])
    I['gqa_k_gT'] = P.din('gqa_k_gT', [128, 1])
    I['gla_w_a2'] = P.din('gla_w_a2', [2, 16, 512])
    I['gla_bT'] = P.din('gla_bT', [128, 2, 4])
    I['gla_gT'] = P.din('gla_gT', [128, 2])
    I['cosC'] = P.din('cosC', [128, LAT])
    I['sinC'] = P.din('sinC', [128, LAT])
    I['router_w'] = P.din('router_w', [D, 16])
    I['router_b'] = P.din('router_b', [1, 16])
    I['moe_w1'] = P.din('moe_w1', [2, NE, D, EDIM])
    I['moe_w3'] = P.din('moe_w3', [2, NE, D, EDIM])
    I['moe_w2'] = P.din('moe_w2', [2, NE, EDIM, D])
    I['consts'] = P.din('consts', [128, 1024])
    out = P.dout('out', [LAT, D])
    modv = [P.dscr('modv%d' % l, [2, 6 * D]) for l in range(2)]
    PF0 = P.dscr('PF0', [5952, T])
    PV0 = P.dscr('PV0', [T, 1024], BF16)
    OF0 = P.dscr('OF0', [1024, T])
    XR = P.dscr('XR', [T, D])
    PF1 = P.dscr('PF1', [4640, T])
    PV1 = P.dscr('PV1', [T, 1280], BF16)
    OF1 = P.dscr('OF1', [1024, T])

    with contextlib.ExitStack() as es:
        S = Sch(nc, es, sbuf_bytes=sbuf_kb * 1024)
        cst = S.sb([128, 1024], F32)
        S.dma('sp', cst, I['consts'], 'ld_small')
        C = {}
        C['ident'] = cst[:, 0:128]
        C['ident_bf'] = S.sb([128, 128], BF16)
        S.cp('dve', C['ident_bf'], cst[:, 0:128])
        C['perm128_bf'] = S.sb([128, 128], BF16)
        S.cp('dve', C['perm128_bf'], cst[:, 128:256])
        C['perm64_bf'] = S.sb([64, 64], BF16)
        S.cp('dve', C['perm64_bf'], cst[0:64, 256:320])
        C['maskf'] = cst[0:64, 320:384]
        C['maskb'] = cst[0:64, 384:448]
        C['rmask'] = cst[:, 512:1024]
        C['ones_bf'] = S.sb([128, 128], BF16)
        S.memset('dve', C['ones_bf'], 1.0)

        def dump(name, src_ap, shape, dt=F32):
            if name in P.dbg:
                d = P.dout(name, shape, dt)
                S.dma('sp', d, src_ap, 'st_small')

        def done():
            return stop_after is not None and stop_after in done.passed
        done.passed = set()

        def layer0():
            stage_mod(P, S, I, modv)
            m0 = S.mark()
            hT = S.sb([128, 16, T], BF16)
            stage_A(P, S, I, I['xin'], modv[0], 0, 1, hT, C['ident_bf'], [(i, i * 128) for i in range(NT)])
            segs = [(0, 1024, 'fm', AF.Silu, 0), (1024, 3072, 'fm', AF.Copy, 1024), (3072, 4096, 'tm', None, 0),
                    (4096, 5120, 'fm', AF.Sigmoid, 4096), (5120, 5952, 'fm', AF.Copy, 5120)]
            stage_inproj(P, S, I['ab_w_in'], 5952, hT, segs, PF0, PV0)
            S.release(m0)
            if stop_after == 'inproj0':
                return
            mixT = S.sb([128, 16, T], BF16)
            if 'skip_mla' not in P.dbg:
                stage_mla(P, S, I, PF0, mixT, C)
            if stop_after == 'mla':
                dump('mixT', mixT.rearrange('p c t -> p (c t)'), [128, 16 * T], BF16)
                return
            cfg = dict(H=8, DVH=1, kind='hgrn', qscale=128 ** -0.5, qbase=0, gbase=4096, mixbase=0, gcol=I['hgrn_gT'])
            stage_scan(P, S, I, cfg, PF0, PV0, OF0, mixT, C)
            dump('mixT', mixT.rearrange('p c t -> p (c t)'), [128, 16 * T], BF16)
            if stop_after == 'scan0':
                return
            stage_outproj(P, S, I, I['ab_w_out'], mixT, modv[0], I['xin'], XR, list(range(NT)))
            S.release(m0)
            dumpXR('XR_a')
            if stop_after == 'out0':
                return
            stage_moe(P, S, I, 0, modv[0], XR, C, [list(range(0, 9)), list(range(9, 18))])

        def dumpXR(name):
            if name in P.dbg:
                d = P.dout(name, [T, D])
                for ti in range(NT):
                    S.dma('sp', d[ti * 128:(ti + 1) * 128, :], XR[ti * 128:(ti + 1) * 128, :], 'st_small')

        def layer1():
            m0 = S.mark()
            hT = S.sb([128, 16, T], BF16)
            stage_A(P, S, I, XR, modv[1], 0, 1, hT, C['ident_bf'], [(i, i * 128) for i in range(NT)])
            segs = [(0, 1024, 'fm', AF.Copy, 0), (1024, 1280, 'fm', AF.Copy, 1024), (1280, 1536, 'tm', None, 0),
                    (1536, 2560, 'fm', AF.Copy, 1536), (2560, 3584, 'tm', None, 256),
                    (3584, 4608, 'fm', AF.Silu, 3584), (4608, 4640, 'fm', AF.Copy, 4608)]
            stage_inproj(P, S, I['cd_w_in'], 4640, hT, segs, PF1, PV1)
            S.release(m0)
            mixT = S.sb([128, 16, T], BF16)
            stage_gqa(P, S, I, PF1, PV1, mixT, C)
            cfg = dict(H=4, DVH=2, kind='gla', qscale=128 ** -0.5, qbase=1536, kbase=2048, abase=4608, gbase=3584,
                       mixbase=8, vbase=256, gcol=I['gla_gT'])
            stage_scan(P, S, I, cfg, PF1, PV1, OF1, mixT, C)
            lat_tiles = list(range(CTX // 128, NT))
            stage_outproj(P, S, I, I['cd_w_out'], mixT, modv[1], XR, XR, lat_tiles)
            S.release(m0)
            dumpXR('XR_c')
            stage_moe(P, S, I, 1, modv[1], XR, C, [lat_tiles[0:8], lat_tiles[8:16]], final_out=out)

        layer0()
        if stop_after is None:
            dumpXR('XR_b')
            layer1()
        else:
            z = S.sb([128, D], F32)
            S.memset('dve', z, 0.0)
            S.dma('sp', out[0:128, :], z, 'st_small')
        S.finish()
        print("instructions:", S.n_inst, "sbuf top", S.sb_top, "dma sems", len(S.dma_sems), sorted(S.dma_sems))
    return P


def _rope_tables(d_rope):
    quarter = d_rope // 4
    half = d_rope // 2
    freqs = (np.float32(10000.0) ** (-np.arange(quarter, dtype=np.float32) / np.float32(quarter))).astype(np.float32)
    rows = LAT // 64
    row = np.repeat(np.arange(rows, dtype=np.float32), 64)
    col = np.tile(np.arange(64, dtype=np.float32), rows)
    ang = np.concatenate([row[:, None] * freqs, col[:, None] * freqs], axis=-1).astype(np.float32)
    c = np.cos(ang).astype(np.float32).T
    s_ = np.sin(ang).astype(np.float32).T
    return np.ascontiguousarray(np.concatenate([c, c], 0)), np.ascontiguousarray(np.concatenate([-s_, s_], 0))


def _consts():
    c = np.zeros((128, 1024), np.float32)
    c[:, 0:128] = np.eye(128, dtype=np.float32)
    k = np.arange(128)
    c[(k + 64) % 128, 128 + k] = 1.0
    k = np.arange(64)
    c[(k + 32) % 64, 256 + k] = 1.0
    c[0:64, 320:384] = np.triu(np.ones((64, 64), np.float32))
    c[0:64, 384:448] = np.tril(np.ones((64, 64), np.float32))
    c[:, 512:1024] = 1.0
    c[:, 512:1024:64] = 0.0
    return c


def host_inputs(inp, b):
    f = lambda a: np.ascontiguousarray(np.asarray(a, np.float32))
    m = {}
    m['xin'] = f(np.concatenate([inp['ctx'][b], inp['x'][b]], 0))
    cc = np.stack([inp['c'][b], inp['c_ctx']], 1)
    m['cT'] = f(cc.reshape(16, 128, 2).transpose(1, 0, 2))
    for k_ in ['mod_w', 'mod_b', 'norm_attn_g', 'norm_ffn_g', 'final_norm_g', 'router_w', 'moe_w1', 'moe_w3', 'moe_w2']:
        m[k_] = f(inp[k_])
    m['ab_w_in'] = f(inp['ab_w_in'][0])
    m['ab_w_out'] = f(inp['ab_w_out'][0])
    m['mla_w_uq'] = f(inp['mla_w_uq'][0])
    m['mla_w_ukv'] = f(inp['mla_w_ukv'][0])
    m['mla_q_gT'] = f(inp['mla_q_norm_g'][0].reshape(4, 128).T)
    m['mla_kv_gT'] = f(inp['mla_kv_norm_g'][0].reshape(2, 128).T)
    m['hgrn_gT'] = f(inp['hgrn_norm_g'][0].reshape(1, 128).T)
    m['lbT'] = f(inp['hgrn_lb_logits'].reshape(2, 3, 8, 128).transpose(3, 0, 1, 2))
    m['cosB'], m['sinB'] = _rope_tables(64)
    m['cd_w_in'] = f(inp['cd_w_in'][0])
    m['cd_w_out'] = f(inp['cd_w_out'][0])
    m['gqa_q_gT'] = f(inp['gqa_q_norm_g'][0].reshape(1, 128).T)
    m['gqa_k_gT'] = f(inp['gqa_k_norm_g'][0].reshape(1, 128).T)
    m['gla_w_a2'] = f(inp['gla_w_a2'][0])
    m['gla_bT'] = f(inp['gla_b_a'][0].reshape(2, 4, 128).transpose(2, 0, 1))
    m['gla_gT'] = f(inp['gla_norm_g'][0].reshape(2, 128).T)
    m['cosC'], m['sinC'] = _rope_tables(128)
    m['router_b'] = f(inp['router_b'].reshape(1, 16))
    m['consts'] = _consts()
    return m


_PROG = None
NCORES = 4


def kernel(**inputs):
    global _PROG
    if _PROG is None:
        _PROG = build()
    P = _PROG
    inp = {k: np.asarray(v) for k, v in inputs.items()}
    in_maps = []
    for c in range(NCORES):
        m = host_inputs(inp, c % 4)
        in_maps.append({k: v for k, v in m.items() if k in P.inputs})
    res = run_bass_kernel_spmd(P.nc, in_maps, core_ids=list(range(NCORES)))
    outs = [np.asarray(res.results[b]['out'], np.float32) for b in range(4)]
    return np.stack(outs, 0)
```

```python
import contextlib
import numpy as np
import concourse.bass as bass
import concourse.mybir as mybir
from concourse.bass_utils import run_bass_kernel_spmd

F32 = mybir.dt.float32
BF16 = mybir.dt.bfloat16
AF = mybir.ActivationFunctionType
ALU = mybir.AluOpType
DT_SIZE = {F32: 4, BF16: 2}

D = 2048
CTX = 256
LAT = 2048
T = CTX + LAT
NT = T // 128
EPS = 1e-6
NE = 16
EDIM = 512


def _box(ap):
    esz = DT_SIZE[ap.dtype]
    pat = ap.ap
    off = ap.offset
    name = ap.tensor.name
    if str(ap.space) == 'DRAM':
        hi = off + sum((c - 1) * s for s, c in pat) + 1
        return (name, 0, 1, off * esz, hi * esz)
    pstep, pcnt = pat[0]
    if pstep == 0:
        p0 = 0
        f0 = off
        pcnt = 128
    else:
        p0 = off // pstep
        f0 = off - p0 * pstep
    f1 = f0 + sum((c - 1) * s for s, c in pat[1:]) + 1
    return (name, p0, p0 + pcnt, f0 * esz, f1 * esz)


def _ov(a, b):
    return a[1] < b[2] and b[1] < a[2] and a[3] < b[4] and b[3] < a[4]


class Sch:
    ENG = ('pe', 'act', 'dve', 'pool', 'sp')

    def __init__(s, nc, es, sbuf_bytes=192 * 1024):
        s.nc = nc
        s.es = es
        s.eng = {'pe': nc.tensor, 'act': nc.scalar, 'dve': nc.vector, 'pool': nc.gpsimd, 'sp': nc.sync}
        s.sem = {e: es.enter_context(nc.semaphore('sem_' + e)) for e in s.ENG}
        s.cnt = {e: 0 for e in s.ENG}
        s.waited = {}
        s.regions = {}
        s.dma_sems = {}
        s.big = es.enter_context(nc.sbuf_tensor('SB', [128, sbuf_bytes // 4], F32))
        s.sb_top = 0
        s.sb_cap = sbuf_bytes
        s.ps = es.enter_context(nc.psum_tensor('PS', [128, 4096], F32))
        s.n_inst = 0
        s.dom_map = {}
        import os
        s.npool = int(os.environ.get('NPOOL', '1000'))
        s.waitall = os.environ.get('WAITALL', '1') == '1'
        s.serial = os.environ.get('SERIAL', '0') == '1'

    def mark(s):
        return s.sb_top

    def release(s, m):
        s.sb_top = m

    @staticmethod
    def _shape(v, shape):
        if len(shape) > 2:
            names = ' '.join('d%d' % i for i in range(1, len(shape)))
            v = v.rearrange('p (%s) -> p %s' % (names, names), **{'d%d' % i: shape[i] for i in range(2, len(shape))})
        return v

    def sb(s, shape, dt):
        n = int(np.prod(shape[1:]))
        nb = (n * DT_SIZE[dt] + 63) // 64 * 64
        assert s.sb_top + nb <= s.sb_cap, ('SBUF OOM', s.sb_top, nb)
        v = s.big[0:shape[0], s.sb_top // 4:(s.sb_top + nb) // 4]
        s.sb_top += nb
        if dt != F32:
            v = v.bitcast(dt)
        return s._shape(v[:, 0:n], shape)

    def psum(s, bank, shape, dt=F32, off=0):
        n = int(np.prod(shape[1:]))
        nb = n * DT_SIZE[dt]
        assert off % 4 == 0 and off + nb <= 2048 * (8 - bank)
        v = s.ps[0:shape[0], bank * 512 + off // 4: bank * 512 + (off + nb + 3) // 4]
        if dt != F32:
            v = v.bitcast(dt)
        return s._shape(v[:, 0:n], shape)

    def _deps(s, reads, writes, me):
        deps = {}
        rb = [_box(a) for a in reads]
        wb = [_box(a) for a in writes]
        for b in rb:
            for r in s.regions.get(b[0], ()):
                if _ov(r[0], b):
                    for dom, val in r[1].items():
                        if deps.get(dom, 0) < val:
                            deps[dom] = val
        for b in wb:
            for r in s.regions.get(b[0], ()):
                if _ov(r[0], b):
                    for dom, val in r[1].items():
                        if deps.get(dom, 0) < val:
                            deps[dom] = val
                    for dom, val in r[2].items():
                        if dom != me and deps.get(dom, 0) < val:
                            deps[dom] = val
        return deps, rb, wb

    def _record(s, rb, wb, dom, val):
        for b in rb:
            lst = s.regions.setdefault(b[0], [])
            found = None
            for r in lst:
                if r[0] == b:
                    found = r
                elif _ov(r[0], b):
                    r[2][dom] = val
            if found is None:
                w = {}
                for r in lst:
                    if _ov(r[0], b):
                        for d, v in r[1].items():
                            if w.get(d, 0) < v:
                                w[d] = v
                found = [b, w, {}]
                lst.append(found)
            found[2][dom] = val
        for b in wb:
            lst = s.regions.setdefault(b[0], [])
            found = None
            for r in lst:
                if r[0] == b:
                    found = r
                elif _ov(r[0], b):
                    q = r[0]
                    if b[1] <= q[1] and q[2] <= b[2] and b[3] <= q[3] and q[4] <= b[4]:
                        r[1] = {dom: val}
                        r[2] = {}
                    else:
                        r[1][dom] = val
            if found is None:
                found = [b, {}, {}]
                lst.append(found)
            found[1] = {dom: val}
            found[2] = {}
        for b in rb + wb:
            lst = s.regions[b[0]]
            if len(lst) > 400:
                s._compact(b[0])

    def _compact(s, name):
        lst = s.regions[name]
        half = len(lst) // 2
        old = lst[:half]
        p0 = min(r[0][1] for r in old)
        p1 = max(r[0][2] for r in old)
        f0 = min(r[0][3] for r in old)
        f1 = max(r[0][4] for r in old)
        w = {}
        rd = {}
        for r in old:
            for d, v in r[1].items():
                if w.get(d, 0) < v:
                    w[d] = v
            for d, v in r[2].items():
                if rd.get(d, 0) < v:
                    rd[d] = v
        s.regions[name] = [[(name + '#old', p0, p1, f0, f1), w, rd]] + lst[half:]

    def _emit_waits(s, e, deps):
        eng = s.eng[e]
        if s.serial:
            deps = {d: v for d, v in s.cnt.items() if v > 0}
        for dom, val in deps.items():
            if dom == e and e == 'pe':
                continue
            if dom in s.dma_sems and s.waitall:
                val = s.cnt[dom]
            if s.waited.get((e, dom), 0) >= val:
                continue
            s.waited[(e, dom)] = val
            sem = s.sem[dom] if dom in s.sem else s.dma_sems[dom]
            eng.wait_ge(sem, val)
            s.n_inst += 1

    def op(s, e, fn, reads, writes):
        deps, rb, wb = s._deps(reads, writes, e)
        s._emit_waits(e, deps)
        ins = fn(s.eng[e])
        s.n_inst += 1
        s.cnt[e] += 1
        ins.then_inc(s.sem[e], 1)
        s._record(rb, wb, e, s.cnt[e])
        return ins

    def dma(s, e, out, in_, dom):
        if dom not in s.dom_map:
            s.dom_map[dom] = 'dq%d' % (len(s.dom_map) % s.npool)
        dom = s.dom_map[dom]
        if dom not in s.dma_sems:
            s.dma_sems[dom] = s.es.enter_context(s.nc.semaphore(dom))
            s.cnt[dom] = 0
        deps, rb, wb = s._deps([in_], [out], dom)
        if s.cnt[dom] > 0:
            deps[dom] = s.cnt[dom]
        s._emit_waits(e, deps)
        s.cnt[dom] += 16
        s.eng[e].dma_start(out=out, in_=in_).then_inc(s.dma_sems[dom], 16)
        s.n_inst += 1
        s._record(rb, wb, dom, s.cnt[dom])

    def finish(s):
        for dom in s.dma_sems:
            if s.cnt[dom] > 0:
                s.eng['sp'].wait_ge(s.dma_sems[dom], s.cnt[dom])

    def act(s, out, in_, func, bias=None, scale=None, accum_out=None, eng='act'):
        kw = {}
        rd = [in_]
        wr = [out]
        if bias is not None:
            kw['bias'] = bias
            if not isinstance(bias, (int, float)):
                rd.append(bias)
        if scale is not None:
            kw['scale'] = scale
            if not isinstance(scale, (int, float)):
                rd.append(scale)
        if accum_out is not None:
            kw['accum_out'] = accum_out
            wr.append(accum_out)
        return s.op('act', lambda e: e.activation(out=out, in_=in_, func=func, **kw), rd, wr)

    def tt(s, eng, out, in0, in1, op):
        return s.op(eng, lambda e: e.tensor_tensor(out=out, in0=in0, in1=in1, op=op), [in0, in1], [out])

    def ts(s, eng, out, in0, s1, op0, s2=None, op1=None):
        rd = [in0] + [x for x in (s1, s2) if x is not None and not isinstance(x, (int, float))]
        if op1 is None:
            return s.op(eng, lambda e: e.tensor_scalar(out=out, in0=in0, scalar1=s1, scalar2=None, op0=op0), rd, [out])
        return s.op(eng, lambda e: e.tensor_scalar(out=out, in0=in0, scalar1=s1, scalar2=s2, op0=op0, op1=op1), rd, [out])

    def stt(s, out, in0, sc, in1, op0, op1):
        rd = [in0, in1] + ([] if isinstance(sc, (int, float)) else [sc])
        return s.op('dve', lambda e: e.scalar_tensor_tensor(out=out, in0=in0, scalar=sc, in1=in1, op0=op0, op1=op1), rd, [out])

    def cp(s, eng, out, in_):
        if eng == 'act':
            return s.op('act', lambda e: e.copy(out=out, in_=in_), [in_], [out])
        return s.op(eng, lambda e: e.tensor_copy(out=out, in_=in_), [in_], [out])

    def recip(s, out, in_):
        return s.op('dve', lambda e: e.reciprocal(out=out, in_=in_), [in_], [out])

    def memset(s, eng, out, val):
        return s.op(eng, lambda e: e.memset(out, val), [], [out])

    def mm(s, out, pairs):
        n = len(pairs)

        def fn(e):
            ins = None
            for i, (l, r) in enumerate(pairs):
                ins = e.matmul(out, l, r, start=(i == 0), stop=(i == n - 1))
            return ins
        rd = []
        for l, r in pairs:
            rd.append(l)
            rd.append(r)
        s.n_inst += n - 1
        return s.op('pe', fn, rd, [out])

    def mm1(s, out, l, r, start, stop):
        return s.op('pe', lambda e: e.matmul(out, l, r, start=start, stop=stop), [l, r], [out])

    def tr(s, out, in_, ident):
        return s.op('pe', lambda e: e.transpose(out, in_, ident), [in_, ident], [out])


class Prog:
    def __init__(p, dbg=()):
        p.dbg = set(dbg)
        p.nc = bass.Bass("TRN2", target_bir_lowering=False)
        p.inputs = {}
        p.outputs = {}

    def din(p, name, shape, dt=F32):
        a = p.nc.dram_tensor(name, list(shape), dt, kind="ExternalInput").ap()
        p.inputs[name] = a
        return a

    def dscr(p, name, shape, dt=F32):
        kind = "ExternalOutput" if name in p.dbg else "Internal"
        a = p.nc.dram_tensor(name, list(shape), dt, kind=kind).ap()
        if name in p.dbg:
            p.outputs[name] = a
        return a

    def dout(p, name, shape, dt=F32):
        a = p.nc.dram_tensor(name, list(shape), dt, kind="ExternalOutput").ap()
        p.outputs[name] = a
        return a


def kc_view(w, cols=None):
    v = w.rearrange("(c p) f -> p c f", p=128)
    if cols is not None:
        v = v[:, :, cols[0]:cols[1]]
    return v


def stage_mod(P, S, I, modv):
    m = S.mark()
    cs32 = S.sb([128, 16, 2], F32)
    cs = S.sb([128, 16, 2], BF16)
    S.dma('sp', cs32, I['cT'], 'ld_small')
    S.act(cs, cs32, AF.Silu)
    msb = S.sb([2, 6 * D], F32)
    mb = S.sb([2, 6 * D], F32)
    gA = S.sb([2, D], F32)
    gF = S.sb([2, D], F32)
    wb = [S.sb([128, 16, 512], BF16) for _ in range(3)]
    k = 0
    for l in range(2):
        S.dma('sp', mb, I['mod_b'][l:l + 1, :].to_broadcast([2, 6 * D]), 'ld_small')
        S.dma('sp', gA, I['norm_attn_g'][l:l + 1, :].to_broadcast([2, D]), 'ld_small')
        S.dma('sp', gF, I['norm_ffn_g'][l:l + 1, :].to_broadcast([2, D]), 'ld_small')
        for j in range(24):
            w = wb[k % 3]
            S.dma('pool', w, kc_view(I['mod_w'][l], (j * 512, (j + 1) * 512)), 'ld_w%d' % (k % 3))
            ps = S.psum(k % 2, [2, 512])
            S.mm(ps, [(cs[:, c, :], w[:, c, :]) for c in range(16)])
            S.tt('dve', msb[:, j * 512:(j + 1) * 512], ps, mb[:, j * 512:(j + 1) * 512], ALU.add)
            k += 1
        S.stt(msb[:, D:2 * D], msb[:, D:2 * D], 1.0, gA, ALU.add, ALU.mult)
        S.stt(msb[:, 4 * D:5 * D], msb[:, 4 * D:5 * D], 1.0, gF, ALU.add, ALU.mult)
        S.dma('sp', modv[l], msb, 'st_small')
    S.release(m)


def load_bc(S, dst, modv_l, row, k, dom):
    S.dma('sp', dst, modv_l[row:row + 1, k * D:(k + 1) * D].to_broadcast([128, D]), dom)


def norm_mod_transpose(S, xt, G, Sf, hT_dst, ident_bf, junk, hb, small, ps_banks):
    ssq = small[:, 0:1]
    S.act(junk, xt, AF.Square)
    S.op('dve', lambda e, ssq=ssq, junk=junk: e.tensor_reduce(out=ssq, in_=junk, axis=mybir.AxisListType.X, op=ALU.add), [junk], [ssq])
    S.ts('dve', ssq, ssq, 1.0 / D, ALU.mult, EPS, ALU.add)
    S.act(ssq, ssq, AF.Sqrt)
    S.recip(ssq, ssq)
    S.stt(junk, xt, ssq, G, ALU.mult, ALU.mult)
    S.tt('pool', hb, junk, Sf, ALU.add)
    for half in range(2):
        ps = S.psum(ps_banks[half], [128, 8, 128], BF16)
        for c in range(8):
            S.tr(ps[:, c, :], hb[:, (half * 8 + c) * 128:(half * 8 + c + 1) * 128], ident_bf)
        if half == 0:
            S.cp('act', hT_dst[:, 0:8, :], ps)
        else:
            S.cp('dve', hT_dst[:, 8:16, :], ps)


def stage_A(P, S, I, xres, modv_l, kS, kG, hT, ident_bf, tiles, ps_banks=(6, 7)):
    m = S.mark()
    Gl = S.sb([128, D], F32)
    Sl = S.sb([128, D], F32)
    Gc = S.sb([128, D], F32)
    Sc = S.sb([128, D], F32)
    load_bc(S, Gl, modv_l, 0, kG, 'ld_bc')
    load_bc(S, Sl, modv_l, 0, kS, 'ld_bc')
    load_bc(S, Gc, modv_l, 1, kG, 'ld_bc')
    load_bc(S, Sc, modv_l, 1, kS, 'ld_bc')
    xts = [S.sb([128, D], F32) for _ in range(2)]
    junk = S.sb([128, D], F32)
    hb = S.sb([128, D], BF16)
    small = S.sb([128, 4], F32)
    for n, (ti, dc) in enumerate(tiles):
        xt = xts[n % 2]
        S.dma('sp', xt, xres[ti * 128:(ti + 1) * 128, :], 'ld_x%d' % (n % 2))
        isctx = ti < CTX // 128
        norm_mod_transpose(S, xt, Gc if isctx else Gl, Sc if isctx else Sl, hT[:, :, dc:dc + 128], ident_bf,
                           junk, hb, small, ps_banks)
    S.release(m)


def stage_inproj(P, S, w_in, F, hT, segs, PF, PV, wdom='ld_w'):
    m = S.mark()
    wb = [S.sb([128, 16, 512], BF16) for _ in range(3)]
    stg = [S.sb([128, 512], F32) for _ in range(3)]
    stgb = [S.sb([128, 512], BF16) for _ in range(2)]
    nblk = (F + 511) // 512
    k = 0
    kk = 0
    ttiles = [(t0, min(512, T - t0)) for t0 in range(0, T, 512)]
    for bi in range(nblk):
        c0 = bi * 512
        c1 = min(F, c0 + 512)
        w = wb[bi % 3]
        S.dma('pool', w[:, :, 0:c1 - c0], kc_view(w_in, (c0, c1)), '%s%d' % (wdom, bi % 3))
        for (lo, hi, kind, func, base) in segs:
            a = max(lo, c0)
            b = min(hi, c1)
            if a >= b:
                continue
            if kind == 'fm':
                for cc in range(a, b, 128):
                    ncol = min(128, b - cc)
                    for (t0, tn) in ttiles:
                        ps = S.psum(k % 4, [ncol, tn])
                        S.mm(ps, [(w[:, c, cc - c0:cc - c0 + ncol], hT[:, c, t0:t0 + tn]) for c in range(16)])
                        st = stg[k % 3]
                        if k % 2 == 0 or func != AF.Copy:
                            S.act(st[0:ncol, 0:tn], ps, func)
                        else:
                            S.cp('dve', st[0:ncol, 0:tn], ps)
                        S.dma('sp', PF[base + cc - lo: base + cc - lo + ncol, t0:t0 + tn], st[0:ncol, 0:tn], 'st_pf%d' % (k % 3))
                        k += 1
            else:
                wd = b - a
                for ti in range(NT):
                    ps = S.psum(4 + kk % 2, [128, wd])
                    S.mm(ps, [(hT[:, c, ti * 128:(ti + 1) * 128], w[:, c, a - c0:b - c0]) for c in range(16)])
                    st = stgb[kk % 2][:, 0:wd]
                    if kk % 2 == 0:
                        S.cp('act', st, ps)
                    else:
                        S.cp('dve', st, ps)
                    S.dma('sp', PV[ti * 128:(ti + 1) * 128, base + a - lo: base + b - lo], st, 'st_pv%d' % (kk % 2))
                    kk += 1
    S.release(m)


LT_TILES = [(0, 256)] + [(256 + 512 * i, 512) for i in range(4)]


def bc_mid(ap, n):
    return ap.to_broadcast([ap.shape[0], ap.shape[1], n])


def ssq_rstd(S, chunks, n, dim, ones_bf, sqb, rstd_out, bank):
    ps = S.psum(bank, [128, n])
    for i, ch in enumerate(chunks):
        R = ch.shape[0]
        sq = sqb[i % len(sqb)][0:R, 0:n]
        S.act(sq, ch, AF.Square)
        S.mm1(ps, ones_bf[0:R, :], sq, i == 0, i == len(chunks) - 1)
    S.ts('dve', rstd_out, ps, 1.0 / dim, ALU.mult, EPS, ALU.add)
    S.act(rstd_out, rstd_out, AF.Sqrt)
    S.recip(rstd_out, rstd_out)


def rope_fm(S, x32, R, n, cos, sin, perm_bf, out_bf, xb, tb, bank):
    S.cp('pool', xb[0:R, 0:n], x32)
    ps = S.psum(bank, [R, n])
    S.mm(ps, [(perm_bf[0:R, 0:R], xb[0:R, 0:n])])
    S.tt('pool', tb[0:R, 0:n], x32, cos, ALU.mult)
    S.tt('dve', x32, ps, sin, ALU.mult)
    S.tt('dve', out_bf, x32, tb[0:R, 0:n], ALU.add)


def attn_core(S, qparts, kparts, v, q0, nq, stiles, scale, dst, ones_bf, PTs, rden, par, cnt):
    oT = S.psum(2 + 2 * par, [128, nq])
    den = S.psum(3 + 2 * par, [128, nq])
    ns = len(stiles)
    for i, si in enumerate(stiles):
        sc = S.psum(cnt[0] % 2, [128, nq])
        S.mm(sc, [(kp[:, si * 128:(si + 1) * 128], qp[:, q0:q0 + nq]) for kp, qp in zip(kparts, qparts)])
        pt = PTs[cnt[0] % 2][:, 0:nq]
        cnt[0] += 1
        S.act(pt, sc, AF.Exp, scale=scale)
        S.mm1(oT, v[:, si, :], pt, i == 0, i == ns - 1)
        S.mm1(den, ones_bf, pt, i == 0, i == ns - 1)
    S.recip(rden[:, 0:nq], den)
    S.tt('dve', dst, oT, rden[:, 0:nq], ALU.mult)


def stage_mla(P, S, I, PF, mixT, C, need_ctx=True):
    import os
    ROT = 0 if os.environ.get('MLA_NOROPE') == '1' else CTX
    ROT = ROT if ROT else 10 ** 9
    m = S.mark()
    ones_bf = C['ones_bf']
    wuq = S.sb([128, 4, 1536], BF16)
    wukv = S.sb([128, 2, 2048], BF16)
    S.dma('pool', wuq, kc_view(I['mla_w_uq']), 'ld_w0')
    S.dma('pool', wukv, kc_view(I['mla_w_ukv']), 'ld_w1')
    gq = S.sb([128, 4], F32)
    gkv = S.sb([128, 2], F32)
    S.dma('sp', gq, I['mla_q_gT'], 'ld_small')
    S.dma('sp', gkv, I['mla_kv_gT'], 'ld_small')
    cosB = S.sb([64, LAT], F32)
    sinB = S.sb([64, LAT], F32)
    S.dma('sp', cosB, I['cosB'], 'ld_small')
    S.dma('sp', sinB, I['sinB'], 'ld_small')
    nqT = S.sb([128, 4, T], BF16)
    nkvT = S.sb([128, 2, T], BF16)
    krT = S.sb([64, T], BF16)
    sqb = [S.sb([128, 512], BF16) for _ in range(2)]
    rstd = S.sb([128, 512], F32)
    xb = S.sb([128, 512], BF16)
    tb = S.sb([128, 512], F32)
    x32 = S.sb([64, 512], F32)
    m1 = S.mark()
    p5 = S.sb([128, 4, 512], F32)
    p6 = S.sb([128, 2, 512], F32)
    p7 = S.sb([64, 512], F32)
    for (t0, n) in LT_TILES:
        S.dma('sp', p5[:, :, 0:n], PF[5120:5632, t0:t0 + n].rearrange('(c p) t -> p c t', p=128), 'ld_p5')
        S.dma('sp', p6[:, :, 0:n], PF[5632:5888, t0:t0 + n].rearrange('(c p) t -> p c t', p=128), 'ld_p6')
        S.dma('sp', p7[:, 0:n], PF[5888:5952, t0:t0 + n], 'ld_p7')
        ssq_rstd(S, [p5[:, c, 0:n] for c in range(4)], n, 512, ones_bf, sqb, rstd[:, 0:n], 6)
        for c in range(4):
            S.stt(nqT[:, c, t0:t0 + n], p5[:, c, 0:n], gq[:, c:c + 1], rstd[:, 0:n], ALU.mult, ALU.mult)
        ssq_rstd(S, [p6[:, c, 0:n] for c in range(2)], n, 256, ones_bf, sqb, rstd[:, 0:n], 7)
        for c in range(2):
            S.stt(nkvT[:, c, t0:t0 + n], p6[:, c, 0:n], gkv[:, c:c + 1], rstd[:, 0:n], ALU.mult, ALU.mult)
        if t0 >= ROT:
            l0 = t0 - CTX
            rope_fm(S, p7[:, 0:n], 64, n, cosB[:, l0:l0 + n], sinB[:, l0:l0 + n], C['perm64_bf'], krT[:, t0:t0 + n], xb, tb, 6)
        else:
            S.cp('pool', krT[:, t0:t0 + n], p7[:, 0:n])
    S.release(m1)
    hb = []
    for _ in range(1):
        hb.append((S.sb([128, T], BF16), S.sb([64, T], BF16), S.sb([128, T], BF16), S.sb([128, NT, 128], BF16)))
    PTs = [S.sb([128, 512], BF16) for _ in range(2)]
    rden = S.sb([128, 512], F32)
    cnt = [0]
    scale = float((128 + 64) ** -0.5)
    par = 0
    for h in range(8):
        qn, qr, kn, vh = hb[0]
        for (t0, n) in LT_TILES:
            ps = S.psum(6, [128, n])
            S.mm(ps, [(wuq[:, c, h * 192:h * 192 + 128], nqT[:, c, t0:t0 + n]) for c in range(4)])
            S.cp('act', qn[:, t0:t0 + n], ps)
            ps2 = S.psum(7, [64, n])
            S.mm(ps2, [(wuq[:, c, h * 192 + 128:h * 192 + 192], nqT[:, c, t0:t0 + n]) for c in range(4)])
            if t0 >= ROT:
                l0 = t0 - CTX
                S.cp('act', x32[:, 0:n], ps2)
                rope_fm(S, x32[:, 0:n], 64, n, cosB[:, l0:l0 + n], sinB[:, l0:l0 + n], C['perm64_bf'], qr[:, t0:t0 + n], xb, tb, 7)
            else:
                S.cp('act', qr[:, t0:t0 + n], ps2)
            ps = S.psum(6, [128, n])
            S.mm(ps, [(wukv[:, c, h * 256:h * 256 + 128], nkvT[:, c, t0:t0 + n]) for c in range(2)])
            S.cp('dve', kn[:, t0:t0 + n], ps)
            nb = n // 128
            psv = S.psum(7, [128, nb, 128])
            for j in range(nb):
                tk = t0 + j * 128
                S.mm(psv[:, j, :], [(nkvT[:, c, tk:tk + 128], wukv[:, c, h * 256 + 128:(h + 1) * 256]) for c in range(2)])
            S.cp('dve', vh[:, t0 // 128:t0 // 128 + nb, :], psv)
        if need_ctx:
            attn_core(S, [qn, qr], [kn, krT], vh, 0, 256, [0, 1], scale, mixT[:, 8 + h, 0:256], ones_bf, PTs, rden, par, cnt)
            par ^= 1
        for qi in range(4):
            q0 = CTX + 512 * qi
            attn_core(S, [qn, qr], [kn, krT], vh, q0, 512, list(range(NT)), scale, mixT[:, 8 + h, q0:q0 + 512], ones_bf, PTs, rden, par, cnt)
            par ^= 1
    S.release(m)


def stage_gqa(P, S, I, PF, PV, mixT, C):
    m = S.mark()
    ones_bf = C['ones_bf']
    gq = S.sb([128, 1], F32)
    gk = S.sb([128, 1], F32)
    S.dma('sp', gq, I['gqa_q_gT'], 'ld_small')
    S.dma('sp', gk, I['gqa_k_gT'], 'ld_small')
    cosC = S.sb([128, LAT], F32)
    sinC = S.sb([128, LAT], F32)
    S.dma('sp', cosC, I['cosC'], 'ld_small')
    S.dma('sp', sinC, I['sinC'], 'ld_small')
    kT = S.sb([128, T], BF16)
    qT = S.sb([128, T], BF16)
    vh = S.sb([128, NT, 128], BF16)
    sqb = [S.sb([128, 512], BF16) for _ in range(2)]
    rstd = S.sb([128, 512], F32)
    xb = S.sb([128, 512], BF16)
    tb = S.sb([128, 512], F32)
    x32 = S.sb([128, 512], F32)
    p = S.sb([128, 512], F32)
    PTs = [S.sb([128, 512], BF16) for _ in range(2)]
    rden = S.sb([128, 512], F32)
    cnt = [0]
    par = 0
    scale = float(128 ** -0.5)

    def prep(row0, gcol, dst, tiles):
        for (t0, n) in tiles:
            S.dma('sp', p[:, 0:n], PF[row0:row0 + 128, t0:t0 + n], 'ld_p5')
            ssq_rstd(S, [p[:, 0:n]], n, 128, ones_bf, sqb, rstd[:, 0:n], 6)
            if t0 >= CTX:
                l0 = t0 - CTX
                S.stt(x32[:, 0:n], p[:, 0:n], gcol, rstd[:, 0:n], ALU.mult, ALU.mult)
                rope_fm(S, x32[:, 0:n], 128, n, cosC[:, l0:l0 + n], sinC[:, l0:l0 + n], C['perm128_bf'], dst[:, t0:t0 + n], xb, tb, 7)
            else:
                S.stt(dst[:, t0:t0 + n], p[:, 0:n], gcol, rstd[:, 0:n], ALU.mult, ALU.mult)

    for g in range(2):
        prep(1024 + g * 128, gk[:, 0:1], kT, LT_TILES)
        S.dma('sp', vh, PV[:, g * 128:(g + 1) * 128].rearrange('(j p) v -> p j v', p=128), 'ld_v0')
        for i in range(4):
            h = g * 4 + i
            prep(h * 128, gq[:, 0:1], qT, LT_TILES[1:])
            for qi in range(4):
                q0 = CTX + 512 * qi
                attn_core(S, [qT], [kT], vh, q0, 512, list(range(NT)), scale, mixT[:, h, q0:q0 + 512], ones_bf, PTs, rden, par, cnt)
                par ^= 1
    S.release(m)


def stage_scan(P, S, I, cfg, PF, PV, OF, mixT, C):
    H = cfg['H']
    DVH = cfg['DVH']
    dv = 128 * DVH
    X = H * DVH
    kind = cfg['kind']
    vbase = cfg.get('vbase', 0)
    m = S.mark()
    ident_bf = C['ident_bf']
    ones_bf = C['ones_bf']
    NB = 1
    qt = [S.sb([128, H, 512], BF16) for _ in range(NB)]
    kt = [S.sb([128, H, 512], BF16) for _ in range(NB)]
    kh = [S.sb([64, H, 8, 128], BF16) for _ in range(NB)]
    vv = [S.sb([64, H, 8, dv], BF16) for _ in range(NB)]
    dec = [S.sb([128, H, 8], F32) for _ in range(NB)]
    tq = [S.sb([128, 512], F32) for _ in range(2)]
    tk = [S.sb([128, 512], F32) for _ in range(2)]
    tg = [S.sb([128, 512], F32) for _ in range(2)]
    tc_ = [S.sb([128, 512], F32) for _ in range(2)]
    ta = [S.sb([128, 512], F32) for _ in range(2)]
    te = [S.sb([128, 512], F32) for _ in range(2)]
    kht = [S.sb([128, 512], BF16) for _ in range(2)]
    Sst = S.sb([128, H, dv], F32)
    Sbf = S.sb([128, H, dv], BF16)
    otile = S.sb([128, X, 512], F32)
    scmb = [S.sb([64, H, 64], BF16) for _ in range(2)]
    gcol = S.sb([128, DVH], F32)
    S.dma('sp', gcol, cfg['gcol'], 'ld_small')
    if kind == 'hgrn':
        lbt = S.sb([128, 2, 3, 8], F32)
        S.dma('sp', lbt, I['lbT'], 'ld_small')
        S.act(lbt, lbt, AF.Exp)
        lsum = S.sb([128, 2, 8], F32)
        S.tt('dve', lsum, lbt[:, :, 0, :], lbt[:, :, 1, :], ALU.add)
        S.tt('dve', lsum, lsum, lbt[:, :, 2, :], ALU.add)
        S.recip(lsum, lsum)
        lb = S.sb([128, 2, 8], F32)
        oml = S.sb([128, 2, 8], F32)
        S.tt('dve', lb, lbt[:, :, 0, :], lsum, ALU.mult)
        S.ts('dve', oml, lb, -1.0, ALU.mult, 1.0, ALU.add)
    else:
        wa2 = S.sb([16, 2, 512], BF16)
        S.dma('pool', wa2, I['gla_w_a2'].rearrange('d r f -> r d f'), 'ld_w0')
        negb = S.sb([128, 2, 4], F32)
        S.dma('sp', negb, I['gla_bT'], 'ld_small')
        S.ts('dve', negb, negb, -1.0, ALU.mult)
        a32 = S.sb([16, 512], F32)
        abf = S.sb([16, 512], BF16)
    import os
    _dirs = [int(x) for x in os.environ.get('SCAN_DIRS', '0,1').split(',')]
    _nt = int(os.environ.get('SCAN_TILES', '5'))
    _noch = os.environ.get('SCAN_NOCHUNK') == '1'
    _nopost = os.environ.get('SCAN_NOPOST') == '1'
    k = 0
    for d in _dirs:
        order = LT_TILES if d == 0 else [LT_TILES[0]] + LT_TILES[:0:-1]
        mask = C['maskf'] if d == 0 else C['maskb']
        mask_b = mask.unsqueeze(1).to_broadcast([64, H, 64])
        S.memset('dve', Sst, 0.0)
        S.memset('pool', Sbf, 0.0)
        for ti_, (t0, n) in enumerate(order[:_nt]):
            nch = n // 64
            b = ti_ % NB
            if kind == 'gla':
                S.dma('sp', a32[:, 0:n], PF[cfg['abase'] + 16 * d: cfg['abase'] + 16 * d + 16, t0:t0 + n], 'ld_a')
                S.cp('act', abf[:, 0:n], a32[:, 0:n])
            for h in range(H):
                r = k % 2
                k += 1
                q = tq[r][:, 0:n]
                kk = tk[r][:, 0:n]
                g = tg[r][:, 0:n]
                c = tc_[r][:, 0:n]
                a = ta[r][:, 0:n]
                e_ = te[r][:, 0:n]
                S.dma('sp', q, PF[cfg['qbase'] + h * 128: cfg['qbase'] + (h + 1) * 128, t0:t0 + n], 'ld_q%d' % r)
                if kind == 'hgrn':
                    lo = 1024 * (1 + d) + h * 128
                    S.dma('sp', kk, PF[lo:lo + 128, t0:t0 + n], 'ld_k%d' % r)
                    S.act(kk, kk, AF.Sigmoid)
                    S.ts('dve', kk, kk, oml[:, d, h:h + 1], ALU.mult, lb[:, d, h:h + 1], ALU.add)
                    S.act(g, kk, AF.Ln)
                    S.ts('pool', kk, kk, -1.0, ALU.mult, 1.0, ALU.add)
                else:
                    S.dma('sp', kk, PF[cfg['kbase'] + h * 128: cfg['kbase'] + (h + 1) * 128, t0:t0 + n], 'ld_k%d' % r)
                    zp = S.psum(7, [128, n])
                    S.mm(zp, [(wa2[:, d, h * 128:(h + 1) * 128], abf[:, 0:n])])
                    S.act(e_, zp, AF.Exp, scale=-1.0, bias=negb[:, d, h:h + 1])
                    S.act(g, e_, AF.Ln, bias=1.0)
                    S.ts('dve', g, g, -1.0 / 16.0, ALU.mult)
                S.op('dve', lambda e, c=c, g=g, n=n: e.tensor_tensor_scan(out=c, data0=C['rmask'][:, 0:n], data1=g, initial=0.0,
                                                                            op0=ALU.mult, op1=ALU.add), [C['rmask'][:, 0:n], g], [c])
                c3 = c.rearrange('p (j s) -> p j s', s=64)
                tot = c3[:, :, 63:64]
                if d == 1:
                    S.tt('dve', a, g, c, ALU.subtract)
                    S.tt('dve', a.rearrange('p (j s) -> p j s', s=64), a.rearrange('p (j s) -> p j s', s=64), bc_mid(tot, 64), ALU.add)
                else:
                    a = c
                S.act(e_, a, AF.Exp)
                S.stt(qt[b][:, h, 0:n], q, float(cfg['qscale']), e_, ALU.mult, ALU.mult)
                S.act(e_, a, AF.Exp, scale=-1.0)
                S.tt('dve', kt[b][:, h, 0:n], kk, e_, ALU.mult)
                S.tt('dve', g.rearrange('p (j s) -> p j s', s=64), bc_mid(tot, 64), a.rearrange('p (j s) -> p j s', s=64), ALU.subtract)
                S.act(g, g, AF.Exp)
                S.tt('pool', kht[r][:, 0:n], kk, g, ALU.mult)
                S.act(dec[b][:, h, 0:nch], c3[:, :, 63], AF.Exp)
                psT = S.psum(6, [64, 8, 128], BF16)
                for j in range(nch):
                    S.tr(psT[:, j, :], kht[r][:, j * 64:(j + 1) * 64], ident_bf)
                S.cp('act', kh[b][:, h, 0:nch, :], psT[:, 0:nch, :])
                S.dma('sp', vv[b][:, h, 0:nch, :], PV[t0:t0 + n, vbase + h * dv:vbase + (h + 1) * dv].rearrange('(j p) v -> p j v', p=64), 'ld_v%d' % b)
            chs = list(range(nch)) if d == 0 else list(range(nch - 1, -1, -1))
            if _noch:
                chs = []
            for ji, j in enumerate(chs):
                sc = S.psum(ji % 2, [64, H, 64])
                for h in range(H):
                    S.mm(sc[:, h, :], [(kt[b][:, h, j * 64:(j + 1) * 64], qt[b][:, h, j * 64:(j + 1) * 64])])
                scm = scmb[ji % 2]
                S.tt('dve', scm, sc, mask_b, ALU.mult)
                ops = S.psum(2 + ji % 2, [128, X, 64])
                kv = S.psum(4, [128, H, dv])
                for h in range(H):
                    for hf in range(DVH):
                        S.mm(ops[:, h * DVH + hf, :], [(vv[b][:, h, j, hf * 128:(hf + 1) * 128], scm[:, h, :]),
                                                       (Sbf[:, h, hf * 128:(hf + 1) * 128], qt[b][:, h, j * 64:(j + 1) * 64])])
                    S.mm(kv[:, h, :], [(kh[b][:, h, j, :], vv[b][:, h, j, :])])
                    S.stt(Sst[:, h, :], Sst[:, h, :], dec[b][:, h, j:j + 1], kv[:, h, :], ALU.mult, ALU.add)
                    S.cp('act', Sbf[:, h, :], Sst[:, h, :])
                S.cp('act', otile[:, :, j * 64:(j + 1) * 64], ops)
            OFv = OF.rearrange('(x p) t -> p x t', p=128)[:, :, t0:t0 + n]
            if _nopost:
                continue
            if d == 0:
                S.dma('sp', OFv, otile[:, :, 0:n], 'st_of')
            else:
                m2 = S.mark()
                sqb = [kht[0], kht[1]]
                for h in range(H):
                    r = k % 2
                    k += 1
                    for hf in range(DVH):
                        x = h * DVH + hf
                        oft = tc_[(r + hf) % 2][:, 0:n]
                        S.dma('sp', oft, OF[x * 128:(x + 1) * 128, t0:t0 + n], 'ld_of%d' % ((r + hf) % 2))
                        S.tt('pool', otile[:, x, 0:n], otile[:, x, 0:n], oft, ALU.add)
                    rstd = tq[r][:, 0:n]
                    ssq_rstd(S, [otile[:, h * DVH + hf, 0:n] for hf in range(DVH)], n, dv, ones_bf, sqb, rstd, 7)
                    for hf in range(DVH):
                        x = h * DVH + hf
                        gt = tk[(r + hf) % 2][:, 0:n]
                        gl = cfg['gbase'] + x * 128
                        S.dma('sp', gt, PF[gl:gl + 128, t0:t0 + n], 'ld_g%d' % ((r + hf) % 2))
                        S.stt(otile[:, x, 0:n], otile[:, x, 0:n], gcol[:, hf:hf + 1], rstd, ALU.mult, ALU.mult)
                        S.tt('dve', mixT[:, cfg['mixbase'] + x, t0:t0 + n], otile[:, x, 0:n], gt, ALU.mult)
                S.release(m2)
    S.release(m)


def stage_outproj(P, S, I, w_out, mixT, modv_l, xsrc, XR, tiles):
    m = S.mark()
    wb = [S.sb([128, 16, 512], BF16) for _ in range(2)]
    gl = [S.sb([128, 512], F32) for _ in range(2)]
    gc = [S.sb([128, 512], F32) for _ in range(2)]
    xt = [S.sb([128, 512], F32) for _ in range(3)]
    yt = [S.sb([128, 512], F32) for _ in range(3)]
    k = 0
    for cb in range(4):
        w = wb[cb % 2]
        S.dma('pool', w, kc_view(w_out, (cb * 512, (cb + 1) * 512)), 'ld_w%d' % (cb % 2))
        S.dma('sp', gl[cb % 2], modv_l[0:1, 2 * D + cb * 512: 2 * D + (cb + 1) * 512].to_broadcast([128, 512]), 'ld_bc')
        S.dma('sp', gc[cb % 2], modv_l[1:2, 2 * D + cb * 512: 2 * D + (cb + 1) * 512].to_broadcast([128, 512]), 'ld_bc')
        for ti in tiles:
            ps = S.psum(k % 4, [128, 512])
            S.mm(ps, [(mixT[:, c, ti * 128:(ti + 1) * 128], w[:, c, :]) for c in range(16)])
            x = xt[k % 3]
            y = yt[k % 3]
            S.dma('sp', x, xsrc[ti * 128:(ti + 1) * 128, cb * 512:(cb + 1) * 512], 'ld_xo%d' % (k % 3))
            gate = gc[cb % 2] if ti < CTX // 128 else gl[cb % 2]
            S.tt('dve', y, ps, gate, ALU.mult)
            S.tt('pool', y, y, x, ALU.add)
            S.dma('sp', XR[ti * 128:(ti + 1) * 128, cb * 512:(cb + 1) * 512], y, 'st_xo%d' % (k % 3))
            k += 1
    S.release(m)


def stage_moe(P, S, I, l, modv_l, XR, C, supers, final_out=None):
    ident = C['ident']
    ident_bf = C['ident_bf']
    w1 = I['moe_w1'][l]
    w3 = I['moe_w3'][l]
    w2 = I['moe_w2'][l]
    for tiles in supers:
        nsub = len(tiles)
        TS = nsub * 128
        m = S.mark()
        h2T = S.sb([128, 16, TS], BF16)
        yacc = S.sb([128, nsub, D], F32)
        comb = S.sb([128, nsub, 16], F32)
        m1 = S.mark()
        Gl = S.sb([128, D], F32)
        Sl = S.sb([128, D], F32)
        Gc = S.sb([128, D], F32)
        Sc = S.sb([128, D], F32)
        load_bc(S, Gl, modv_l, 0, 4, 'ld_bc')
        load_bc(S, Sl, modv_l, 0, 3, 'ld_bc')
        if any(t < CTX // 128 for t in tiles):
            load_bc(S, Gc, modv_l, 1, 4, 'ld_bc')
            load_bc(S, Sc, modv_l, 1, 3, 'ld_bc')
        xts = [S.sb([128, D], F32) for _ in range(2)]
        junk = S.sb([128, D], F32)
        hf = S.sb([128, D], F32)
        hb = S.sb([128, D], BF16)
        h32T = S.sb([128, 16, 128], F32)
        rw = S.sb([128, 16, 16], F32)
        S.dma('sp', rw, kc_view(I['router_w']), 'ld_small')
        rb = S.sb([128, 16], F32)
        S.dma('sp', rb, I['router_b'].to_broadcast([128, 16]), 'ld_small')
        sm = S.sb([128, 4], F32)
        sc_ = S.sb([128, 16], F32)
        sel = S.sb([128, 16], F32)
        t4 = [S.sb([128, 4], F32) for _ in range(8)]
        em = S.sb([128, 16], F32)
        for n, ti in enumerate(tiles):
            xt = xts[n % 2]
            S.dma('sp', xt, XR[ti * 128:(ti + 1) * 128, :], 'ld_x%d' % (n % 2))
            isctx = ti < CTX // 128
            G = Gc if isctx else Gl
            Sf = Sc if isctx else Sl
            ssq = sm[:, 0:1]
            S.act(junk, xt, AF.Square)
            S.op('dve', lambda e, ssq=ssq, junk=junk: e.tensor_reduce(out=ssq, in_=junk, axis=mybir.AxisListType.X, op=ALU.add), [junk], [ssq])
            S.ts('dve', ssq, ssq, 1.0 / D, ALU.mult, EPS, ALU.add)
            S.act(ssq, ssq, AF.Sqrt)
            S.recip(ssq, ssq)
            S.stt(junk, xt, ssq, G, ALU.mult, ALU.mult)
            S.tt('pool', hf, junk, Sf, ALU.add)
            S.cp('act', hb, hf)
            for half in range(2):
                ps = S.psum(6 + half, [128, 8, 128], BF16)
                for c in range(8):
                    S.tr(ps[:, c, :], hb[:, (half * 8 + c) * 128:(half * 8 + c + 1) * 128], ident_bf)
                S.cp('act' if half == 0 else 'dve', h2T[:, half * 8:half * 8 + 8, n * 128:(n + 1) * 128], ps)
            for q4 in range(4):
                ps = S.psum(q4 % 2, [128, 4, 128])
                for c in range(4):
                    cc = q4 * 4 + c
                    S.tr(ps[:, c, :], hf[:, cc * 128:(cc + 1) * 128], ident)
                S.cp('act' if q4 % 2 == 0 else 'dve', h32T[:, q4 * 4:q4 * 4 + 4, :], ps)
            lg = S.psum(2, [128, 16])
            S.mm(lg, [(h32T[:, c, :], rw[:, c, :]) for c in range(16)])
            S.act(sc_, lg, AF.Sigmoid)
            S.tt('dve', sel, sc_, rb, ALU.add)
            s4 = sel.rearrange('p (g e) -> p g e', e=4)
            a_, b_, c_, d_ = s4[:, :, 0], s4[:, :, 1], s4[:, :, 2], s4[:, :, 3]
            m1_, n1_, m2_, n2_, top1, xx, yy, sec = t4
            S.tt('dve', m1_, a_, b_, ALU.max)
            S.tt('dve', n1_, a_, b_, ALU.min)
            S.tt('dve', m2_, c_, d_, ALU.max)
            S.tt('dve', n2_, c_, d_, ALU.min)
            S.tt('dve', top1, m1_, m2_, ALU.max)
            S.tt('dve', xx, m1_, m2_, ALU.min)
            S.tt('dve', yy, n1_, n2_, ALU.max)
            S.tt('dve', sec, xx, yy, ALU.max)
            S.tt('dve', top1, top1, sec, ALU.add)
            gmax = sm[:, 1:2]
            S.op('dve', lambda e, gmax=gmax, top1=top1: e.tensor_reduce(out=gmax, in_=top1, axis=mybir.AxisListType.X, op=ALU.max), [top1], [gmax])
            S.ts('dve', xx, top1, gmax, ALU.is_ge)
            e4 = em.rearrange('p (g e) -> p g e', e=4)
            S.tt('dve', e4, s4, bc_mid(sec.unsqueeze(2), 4), ALU.is_ge)
            S.tt('dve', e4, e4, bc_mid(xx.unsqueeze(2), 4), ALU.mult)
            S.tt('dve', em, em, sc_, ALU.mult)
            den = sm[:, 2:3]
            S.op('dve', lambda e, den=den: e.tensor_reduce(out=den, in_=em, axis=mybir.AxisListType.X, op=ALU.add), [em], [den])
            S.recip(den, den)
            S.ts('dve', comb[:, n, :], em, den, ALU.mult)
        S.release(m1)
        m1 = S.mark()
        NBUF = 2
        w1b = [S.sb([128, 16, 256], BF16) for _ in range(NBUF)]
        w3b = [S.sb([128, 16, 256], BF16) for _ in range(NBUF)]
        w2b = [S.sb([128, 2, D], BF16) for _ in range(NBUF)]
        s1 = [S.sb([128, 512], F32) for _ in range(2)]
        hid = [S.sb([128, 2, 512], BF16) for _ in range(2)]
        ntiles = [(t0, min(512, TS - t0)) for t0 in range(0, TS, 512)]
        k = 0
        kq = 0
        for e in range(NE):
            for hfx in range(2):
                bsel = k % NBUF
                f0 = hfx * 256
                S.dma('pool', w1b[bsel], kc_view(w1[e], (f0, f0 + 256)), 'ld_w1_%d' % bsel)
                S.dma('pool', w3b[bsel], kc_view(w3[e], (f0, f0 + 256)), 'ld_w3_%d' % bsel)
                S.dma('pool', w2b[bsel], w2[e][f0:f0 + 256, :].rearrange('(c p) d -> p c d', p=128), 'ld_w2_%d' % bsel)
                first = (k == 0)
                k += 1
                for (t0, tn) in ntiles:
                    hd = hid[kq % 2]
                    for fc in range(2):
                        p1 = S.psum(0 + fc, [128, tn])
                        p3 = S.psum(2 + fc, [128, tn])
                        S.mm(p1, [(w1b[bsel][:, c, fc * 128:(fc + 1) * 128], h2T[:, c, t0:t0 + tn]) for c in range(16)])
                        S.mm(p3, [(w3b[bsel][:, c, fc * 128:(fc + 1) * 128], h2T[:, c, t0:t0 + tn]) for c in range(16)])
                        st = s1[fc][:, 0:tn]
                        S.act(st, p1, AF.Silu)
                        S.tt('dve', hd[:, fc, 0:tn], st, p3, ALU.mult)
                    kq += 1
                    for ts_ in range(tn // 128):
                        sub = t0 // 128 + ts_
                        for dc in range(4):
                            yp = S.psum(4 + (ts_ * 4 + dc) % 4, [128, 512])
                            S.mm(yp, [(hd[:, fc, ts_ * 128:(ts_ + 1) * 128], w2b[bsel][:, fc, dc * 512:(dc + 1) * 512]) for fc in range(2)])
                            ya = yacc[:, sub, dc * 512:(dc + 1) * 512]
                            if first:
                                S.ts('dve', ya, yp, comb[:, sub, e:e + 1], ALU.mult)
                            else:
                                S.stt(ya, yp, comb[:, sub, e:e + 1], ya, ALU.mult, ALU.add)
        S.release(m1)
        m1 = S.mark()
        g2l = S.sb([128, D], F32)
        g2c = S.sb([128, D], F32)
        load_bc(S, g2l, modv_l, 0, 5, 'ld_bc')
        if any(t < CTX // 128 for t in tiles):
            load_bc(S, g2c, modv_l, 1, 5, 'ld_bc')
        xts = [S.sb([128, D], F32) for _ in range(2)]
        if final_out is not None:
            fg = S.sb([128, D], F32)
            S.dma('sp', fg, I['final_norm_g'].unsqueeze(0).to_broadcast([128, D]), 'ld_bc')
            junk = S.sb([128, D], F32)
            sm = S.sb([128, 4], F32)
        for n, ti in enumerate(tiles):
            xt = xts[n % 2]
            S.dma('sp', xt, XR[ti * 128:(ti + 1) * 128, :], 'ld_x%d' % (n % 2))
            g2 = g2c if ti < CTX // 128 else g2l
            S.tt('dve', yacc[:, n, :], yacc[:, n, :], g2, ALU.mult)
            S.tt('pool', xt, xt, yacc[:, n, :], ALU.add)
            if final_out is None:
                S.dma('sp', XR[ti * 128:(ti + 1) * 128, :], xt, 'st_x%d' % (n % 2))
            else:
                ssq = sm[:, 0:1]
                S.act(junk, xt, AF.Square)
                S.op('dve', lambda e, ssq=ssq, junk=junk: e.tensor_reduce(out=ssq, in_=junk, axis=mybir.AxisListType.X, op=ALU.add), [junk], [ssq])
                S.ts('dve', ssq, ssq, 1.0 / D, ALU.mult, EPS, ALU.add)
                S.act(ssq, ssq, AF.Sqrt)
                S.recip(ssq, ssq)
                S.stt(xt, xt, ssq, fg, ALU.mult, ALU.mult)
                r0 = ti * 128 - CTX
                S.dma('sp', final_out[r0:r0 + 128, :], xt, 'st_x%d' % (n % 2))
        S.release(m1)
        S.release(m)


def build(dbg=(), stop_after=None, sbuf_kb=192):
    P = Prog(dbg)
    nc = P.nc
    I = {}
    I['xin'] = P.din('xin', [T, D])
    I['cT'] = P.din('cT', [128, 16, 2])
    I['mod_w'] = P.din('mod_w', [2, D, 6 * D])
    I['mod_b'] = P.din('mod_b', [2, 6 * D])
    I['norm_attn_g'] = P.din('norm_attn_g', [2, D])
    I['norm_ffn_g'] = P.din('norm_ffn_g', [2, D])
    I['final_norm_g'] = P.din('final_norm_g', [D])
    I['ab_w_in'] = P.din('ab_w_in', [D, 5952])
    I['ab_w_out'] = P.din('ab_w_out', [D, D])
    I['mla_w_uq'] = P.din('mla_w_uq', [512, 1536])
    I['mla_w_ukv'] = P.din('mla_w_ukv', [256, 2048])
    I['mla_q_gT'] = P.din('mla_q_gT', [128, 4])
    I['mla_kv_gT'] = P.din('mla_kv_gT', [128, 2])
    I['hgrn_gT'] = P.din('hgrn_gT', [128, 1])
    I['lbT'] = P.din('lbT', [128, 2, 3, 8])
    I['cosB'] = P.din('cosB', [64, LAT])
    I['sinB'] = P.din('sinB', [64, LAT])
    I['cd_w_in'] = P.din('cd_w_in', [D, 4640])
    I['cd_w_out'] = P.din('cd_w_out', [D, D])
    I['gqa_q_gT'] = P.din('gqa_q_gT', [128, 1])
    I['gqa_k_gT'] = P.din('gqa_k_gT', [128, 1])
    I['gla_w_a2'] = P.din('gla_w_a2', [2, 16, 512])
    I['gla_bT'] = P.din('gla_bT', [128, 2, 4])
    I['gla_gT'] = P.din('gla_gT', [128, 2])
    I['cosC'] = P.din('cosC', [128, LAT])
    I['sinC'] = P.din('sinC', [128, LAT])
    I['router_w'] = P.din('router_w', [D, 16])
    I['router_b'] = P.din('router_b', [1, 16])
    I['moe_w1'] = P.din('moe_w1', [2, NE, D, EDIM])
    I['moe_w3'] = P.din('moe_w3', [2, NE, D, EDIM])
    I['moe_w2'] = P.din('moe_w2', [2, NE, EDIM, D])
    I['consts'] = P.din('consts', [128, 1024])
    out = P.dout('out', [LAT, D])
    modv = [P.dscr('modv%d' % l, [2, 6 * D]) for l in range(2)]
    PF0 = P.dscr('PF0', [5952, T])
    PV0 = P.dscr('PV0', [T, 1024], BF16)
    OF0 = P.dscr('OF0', [1024, T])
    XR = P.dscr('XR', [T, D])
    PF1 = P.dscr('PF1', [4640, T])
    PV1 = P.dscr('PV1', [T, 1280], BF16)
    OF1 = P.dscr('OF1', [1024, T])

    with contextlib.ExitStack() as es:
        S = Sch(nc, es, sbuf_bytes=sbuf_kb * 1024)
        cst = S.sb([128, 1024], F32)
        S.dma('sp', cst, I['consts'], 'ld_small')
        C = {}
        C['ident'] = cst[:, 0:128]
        C['ident_bf'] = S.sb([128, 128], BF16)
        S.cp('dve', C['ident_bf'], cst[:, 0:128])
        C['perm128_bf'] = S.sb([128, 128], BF16)
        S.cp('dve', C['perm128_bf'], cst[:, 128:256])
        C['perm64_bf'] = S.sb([64, 64], BF16)
        S.cp('dve', C['perm64_bf'], cst[0:64, 256:320])
        C['maskf'] = cst[0:64, 320:384]
        C['maskb'] = cst[0:64, 384:448]
        C['rmask'] = cst[:, 512:1024]
        C['ones_bf'] = S.sb([128, 128], BF16)
        S.memset('dve', C['ones_bf'], 1.0)

        def dump(name, src_ap, shape, dt=F32):
            if name in P.dbg:
                d = P.dout(name, shape, dt)
                S.dma('sp', d, src_ap, 'st_small')

        def done():
            return stop_after is not None and stop_after in done.passed
        done.passed = set()

        def layer0():
            stage_mod(P, S, I, modv)
            m0 = S.mark()
            hT = S.sb([128, 16, T], BF16)
            stage_A(P, S, I, I['xin'], modv[0], 0, 1, hT, C['ident_bf'], [(i, i * 128) for i in range(NT)])
            segs = [(0, 1024, 'fm', AF.Silu, 0), (1024, 3072, 'fm', AF.Copy, 1024), (3072, 4096, 'tm', None, 0),
                    (4096, 5120, 'fm', AF.Sigmoid, 4096), (5120, 5952, 'fm', AF.Copy, 5120)]
            stage_inproj(P, S, I['ab_w_in'], 5952, hT, segs, PF0, PV0)
            S.release(m0)
            if stop_after == 'inproj0':
                return
            mixT = S.sb([128, 16, T], BF16)
            if 'skip_mla' not in P.dbg:
                stage_mla(P, S, I, PF0, mixT, C)
            if stop_after == 'mla':
                dump('mixT', mixT.rearrange('p c t -> p (c t)'), [128, 16 * T], BF16)
                return
            cfg = dict(H=8, DVH=1, kind='hgrn', qscale=128 ** -0.5, qbase=0, gbase=4096, mixbase=0, gcol=I['hgrn_gT'])
            stage_scan(P, S, I, cfg, PF0, PV0, OF0, mixT, C)
            dump('mixT', mixT.rearrange('p c t -> p (c t)'), [128, 16 * T], BF16)
            if stop_after == 'scan0':
                return
            stage_outproj(P, S, I, I['ab_w_out'], mixT, modv[0], I['xin'], XR, list(range(NT)))
            S.release(m0)
            dumpXR('XR_a')
            if stop_after == 'out0':
                return
            stage_moe(P, S, I, 0, modv[0], XR, C, [list(range(0, 9)), list(range(9, 18))])

        def dumpXR(name):
            if name in P.dbg:
                d = P.dout(name, [T, D])
                for ti in range(NT):
                    S.dma('sp', d[ti * 128:(ti + 1) * 128, :], XR[ti * 128:(ti + 1) * 128, :], 'st_small')

        def layer1():
            m0 = S.mark()
            hT = S.sb([128, 16, T], BF16)
            stage_A(P, S, I, XR, modv[1], 0, 1, hT, C['ident_bf'], [(i, i * 128) for i in range(NT)])
            segs = [(0, 1024, 'fm', AF.Copy, 0), (1024, 1280, 'fm', AF.Copy, 1024), (1280, 1536, 'tm', None, 0),
                    (1536, 2560, 'fm', AF.Copy, 1536), (2560, 3584, 'tm', None, 256),
                    (3584, 4608, 'fm', AF.Silu, 3584), (4608, 4640, 'fm', AF.Copy, 4608)]
            stage_inproj(P, S, I['cd_w_in'], 4640, hT, segs, PF1, PV1)
            S.release(m0)
            mixT = S.sb([128, 16, T], BF16)
            stage_gqa(P, S, I, PF1, PV1, mixT, C)
            cfg = dict(H=4, DVH=2, kind='gla', qscale=128 ** -0.5, qbase=1536, kbase=2048, abase=4608, gbase=3584,
                       mixbase=8, vbase=256, gcol=I['gla_gT'])
            stage_scan(P, S, I, cfg, PF1, PV1, OF1, mixT, C)
            lat_tiles = list(range(CTX // 128, NT))
            stage_outproj(P, S, I, I['cd_w_out'], mixT, modv[1], XR, XR, lat_tiles)
            S.release(m0)
            dumpXR('XR_c')
            stage_moe(P, S, I, 1, modv[1], XR, C, [lat_tiles[0:8], lat_tiles[8:16]], final_out=out)

        layer0()
        if stop_after is None:
            dumpXR('XR_b')
            layer1()
        else:
            z = S.sb([128, D], F32)
            S.memset('dve', z, 0.0)
            S.dma('sp', out[0:128, :], z, 'st_small')
        S.finish()
        print("instructions:", S.n_inst, "sbuf top", S.sb_top, "dma sems", len(S.dma_sems), sorted(S.dma_sems))
    return P


def _rope_tables(d_rope):
    quarter = d_rope // 4
    half = d_rope // 2
    freqs = (np.float32(10000.0) ** (-np.arange(quarter, dtype=np.float32) / np.float32(quarter))).astype(np.float32)
    rows = LAT // 64
    row = np.repeat(np.arange(rows, dtype=np.float32), 64)
    col = np.tile(np.arange(64, dtype=np.float32), rows)
    ang = np.concatenate([row[:, None] * freqs, col[:, None] * freqs], axis=-1).astype(np.float32)
    c = np.cos(ang).astype(np.float32).T
    s_ = np.sin(ang).astype(np.float32).T
    return np.ascontiguousarray(np.concatenate([c, c], 0)), np.ascontiguousarray(np.concatenate([-s_, s_], 0))


def _consts():
    c = np.zeros((128, 1024), np.float32)
    c[:, 0:128] = np.eye(128, dtype=np.float32)
    k = np.arange(128)
    c[(k + 64) % 128, 128 + k] = 1.0
    k = np.arange(64)
    c[(k + 32) % 64, 256 + k] = 1.0
    c[0:64, 320:384] = np.triu(np.ones((64, 64), np.float32))
    c[0:64, 384:448] = np.tril(np.ones((64, 64), np.float32))
    c[:, 512:1024] = 1.0
    c[:, 512:1024:64] = 0.0
    return c


def host_inputs(inp, b):
    f = lambda a: np.ascontiguousarray(np.asarray(a, np.float32))
    m = {}
    m['xin'] = f(np.concatenate([inp['ctx'][b], inp['x'][b]], 0))
    cc = np.stack([inp['c'][b], inp['c_ctx']], 1)
    m['cT'] = f(cc.reshape(16, 128, 2).transpose(1, 0, 2))
    for k_ in ['mod_w', 'mod_b', 'norm_attn_g', 'norm_ffn_g', 'final_norm_g', 'router_w', 'moe_w1', 'moe_w3', 'moe_w2']:
        m[k_] = f(inp[k_])
    m['ab_w_in'] = f(inp['ab_w_in'][0])
    m['ab_w_out'] = f(inp['ab_w_out'][0])
    m['mla_w_uq'] = f(inp['mla_w_uq'][0])
    m['mla_w_ukv'] = f(inp['mla_w_ukv'][0])
    m['mla_q_gT'] = f(inp['mla_q_norm_g'][0].reshape(4, 128).T)
    m['mla_kv_gT'] = f(inp['mla_kv_norm_g'][0].reshape(2, 128).T)
    m['hgrn_gT'] = f(inp['hgrn_norm_g'][0].reshape(1, 128).T)
    m['lbT'] = f(inp['hgrn_lb_logits'].reshape(2, 3, 8, 128).transpose(3, 0, 1, 2))
    m['cosB'], m['sinB'] = _rope_tables(64)
    m['cd_w_in'] = f(inp['cd_w_in'][0])
    m['cd_w_out'] = f(inp['cd_w_out'][0])
    m['gqa_q_gT'] = f(inp['gqa_q_norm_g'][0].reshape(1, 128).T)
    m['gqa_k_gT'] = f(inp['gqa_k_norm_g'][0].reshape(1, 128).T)
    m['gla_w_a2'] = f(inp['gla_w_a2'][0])
    m['gla_bT'] = f(inp['gla_b_a'][0].reshape(2, 4, 128).transpose(2, 0, 1))
    m['gla_gT'] = f(inp['gla_norm_g'][0].reshape(2, 128).T)
    m['cosC'], m['sinC'] = _rope_tables(128)
    m['router_b'] = f(inp['router_b'].reshape(1, 16))
    m['consts'] = _consts()
    return m


_PROG = None
NCORES = 4


def kernel(**inputs):
    global _PROG
    if _PROG is None:
        _PROG = build()
    P = _PROG
    inp = {k: np.asarray(v) for k, v in inputs.items()}
    in_maps = []
    for c in range(NCORES):
        m = host_inputs(inp, c % 4)
        in_maps.append({k: v for k, v in m.items() if k in P.inputs})
    res = run_bass_kernel_spmd(P.nc, in_maps, core_ids=list(range(NCORES)))
    outs = [np.asarray(res.results[b]['out'], np.float32) for b in range(4)]
    return np.stack(outs, 0)
```

```python
import contextlib
import numpy as np
import concourse.bass as bass
import concourse.mybir as mybir
from concourse.bass_utils import run_bass_kernel_spmd

F32 = mybir.dt.float32
BF16 = mybir.dt.bfloat16
AF = mybir.ActivationFunctionType
ALU = mybir.AluOpType
DT_SIZE = {F32: 4, BF16: 2}

D = 2048
CTX = 256
LAT = 2048
T = CTX + LAT
NT = T // 128
EPS = 1e-6
NE = 16
EDIM = 512
HALF = LAT // 2


def _box(ap):
    esz = DT_SIZE[ap.dtype]
    pat = ap.ap
    off = ap.offset
    name = ap.tensor.name
    if str(ap.space) == 'DRAM':
        hi = off + sum((c - 1) * s for s, c in pat) + 1
        return (name, 0, 1, off * esz, hi * esz)
    pstep, pcnt = pat[0]
    if pstep == 0:
        p0 = 0
        f0 = off
        pcnt = 128
    else:
        p0 = off // pstep
        f0 = off - p0 * pstep
    f1 = f0 + sum((c - 1) * s for s, c in pat[1:]) + 1
    return (name, p0, p0 + pcnt, f0 * esz, f1 * esz)


def _ov(a, b):
    return a[1] < b[2] and b[1] < a[2] and a[3] < b[4] and b[3] < a[4]


class Sch:
    ENG = ('pe', 'act', 'dve', 'pool', 'sp')

    def __init__(s, nc, es, sbuf_bytes=192 * 1024):
        s.nc = nc
        s.es = es
        s.eng = {'pe': nc.tensor, 'act': nc.scalar, 'dve': nc.vector, 'pool': nc.gpsimd, 'sp': nc.sync}
        s.sem = {e: es.enter_context(nc.semaphore('sem_' + e)) for e in s.ENG}
        s.cnt = {e: 0 for e in s.ENG}
        s.waited = {}
        s.regions = {}
        s.dma_sems = {}
        s.big = es.enter_context(nc.sbuf_tensor('SB', [128, sbuf_bytes // 4], F32))
        s.sb_top = 0
        s.sb_cap = sbuf_bytes
        s.ps = es.enter_context(nc.psum_tensor('PS', [128, 4096], F32))
        s.n_inst = 0
        s.dom_map = {}
        import os
        s.npool = int(os.environ.get('NPOOL', '1000'))
        s.waitall = os.environ.get('WAITALL', '1') == '1'
        s.serial = os.environ.get('SERIAL', '0') == '1'

    def mark(s):
        return s.sb_top

    def release(s, m):
        s.sb_top = m

    @staticmethod
    def _shape(v, shape):
        if len(shape) > 2:
            names = ' '.join('d%d' % i for i in range(1, len(shape)))
            v = v.rearrange('p (%s) -> p %s' % (names, names), **{'d%d' % i: shape[i] for i in range(2, len(shape))})
        return v

    def sb(s, shape, dt):
        n = int(np.prod(shape[1:]))
        nb = (n * DT_SIZE[dt] + 63) // 64 * 64
        assert s.sb_top + nb <= s.sb_cap, ('SBUF OOM', s.sb_top, nb)
        v = s.big[0:shape[0], s.sb_top // 4:(s.sb_top + nb) // 4]
        s.sb_top += nb
        if dt != F32:
            v = v.bitcast(dt)
        return s._shape(v[:, 0:n], shape)

    def psum(s, bank, shape, dt=F32, off=0):
        n = int(np.prod(shape[1:]))
        nb = n * DT_SIZE[dt]
        assert off % 4 == 0 and off + nb <= 2048 * (8 - bank)
        v = s.ps[0:shape[0], bank * 512 + off // 4: bank * 512 + (off + nb + 3) // 4]
        if dt != F32:
            v = v.bitcast(dt)
        return s._shape(v[:, 0:n], shape)

    def _deps(s, reads, writes, me):
        deps = {}
        rb = [_box(a) for a in reads]
        wb = [_box(a) for a in writes]
        for b in rb:
            for r in s.regions.get(b[0], ()):
                if _ov(r[0], b):
                    for dom, val in r[1].items():
                        if deps.get(dom, 0) < val:
                            deps[dom] = val
        for b in wb:
            for r in s.regions.get(b[0], ()):
                if _ov(r[0], b):
                    for dom, val in r[1].items():
                        if deps.get(dom, 0) < val:
                            deps[dom] = val
                    for dom, val in r[2].items():
                        if dom != me and deps.get(dom, 0) < val:
                            deps[dom] = val
        return deps, rb, wb

    def _record(s, rb, wb, dom, val):
        for b in rb:
            lst = s.regions.setdefault(b[0], [])
            found = None
            for r in lst:
                if r[0] == b:
                    found = r
                elif _ov(r[0], b):
                    r[2][dom] = val
            if found is None:
                w = {}
                for r in lst:
                    if _ov(r[0], b):
                        for d, v in r[1].items():
                            if w.get(d, 0) < v:
                                w[d] = v
                found = [b, w, {}]
                lst.append(found)
            found[2][dom] = val
        for b in wb:
            lst = s.regions.setdefault(b[0], [])
            found = None
            for r in lst:
                if r[0] == b:
                    found = r
                elif _ov(r[0], b):
                    q = r[0]
                    if b[1] <= q[1] and q[2] <= b[2] and b[3] <= q[3] and q[4] <= b[4]:
                        r[1] = {dom: val}
                        r[2] = {}
                    else:
                        r[1][dom] = val
            if found is None:
                found = [b, {}, {}]
                lst.append(found)
            found[1] = {dom: val}
            found[2] = {}
        for b in rb + wb:
            lst = s.regions[b[0]]
            if len(lst) > 400:
                s._compact(b[0])

    def _compact(s, name):
        lst = s.regions[name]
        half = len(lst) // 2
        old = lst[:half]
        p0 = min(r[0][1] for r in old)
        p1 = max(r[0][2] for r in old)
        f0 = min(r[0][3] for r in old)
        f1 = max(r[0][4] for r in old)
        w = {}
        rd = {}
        for r in old:
            for d, v in r[1].items():
                if w.get(d, 0) < v:
                    w[d] = v
            for d, v in r[2].items():
                if rd.get(d, 0) < v:
                    rd[d] = v
        s.regions[name] = [[(name + '#old', p0, p1, f0, f1), w, rd]] + lst[half:]

    def _emit_waits(s, e, deps):
        eng = s.eng[e]
        if s.serial:
            deps = {d: v for d, v in s.cnt.items() if v > 0}
        for dom, val in deps.items():
            if dom == e and e == 'pe':
                continue
            if dom in s.dma_sems and s.waitall:
                val = s.cnt[dom]
            if s.waited.get((e, dom), 0) >= val:
                continue
            s.waited[(e, dom)] = val
            sem = s.sem[dom] if dom in s.sem else s.dma_sems[dom]
            eng.wait_ge(sem, val)
            s.n_inst += 1

    def op(s, e, fn, reads, writes):
        deps, rb, wb = s._deps(reads, writes, e)
        s._emit_waits(e, deps)
        ins = fn(s.eng[e])
        s.n_inst += 1
        s.cnt[e] += 1
        ins.then_inc(s.sem[e], 1)
        s._record(rb, wb, e, s.cnt[e])
        return ins

    def dma(s, e, out, in_, dom):
        if dom not in s.dom_map:
            s.dom_map[dom] = 'dq%d' % (len(s.dom_map) % s.npool)
        dom = s.dom_map[dom]
        if dom not in s.dma_sems:
            s.dma_sems[dom] = s.es.enter_context(s.nc.semaphore(dom))
            s.cnt[dom] = 0
        deps, rb, wb = s._deps([in_], [out], dom)
        if s.cnt[dom] > 0:
            deps[dom] = s.cnt[dom]
        s._emit_waits(e, deps)
        s.cnt[dom] += 16
        s.eng[e].dma_start(out=out, in_=in_).then_inc(s.dma_sems[dom], 16)
        s.n_inst += 1
        s._record(rb, wb, dom, s.cnt[dom])

    def finish(s):
        for dom in s.dma_sems:
            if s.cnt[dom] > 0:
                s.eng['sp'].wait_ge(s.dma_sems[dom], s.cnt[dom])

    def act(s, out, in_, func, bias=None, scale=None, accum_out=None, eng='act'):
        kw = {}
        rd = [in_]
        wr = [out]
        if bias is not None:
            kw['bias'] = bias
            if not isinstance(bias, (int, float)):
                rd.append(bias)
        if scale is not None:
            kw['scale'] = scale
            if not isinstance(scale, (int, float)):
                rd.append(scale)
        if accum_out is not None:
            kw['accum_out'] = accum_out
            wr.append(accum_out)
        return s.op('act', lambda e: e.activation(out=out, in_=in_, func=func, **kw), rd, wr)

    def tt(s, eng, out, in0, in1, op):
        return s.op(eng, lambda e: e.tensor_tensor(out=out, in0=in0, in1=in1, op=op), [in0, in1], [out])

    def ts(s, eng, out, in0, s1, op0, s2=None, op1=None):
        rd = [in0] + [x for x in (s1, s2) if x is not None and not isinstance(x, (int, float))]
        if op1 is None:
            return s.op(eng, lambda e: e.tensor_scalar(out=out, in0=in0, scalar1=s1, scalar2=None, op0=op0), rd, [out])
        return s.op(eng, lambda e: e.tensor_scalar(out=out, in0=in0, scalar1=s1, scalar2=s2, op0=op0, op1=op1), rd, [out])

    def stt(s, out, in0, sc, in1, op0, op1):
        rd = [in0, in1] + ([] if isinstance(sc, (int, float)) else [sc])
        return s.op('dve', lambda e: e.scalar_tensor_tensor(out=out, in0=in0, scalar=sc, in1=in1, op0=op0, op1=op1), rd, [out])

    def cp(s, eng, out, in_):
        if eng == 'act':
            return s.op('act', lambda e: e.copy(out=out, in_=in_), [in_], [out])
        return s.op(eng, lambda e: e.tensor_copy(out=out, in_=in_), [in_], [out])

    def recip(s, out, in_):
        return s.op('dve', lambda e: e.reciprocal(out=out, in_=in_), [in_], [out])

    def memset(s, eng, out, val):
        return s.op(eng, lambda e: e.memset(out, val), [], [out])

    def mm(s, out, pairs):
        n = len(pairs)

        def fn(e):
            ins = None
            for i, (l, r) in enumerate(pairs):
                ins = e.matmul(out, l, r, start=(i == 0), stop=(i == n - 1))
            return ins
        rd = []
        for l, r in pairs:
            rd.append(l)
            rd.append(r)
        s.n_inst += n - 1
        return s.op('pe', fn, rd, [out])

    def mm1(s, out, l, r, start, stop):
        return s.op('pe', lambda e: e.matmul(out, l, r, start=start, stop=stop), [l, r], [out])

    def tr(s, out, in_, ident):
        return s.op('pe', lambda e: e.transpose(out, in_, ident), [in_, ident], [out])


class Prog:
    def __init__(p, dbg=()):
        p.dbg = set(dbg)
        p.nc = bass.Bass("TRN2", target_bir_lowering=False)
        p.inputs = {}
        p.outputs = {}

    def din(p, name, shape, dt=F32):
        a = p.nc.dram_tensor(name, list(shape), dt, kind="ExternalInput").ap()
        p.inputs[name] = a
        return a

    def dscr(p, name, shape, dt=F32):
        kind = "ExternalOutput" if name in p.dbg else "Internal"
        a = p.nc.dram_tensor(name, list(shape), dt, kind=kind).ap()
        if name in p.dbg:
            p.outputs[name] = a
        return a

    def dout(p, name, shape, dt=F32):
        a = p.nc.dram_tensor(name, list(shape), dt, kind="ExternalOutput").ap()
        p.outputs[name] = a
        return a


def kc_view(w, cols=None):
    v = w.rearrange("(c p) f -> p c f", p=128)
    if cols is not None:
        v = v[:, :, cols[0]:cols[1]]
    return v


def stage_mod(P, S, I, modv):
    m = S.mark()
    cs32 = S.sb([128, 16, 2], F32)
    cs = S.sb([128, 16, 2], BF16)
    S.dma('sp', cs32, I['cT'], 'ld_small')
    S.act(cs, cs32, AF.Silu)
    msb = S.sb([2, 6 * D], F32)
    mb = S.sb([2, 6 * D], F32)
    gA = S.sb([2, D], F32)
    gF = S.sb([2, D], F32)
    wb = [S.sb([128, 16, 512], BF16) for _ in range(3)]
    k = 0
    for l in range(2):
        S.dma('sp', mb, I['mod_b'][l:l + 1, :].to_broadcast([2, 6 * D]), 'ld_small')
        S.dma('sp', gA, I['norm_attn_g'][l:l + 1, :].to_broadcast([2, D]), 'ld_small')
        S.dma('sp', gF, I['norm_ffn_g'][l:l + 1, :].to_broadcast([2, D]), 'ld_small')
        for j in range(24):
            w = wb[k % 3]
            S.dma('pool', w, kc_view(I['mod_w'][l], (j * 512, (j + 1) * 512)), 'ld_w%d' % (k % 3))
            ps = S.psum(k % 2, [2, 512])
            S.mm(ps, [(cs[:, c, :], w[:, c, :]) for c in range(16)])
            S.tt('dve', msb[:, j * 512:(j + 1) * 512], ps, mb[:, j * 512:(j + 1) * 512], ALU.add)
            k += 1
        S.stt(msb[:, D:2 * D], msb[:, D:2 * D], 1.0, gA, ALU.add, ALU.mult)
        S.stt(msb[:, 4 * D:5 * D], msb[:, 4 * D:5 * D], 1.0, gF, ALU.add, ALU.mult)
        S.dma('sp', modv[l], msb, 'st_small')
    S.release(m)


def load_bc(S, dst, modv_l, row, k, dom):
    S.dma('sp', dst, modv_l[row:row + 1, k * D:(k + 1) * D].to_broadcast([128, D]), dom)


def norm_mod_transpose(S, xt, G, Sf, hT_dst, ident_bf, junk, hb, small, ps_banks):
    ssq = small[:, 0:1]
    S.act(junk, xt, AF.Square)
    S.op('dve', lambda e, ssq=ssq, junk=junk: e.tensor_reduce(out=ssq, in_=junk, axis=mybir.AxisListType.X, op=ALU.add), [junk], [ssq])
    S.ts('dve', ssq, ssq, 1.0 / D, ALU.mult, EPS, ALU.add)
    S.act(ssq, ssq, AF.Sqrt)
    S.recip(ssq, ssq)
    S.stt(junk, xt, ssq, G, ALU.mult, ALU.mult)
    S.tt('pool', hb, junk, Sf, ALU.add)
    for half in range(2):
        ps = S.psum(ps_banks[half], [128, 8, 128], BF16)
        for c in range(8):
            S.tr(ps[:, c, :], hb[:, (half * 8 + c) * 128:(half * 8 + c + 1) * 128], ident_bf)
        if half == 0:
            S.cp('act', hT_dst[:, 0:8, :], ps)
        else:
            S.cp('dve', hT_dst[:, 8:16, :], ps)


def stage_A(P, S, I, xres, modv_l, kS, kG, hT, ident_bf, tiles, ps_banks=(6, 7)):
    m = S.mark()
    Gl = S.sb([128, D], F32)
    Sl = S.sb([128, D], F32)
    Gc = S.sb([128, D], F32)
    Sc = S.sb([128, D], F32)
    load_bc(S, Gl, modv_l, 0, kG, 'ld_bc')
    load_bc(S, Sl, modv_l, 0, kS, 'ld_bc')
    load_bc(S, Gc, modv_l, 1, kG, 'ld_bc')
    load_bc(S, Sc, modv_l, 1, kS, 'ld_bc')
    xts = [S.sb([128, D], F32) for _ in range(2)]
    junk = S.sb([128, D], F32)
    hb = S.sb([128, D], BF16)
    small = S.sb([128, 4], F32)
    for n, (ti, dc) in enumerate(tiles):
        xt = xts[n % 2]
        S.dma('sp', xt, xres[ti * 128:(ti + 1) * 128, :], 'ld_x%d' % (n % 2))
        isctx = ti < CTX // 128
        norm_mod_transpose(S, xt, Gc if isctx else Gl, Sc if isctx else Sl, hT[:, :, dc:dc + 128], ident_bf,
                           junk, hb, small, ps_banks)
    S.release(m)


def stage_inproj(P, S, w_in, F, hT, segs, PF, PV, wdom='ld_w'):
    m = S.mark()
    wb = [S.sb([128, 16, 512], BF16) for _ in range(3)]
    stg = [S.sb([128, 512], F32) for _ in range(3)]
    stgb = [S.sb([128, 512], BF16) for _ in range(2)]
    nblk = (F + 511) // 512
    k = 0
    kk = 0
    ttiles = [(t0, min(512, T - t0)) for t0 in range(0, T, 512)]
    for bi in range(nblk):
        c0 = bi * 512
        c1 = min(F, c0 + 512)
        w = wb[bi % 3]
        S.dma('pool', w[:, :, 0:c1 - c0], kc_view(w_in, (c0, c1)), '%s%d' % (wdom, bi % 3))
        for (lo, hi, kind, func, base) in segs:
            a = max(lo, c0)
            b = min(hi, c1)
            if a >= b:
                continue
            if kind == 'fm':
                for cc in range(a, b, 128):
                    ncol = min(128, b - cc)
                    for (t0, tn) in ttiles:
                        ps = S.psum(k % 4, [ncol, tn])
                        S.mm(ps, [(w[:, c, cc - c0:cc - c0 + ncol], hT[:, c, t0:t0 + tn]) for c in range(16)])
                        st = stg[k % 3]
                        if k % 2 == 0 or func != AF.Copy:
                            S.act(st[0:ncol, 0:tn], ps, func)
                        else:
                            S.cp('dve', st[0:ncol, 0:tn], ps)
                        S.dma('sp', PF[base + cc - lo: base + cc - lo + ncol, t0:t0 + tn], st[0:ncol, 0:tn], 'st_pf%d' % (k % 3))
                        k += 1
            else:
                wd = b - a
                for ti in range(NT):
                    ps = S.psum(4 + kk % 2, [128, wd])
                    S.mm(ps, [(hT[:, c, ti * 128:(ti + 1) * 128], w[:, c, a - c0:b - c0]) for c in range(16)])
                    st = stgb[kk % 2][:, 0:wd]
                    if kk % 2 == 0:
                        S.cp('act', st, ps)
                    else:
                        S.cp('dve', st, ps)
                    S.dma('sp', PV[ti * 128:(ti + 1) * 128, base + a - lo: base + b - lo], st, 'st_pv%d' % (kk % 2))
                    kk += 1
    S.release(m)


LT_TILES = [(0, 256)] + [(256 + 512 * i, 512) for i in range(4)]


def bc_mid(ap, n):
    return ap.to_broadcast([ap.shape[0], ap.shape[1], n])


def ssq_rstd(S, chunks, n, dim, ones_bf, sqb, rstd_out, bank):
    ps = S.psum(bank, [128, n])
    for i, ch in enumerate(chunks):
        R = ch.shape[0]
        sq = sqb[i % len(sqb)][0:R, 0:n]
        S.act(sq, ch, AF.Square)
        S.mm1(ps, ones_bf[0:R, :], sq, i == 0, i == len(chunks) - 1)
    S.ts('dve', rstd_out, ps, 1.0 / dim, ALU.mult, EPS, ALU.add)
    S.act(rstd_out, rstd_out, AF.Sqrt)
    S.recip(rstd_out, rstd_out)


def rope_fm(S, x32, R, n, cos, sin, perm_bf, out_bf, xb, tb, bank):
    S.cp('pool', xb[0:R, 0:n], x32)
    ps = S.psum(bank, [R, n])
    S.mm(ps, [(perm_bf[0:R, 0:R], xb[0:R, 0:n])])
    S.tt('pool', tb[0:R, 0:n], x32, cos, ALU.mult)
    S.tt('dve', x32, ps, sin, ALU.mult)
    S.tt('dve', out_bf, x32, tb[0:R, 0:n], ALU.add)


def attn_core(S, qparts, kparts, v, q0, nq, stiles, scale, dst, ones_bf, PTs, rden, par, cnt):
    oT = S.psum(2 + 2 * par, [128, nq])
    den = S.psum(3 + 2 * par, [128, nq])
    ns = len(stiles)
    for i, si in enumerate(stiles):
        sc = S.psum(cnt[0] % 2, [128, nq])
        S.mm(sc, [(kp[:, si * 128:(si + 1) * 128], qp[:, q0:q0 + nq]) for kp, qp in zip(kparts, qparts)])
        pt = PTs[cnt[0] % 2][:, 0:nq]
        cnt[0] += 1
        S.act(pt, sc, AF.Exp, scale=scale)
        S.mm1(oT, v[:, si, :], pt, i == 0, i == ns - 1)
        S.mm1(den, ones_bf, pt, i == 0, i == ns - 1)
    S.recip(rden[:, 0:nq], den)
    S.tt('dve', dst, oT, rden[:, 0:nq], ALU.mult)


def stage_mla(P, S, I, PF, mixT, C, need_ctx=True):
    import os
    ROT = 0 if os.environ.get('MLA_NOROPE') == '1' else CTX
    ROT = ROT if ROT else 10 ** 9
    m = S.mark()
    ones_bf = C['ones_bf']
    wuq = S.sb([128, 4, 1536], BF16)
    wukv = S.sb([128, 2, 2048], BF16)
    S.dma('pool', wuq, kc_view(I['mla_w_uq']), 'ld_w0')
    S.dma('pool', wukv, kc_view(I['mla_w_ukv']), 'ld_w1')
    gq = S.sb([128, 4], F32)
    gkv = S.sb([128, 2], F32)
    S.dma('sp', gq, I['mla_q_gT'], 'ld_small')
    S.dma('sp', gkv, I['mla_kv_gT'], 'ld_small')
    cosB = S.sb([64, LAT], F32)
    sinB = S.sb([64, LAT], F32)
    S.dma('sp', cosB, I['cosB'], 'ld_small')
    S.dma('sp', sinB, I['sinB'], 'ld_small')
    nqT = S.sb([128, 4, T], BF16)
    nkvT = S.sb([128, 2, T], BF16)
    krT = S.sb([64, T], BF16)
    sqb = [S.sb([128, 512], BF16) for _ in range(2)]
    rstd = S.sb([128, 512], F32)
    xb = S.sb([128, 512], BF16)
    tb = S.sb([128, 512], F32)
    x32 = S.sb([64, 512], F32)
    m1 = S.mark()
    p5 = S.sb([128, 4, 512], F32)
    p6 = S.sb([128, 2, 512], F32)
    p7 = S.sb([64, 512], F32)
    for (t0, n) in LT_TILES:
        S.dma('sp', p5[:, :, 0:n], PF[5120:5632, t0:t0 + n].rearrange('(c p) t -> p c t', p=128), 'ld_p5')
        S.dma('sp', p6[:, :, 0:n], PF[5632:5888, t0:t0 + n].rearrange('(c p) t -> p c t', p=128), 'ld_p6')
        S.dma('sp', p7[:, 0:n], PF[5888:5952, t0:t0 + n], 'ld_p7')
        ssq_rstd(S, [p5[:, c, 0:n] for c in range(4)], n, 512, ones_bf, sqb, rstd[:, 0:n], 6)
        for c in range(4):
            S.stt(nqT[:, c, t0:t0 + n], p5[:, c, 0:n], gq[:, c:c + 1], rstd[:, 0:n], ALU.mult, ALU.mult)
        ssq_rstd(S, [p6[:, c, 0:n] for c in range(2)], n, 256, ones_bf, sqb, rstd[:, 0:n], 7)
        for c in range(2):
            S.stt(nkvT[:, c, t0:t0 + n], p6[:, c, 0:n], gkv[:, c:c + 1], rstd[:, 0:n], ALU.mult, ALU.mult)
        if t0 >= ROT:
            l0 = t0 - CTX
            rope_fm(S, p7[:, 0:n], 64, n, cosB[:, l0:l0 + n], sinB[:, l0:l0 + n], C['perm64_bf'], krT[:, t0:t0 + n], xb, tb, 6)
        else:
            S.cp('pool', krT[:, t0:t0 + n], p7[:, 0:n])
    S.release(m1)
    hb = []
    for _ in range(1):
        hb.append((S.sb([128, T], BF16), S.sb([64, T], BF16), S.sb([128, T], BF16), S.sb([128, NT, 128], BF16)))
    PTs = [S.sb([128, 512], BF16) for _ in range(2)]
    rden = S.sb([128, 512], F32)
    cnt = [0]
    scale = float((128 + 64) ** -0.5)
    par = 0
    for h in range(8):
        qn, qr, kn, vh = hb[0]
        for (t0, n) in LT_TILES:
            ps = S.psum(6, [128, n])
            S.mm(ps, [(wuq[:, c, h * 192:h * 192 + 128], nqT[:, c, t0:t0 + n]) for c in range(4)])
            S.cp('act', qn[:, t0:t0 + n], ps)
            ps2 = S.psum(7, [64, n])
            S.mm(ps2, [(wuq[:, c, h * 192 + 128:h * 192 + 192], nqT[:, c, t0:t0 + n]) for c in range(4)])
            if t0 >= ROT:
                l0 = t0 - CTX
                S.cp('act', x32[:, 0:n], ps2)
                rope_fm(S, x32[:, 0:n], 64, n, cosB[:, l0:l0 + n], sinB[:, l0:l0 + n], C['perm64_bf'], qr[:, t0:t0 + n], xb, tb, 7)
            else:
                S.cp('act', qr[:, t0:t0 + n], ps2)
            ps = S.psum(6, [128, n])
            S.mm(ps, [(wukv[:, c, h * 256:h * 256 + 128], nkvT[:, c, t0:t0 + n]) for c in range(2)])
            S.cp('dve', kn[:, t0:t0 + n], ps)
            nb = n // 128
            psv = S.psum(7, [128, nb, 128])
            for j in range(nb):
                tk = t0 + j * 128
                S.mm(psv[:, j, :], [(nkvT[:, c, tk:tk + 128], wukv[:, c, h * 256 + 128:(h + 1) * 256]) for c in range(2)])
            S.cp('dve', vh[:, t0 // 128:t0 // 128 + nb, :], psv)
        if need_ctx:
            attn_core(S, [qn, qr], [kn, krT], vh, 0, 256, [0, 1], scale, mixT[:, 8 + h, 0:256], ones_bf, PTs, rden, par, cnt)
            par ^= 1
        for qi in range(4):
            q0 = CTX + 512 * qi
            attn_core(S, [qn, qr], [kn, krT], vh, q0, 512, list(range(NT)), scale, mixT[:, 8 + h, q0:q0 + 512], ones_bf, PTs, rden, par, cnt)
            par ^= 1
    S.release(m)


def stage_gqa(P, S, I, PF, PV, mixT, C):
    m = S.mark()
    ones_bf = C['ones_bf']
    gq = S.sb([128, 1], F32)
    gk = S.sb([128, 1], F32)
    S.dma('sp', gq, I['gqa_q_gT'], 'ld_small')
    S.dma('sp', gk, I['gqa_k_gT'], 'ld_small')
    cosC = S.sb([128, LAT], F32)
    sinC = S.sb([128, LAT], F32)
    S.dma('sp', cosC, I['cosC'], 'ld_small')
    S.dma('sp', sinC, I['sinC'], 'ld_small')
    kT = S.sb([128, T], BF16)
    qT = S.sb([128, T], BF16)
    vh = S.sb([128, NT, 128], BF16)
    sqb = [S.sb([128, 512], BF16) for _ in range(2)]
    rstd = S.sb([128, 512], F32)
    xb = S.sb([128, 512], BF16)
    tb = S.sb([128, 512], F32)
    x32 = S.sb([128, 512], F32)
    p = S.sb([128, 512], F32)
    PTs = [S.sb([128, 512], BF16) for _ in range(2)]
    rden = S.sb([128, 512], F32)
    cnt = [0]
    par = 0
    scale = float(128 ** -0.5)

    def prep(row0, gcol, dst, tiles):
        for (t0, n) in tiles:
            S.dma('sp', p[:, 0:n], PF[row0:row0 + 128, t0:t0 + n], 'ld_p5')
            ssq_rstd(S, [p[:, 0:n]], n, 128, ones_bf, sqb, rstd[:, 0:n], 6)
            if t0 >= CTX:
                l0 = t0 - CTX
                S.stt(x32[:, 0:n], p[:, 0:n], gcol, rstd[:, 0:n], ALU.mult, ALU.mult)
                rope_fm(S, x32[:, 0:n], 128, n, cosC[:, l0:l0 + n], sinC[:, l0:l0 + n], C['perm128_bf'], dst[:, t0:t0 + n], xb, tb, 7)
            else:
                S.stt(dst[:, t0:t0 + n], p[:, 0:n], gcol, rstd[:, 0:n], ALU.mult, ALU.mult)

    for g in range(2):
        prep(1024 + g * 128, gk[:, 0:1], kT, LT_TILES)
        S.dma('sp', vh, PV[:, g * 128:(g + 1) * 128].rearrange('(j p) v -> p j v', p=128), 'ld_v0')
        for i in range(4):
            h = g * 4 + i
            prep(h * 128, gq[:, 0:1], qT, LT_TILES[1:3])
            for qi in range(2):
                q0 = CTX + 512 * qi
                attn_core(S, [qT], [kT], vh, q0, 512, list(range(NT)), scale, mixT[:, h, q0:q0 + 512], ones_bf, PTs, rden, par, cnt)
                par ^= 1
    S.release(m)


def stage_scan(P, S, I, cfg, PF, PV, OF, mixT, C):
    H = cfg['H']
    DVH = cfg['DVH']
    dv = 128 * DVH
    X = H * DVH
    kind = cfg['kind']
    vbase = cfg.get('vbase', 0)
    m = S.mark()
    ident_bf = C['ident_bf']
    ones_bf = C['ones_bf']
    NB = 1
    qt = [S.sb([128, H, 512], BF16) for _ in range(NB)]
    kt = [S.sb([128, H, 512], BF16) for _ in range(NB)]
    kh = [S.sb([64, H, 8, 128], BF16) for _ in range(NB)]
    vv = [S.sb([64, H, 8, dv], BF16) for _ in range(NB)]
    dec = [S.sb([128, H, 8], F32) for _ in range(NB)]
    tq = [S.sb([128, 512], F32) for _ in range(2)]
    tk = [S.sb([128, 512], F32) for _ in range(2)]
    tg = [S.sb([128, 512], F32) for _ in range(2)]
    tc_ = [S.sb([128, 512], F32) for _ in range(2)]
    ta = [S.sb([128, 512], F32) for _ in range(2)]
    te = [S.sb([128, 512], F32) for _ in range(2)]
    kht = [S.sb([128, 512], BF16) for _ in range(2)]
    Sst = S.sb([128, H, dv], F32)
    Sbf = S.sb([128, H, dv], BF16)
    otile = S.sb([128, X, 512], F32)
    scmb = [S.sb([64, H, 64], BF16) for _ in range(2)]
    gcol = S.sb([128, DVH], F32)
    S.dma('sp', gcol, cfg['gcol'], 'ld_small')
    if kind == 'hgrn':
        lbt = S.sb([128, 2, 3, 8], F32)
        S.dma('sp', lbt, I['lbT'], 'ld_small')
        S.act(lbt, lbt, AF.Exp)
        lsum = S.sb([128, 2, 8], F32)
        S.tt('dve', lsum, lbt[:, :, 0, :], lbt[:, :, 1, :], ALU.add)
        S.tt('dve', lsum, lsum, lbt[:, :, 2, :], ALU.add)
        S.recip(lsum, lsum)
        lb = S.sb([128, 2, 8], F32)
        oml = S.sb([128, 2, 8], F32)
        S.tt('dve', lb, lbt[:, :, 0, :], lsum, ALU.mult)
        S.ts('dve', oml, lb, -1.0, ALU.mult, 1.0, ALU.add)
    else:
        wa2 = S.sb([16, 2, 512], BF16)
        S.dma('pool', wa2, I['gla_w_a2'].rearrange('d r f -> r d f'), 'ld_w0')
        negb = S.sb([128, 2, 4], F32)
        S.dma('sp', negb, I['gla_bT'], 'ld_small')
        S.ts('dve', negb, negb, -1.0, ALU.mult)
        a32 = S.sb([16, 512], F32)
        abf = S.sb([16, 512], BF16)
    import os
    _dirs = [int(x) for x in os.environ.get('SCAN_DIRS', '0,1').split(',')]
    _nt = int(os.environ.get('SCAN_TILES', '5'))
    _noch = os.environ.get('SCAN_NOCHUNK') == '1'
    _nopost = os.environ.get('SCAN_NOPOST') == '1'
    k = 0
    for d in _dirs:
        order = LT_TILES if d == 0 else [LT_TILES[0]] + LT_TILES[:0:-1]
        mask = C['maskf'] if d == 0 else C['maskb']
        mask_b = mask.unsqueeze(1).to_broadcast([64, H, 64])
        S.memset('dve', Sst, 0.0)
        S.memset('pool', Sbf, 0.0)
        for ti_, (t0, n) in enumerate(order[:_nt]):
            nch = n // 64
            b = ti_ % NB
            if kind == 'gla':
                S.dma('sp', a32[:, 0:n], PF[cfg['abase'] + 16 * d: cfg['abase'] + 16 * d + 16, t0:t0 + n], 'ld_a')
                S.cp('act', abf[:, 0:n], a32[:, 0:n])
            for h in range(H):
                r = k % 2
                k += 1
                q = tq[r][:, 0:n]
                kk = tk[r][:, 0:n]
                g = tg[r][:, 0:n]
                c = tc_[r][:, 0:n]
                a = ta[r][:, 0:n]
                e_ = te[r][:, 0:n]
                S.dma('sp', q, PF[cfg['qbase'] + h * 128: cfg['qbase'] + (h + 1) * 128, t0:t0 + n], 'ld_q%d' % r)
                if kind == 'hgrn':
                    lo = 1024 * (1 + d) + h * 128
                    S.dma('sp', kk, PF[lo:lo + 128, t0:t0 + n], 'ld_k%d' % r)
                    S.act(kk, kk, AF.Sigmoid)
                    S.ts('dve', kk, kk, oml[:, d, h:h + 1], ALU.mult, lb[:, d, h:h + 1], ALU.add)
                    S.act(g, kk, AF.Ln)
                    S.ts('pool', kk, kk, -1.0, ALU.mult, 1.0, ALU.add)
                else:
                    S.dma('sp', kk, PF[cfg['kbase'] + h * 128: cfg['kbase'] + (h + 1) * 128, t0:t0 + n], 'ld_k%d' % r)
                    zp = S.psum(7, [128, n])
                    S.mm(zp, [(wa2[:, d, h * 128:(h + 1) * 128], abf[:, 0:n])])
                    S.act(e_, zp, AF.Exp, scale=-1.0, bias=negb[:, d, h:h + 1])
                    S.act(g, e_, AF.Ln, bias=1.0)
                    S.ts('dve', g, g, -1.0 / 16.0, ALU.mult)
                S.op('dve', lambda e, c=c, g=g, n=n: e.tensor_tensor_scan(out=c, data0=C['rmask'][:, 0:n], data1=g, initial=0.0,
                                                                            op0=ALU.mult, op1=ALU.add), [C['rmask'][:, 0:n], g], [c])
                c3 = c.rearrange('p (j s) -> p j s', s=64)
                tot = c3[:, :, 63:64]
                if d == 1:
                    S.tt('dve', a, g, c, ALU.subtract)
                    S.tt('dve', a.rearrange('p (j s) -> p j s', s=64), a.rearrange('p (j s) -> p j s', s=64), bc_mid(tot, 64), ALU.add)
                else:
                    a = c
                S.act(e_, a, AF.Exp)
                S.stt(qt[b][:, h, 0:n], q, float(cfg['qscale']), e_, ALU.mult, ALU.mult)
                S.act(e_, a, AF.Exp, scale=-1.0)
                S.tt('dve', kt[b][:, h, 0:n], kk, e_, ALU.mult)
                S.tt('dve', g.rearrange('p (j s) -> p j s', s=64), bc_mid(tot, 64), a.rearrange('p (j s) -> p j s', s=64), ALU.subtract)
                S.act(g, g, AF.Exp)
                S.tt('pool', kht[r][:, 0:n], kk, g, ALU.mult)
                S.act(dec[b][:, h, 0:nch], c3[:, :, 63], AF.Exp)
                psT = S.psum(6, [64, 8, 128], BF16)
                for j in range(nch):
                    S.tr(psT[:, j, :], kht[r][:, j * 64:(j + 1) * 64], ident_bf)
                S.cp('act', kh[b][:, h, 0:nch, :], psT[:, 0:nch, :])
                S.dma('sp', vv[b][:, h, 0:nch, :], PV[t0:t0 + n, vbase + h * dv:vbase + (h + 1) * dv].rearrange('(j p) v -> p j v', p=64), 'ld_v%d' % b)
            chs = list(range(nch)) if d == 0 else list(range(nch - 1, -1, -1))
            if _noch:
                chs = []
            for ji, j in enumerate(chs):
                sc = S.psum(ji % 2, [64, H, 64])
                for h in range(H):
                    S.mm(sc[:, h, :], [(kt[b][:, h, j * 64:(j + 1) * 64], qt[b][:, h, j * 64:(j + 1) * 64])])
                scm = scmb[ji % 2]
                S.tt('dve', scm, sc, mask_b, ALU.mult)
                ops = S.psum(2 + ji % 2, [128, X, 64])
                kv = S.psum(4, [128, H, dv])
                for h in range(H):
                    for hf in range(DVH):
                        S.mm(ops[:, h * DVH + hf, :], [(vv[b][:, h, j, hf * 128:(hf + 1) * 128], scm[:, h, :]),
                                                       (Sbf[:, h, hf * 128:(hf + 1) * 128], qt[b][:, h, j * 64:(j + 1) * 64])])
                    S.mm(kv[:, h, :], [(kh[b][:, h, j, :], vv[b][:, h, j, :])])
                    S.stt(Sst[:, h, :], Sst[:, h, :], dec[b][:, h, j:j + 1], kv[:, h, :], ALU.mult, ALU.add)
                    S.cp('act', Sbf[:, h, :], Sst[:, h, :])
                S.cp('act', otile[:, :, j * 64:(j + 1) * 64], ops)
            OFv = OF.rearrange('(x p) t -> p x t', p=128)[:, :, t0:t0 + n]
            if _nopost:
                continue
            if d == 0:
                S.dma('sp', OFv, otile[:, :, 0:n], 'st_of')
            else:
                m2 = S.mark()
                sqb = [kht[0], kht[1]]
                for h in range(H):
                    r = k % 2
                    k += 1
                    for hf in range(DVH):
                        x = h * DVH + hf
                        oft = tc_[(r + hf) % 2][:, 0:n]
                        S.dma('sp', oft, OF[x * 128:(x + 1) * 128, t0:t0 + n], 'ld_of%d' % ((r + hf) % 2))
                        S.tt('pool', otile[:, x, 0:n], otile[:, x, 0:n], oft, ALU.add)
                    rstd = tq[r][:, 0:n]
                    ssq_rstd(S, [otile[:, h * DVH + hf, 0:n] for hf in range(DVH)], n, dv, ones_bf, sqb, rstd, 7)
                    for hf in range(DVH):
                        x = h * DVH + hf
                        gt = tk[(r + hf) % 2][:, 0:n]
                        gl = cfg['gbase'] + x * 128
                        S.dma('sp', gt, PF[gl:gl + 128, t0:t0 + n], 'ld_g%d' % ((r + hf) % 2))
                        S.stt(otile[:, x, 0:n], otile[:, x, 0:n], gcol[:, hf:hf + 1], rstd, ALU.mult, ALU.mult)
                        S.tt('dve', mixT[:, cfg['mixbase'] + x, t0:t0 + n], otile[:, x, 0:n], gt, ALU.mult)
                S.release(m2)
    S.release(m)


def stage_outproj(P, S, I, w_out, mixT, modv_l, xsrc, XR, tiles):
    m = S.mark()
    wb = [S.sb([128, 16, 512], BF16) for _ in range(2)]
    gl = [S.sb([128, 512], F32) for _ in range(2)]
    gc = [S.sb([128, 512], F32) for _ in range(2)]
    xt = [S.sb([128, 512], F32) for _ in range(3)]
    yt = [S.sb([128, 512], F32) for _ in range(3)]
    k = 0
    for cb in range(4):
        w = wb[cb % 2]
        S.dma('pool', w, kc_view(w_out, (cb * 512, (cb + 1) * 512)), 'ld_w%d' % (cb % 2))
        S.dma('sp', gl[cb % 2], modv_l[0:1, 2 * D + cb * 512: 2 * D + (cb + 1) * 512].to_broadcast([128, 512]), 'ld_bc')
        S.dma('sp', gc[cb % 2], modv_l[1:2, 2 * D + cb * 512: 2 * D + (cb + 1) * 512].to_broadcast([128, 512]), 'ld_bc')
        for ti in tiles:
            ps = S.psum(k % 4, [128, 512])
            S.mm(ps, [(mixT[:, c, ti * 128:(ti + 1) * 128], w[:, c, :]) for c in range(16)])
            x = xt[k % 3]
            y = yt[k % 3]
            S.dma('sp', x, xsrc[ti * 128:(ti + 1) * 128, cb * 512:(cb + 1) * 512], 'ld_xo%d' % (k % 3))
            gate = gc[cb % 2] if ti < CTX // 128 else gl[cb % 2]
            S.tt('dve', y, ps, gate, ALU.mult)
            S.tt('pool', y, y, x, ALU.add)
            S.dma('sp', XR[ti * 128:(ti + 1) * 128, cb * 512:(cb + 1) * 512], y, 'st_xo%d' % (k % 3))
            k += 1
    S.release(m)


def stage_moe(P, S, I, l, modv_l, XR, C, supers, final_out=None):
    ident = C['ident']
    ident_bf = C['ident_bf']
    w1 = I['moe_w1'][l]
    w3 = I['moe_w3'][l]
    w2 = I['moe_w2'][l]
    for tiles in supers:
        nsub = len(tiles)
        TS = nsub * 128
        m = S.mark()
        h2T = S.sb([128, 16, TS], BF16)
        yacc = S.sb([128, nsub, D], F32)
        comb = S.sb([128, nsub, 16], F32)
        m1 = S.mark()
        Gl = S.sb([128, D], F32)
        Sl = S.sb([128, D], F32)
        Gc = S.sb([128, D], F32)
        Sc = S.sb([128, D], F32)
        load_bc(S, Gl, modv_l, 0, 4, 'ld_bc')
        load_bc(S, Sl, modv_l, 0, 3, 'ld_bc')
        if any(t < CTX // 128 for t in tiles):
            load_bc(S, Gc, modv_l, 1, 4, 'ld_bc')
            load_bc(S, Sc, modv_l, 1, 3, 'ld_bc')
        xts = [S.sb([128, D], F32) for _ in range(2)]
        junk = S.sb([128, D], F32)
        hf = S.sb([128, D], F32)
        hb = S.sb([128, D], BF16)
        h32T = S.sb([128, 16, 128], F32)
        rw = S.sb([128, 16, 16], F32)
        S.dma('sp', rw, kc_view(I['router_w']), 'ld_small')
        rb = S.sb([128, 16], F32)
        S.dma('sp', rb, I['router_b'].to_broadcast([128, 16]), 'ld_small')
        sm = S.sb([128, 4], F32)
        sc_ = S.sb([128, 16], F32)
        sel = S.sb([128, 16], F32)
        t4 = [S.sb([128, 4], F32) for _ in range(8)]
        em = S.sb([128, 16], F32)
        for n, ti in enumerate(tiles):
            xt = xts[n % 2]
            S.dma('sp', xt, XR[ti * 128:(ti + 1) * 128, :], 'ld_x%d' % (n % 2))
            isctx = ti < CTX // 128
            G = Gc if isctx else Gl
            Sf = Sc if isctx else Sl
            ssq = sm[:, 0:1]
            S.act(junk, xt, AF.Square)
            S.op('dve', lambda e, ssq=ssq, junk=junk: e.tensor_reduce(out=ssq, in_=junk, axis=mybir.AxisListType.X, op=ALU.add), [junk], [ssq])
            S.ts('dve', ssq, ssq, 1.0 / D, ALU.mult, EPS, ALU.add)
            S.act(ssq, ssq, AF.Sqrt)
            S.recip(ssq, ssq)
            S.stt(junk, xt, ssq, G, ALU.mult, ALU.mult)
            S.tt('pool', hf, junk, Sf, ALU.add)
            S.cp('act', hb, hf)
            for half in range(2):
                ps = S.psum(6 + half, [128, 8, 128], BF16)
                for c in range(8):
                    S.tr(ps[:, c, :], hb[:, (half * 8 + c) * 128:(half * 8 + c + 1) * 128], ident_bf)
                S.cp('act' if half == 0 else 'dve', h2T[:, half * 8:half * 8 + 8, n * 128:(n + 1) * 128], ps)
            for q4 in range(4):
                ps = S.psum(q4 % 2, [128, 4, 128])
                for c in range(4):
                    cc = q4 * 4 + c
                    S.tr(ps[:, c, :], hf[:, cc * 128:(cc + 1) * 128], ident)
                S.cp('act' if q4 % 2 == 0 else 'dve', h32T[:, q4 * 4:q4 * 4 + 4, :], ps)
            lg = S.psum(2, [128, 16])
            S.mm(lg, [(h32T[:, c, :], rw[:, c, :]) for c in range(16)])
            S.act(sc_, lg, AF.Sigmoid)
            S.tt('dve', sel, sc_, rb, ALU.add)
            s4 = sel.rearrange('p (g e) -> p g e', e=4)
            a_, b_, c_, d_ = s4[:, :, 0], s4[:, :, 1], s4[:, :, 2], s4[:, :, 3]
            m1_, n1_, m2_, n2_, top1, xx, yy, sec = t4
            S.tt('dve', m1_, a_, b_, ALU.max)
            S.tt('dve', n1_, a_, b_, ALU.min)
            S.tt('dve', m2_, c_, d_, ALU.max)
            S.tt('dve', n2_, c_, d_, ALU.min)
            S.tt('dve', top1, m1_, m2_, ALU.max)
            S.tt('dve', xx, m1_, m2_, ALU.min)
            S.tt('dve', yy, n1_, n2_, ALU.max)
            S.tt('dve', sec, xx, yy, ALU.max)
            S.tt('dve', top1, top1, sec, ALU.add)
            gmax = sm[:, 1:2]
            S.op('dve', lambda e, gmax=gmax, top1=top1: e.tensor_reduce(out=gmax, in_=top1, axis=mybir.AxisListType.X, op=ALU.max), [top1], [gmax])
            S.ts('dve', xx, top1, gmax, ALU.is_ge)
            e4 = em.rearrange('p (g e) -> p g e', e=4)
            S.tt('dve', e4, s4, bc_mid(sec.unsqueeze(2), 4), ALU.is_ge)
            S.tt('dve', e4, e4, bc_mid(xx.unsqueeze(2), 4), ALU.mult)
            S.tt('dve', em, em, sc_, ALU.mult)
            den = sm[:, 2:3]
            S.op('dve', lambda e, den=den: e.tensor_reduce(out=den, in_=em, axis=mybir.AxisListType.X, op=ALU.add), [em], [den])
            S.recip(den, den)
            S.ts('dve', comb[:, n, :], em, den, ALU.mult)
        S.release(m1)
        m1 = S.mark()
        NBUF = 2
        w1b = [S.sb([128, 16, 256], BF16) for _ in range(NBUF)]
        w3b = [S.sb([128, 16, 256], BF16) for _ in range(NBUF)]
        w2b = [S.sb([128, 2, D], BF16) for _ in range(NBUF)]
        s1 = [S.sb([128, 512], F32) for _ in range(2)]
        hid = [S.sb([128, 2, 512], BF16) for _ in range(2)]
        ntiles = [(t0, min(512, TS - t0)) for t0 in range(0, TS, 512)]
        k = 0
        kq = 0
        for e in range(NE):
            for hfx in range(2):
                bsel = k % NBUF
                f0 = hfx * 256
                S.dma('pool', w1b[bsel], kc_view(w1[e], (f0, f0 + 256)), 'ld_w1_%d' % bsel)
                S.dma('pool', w3b[bsel], kc_view(w3[e], (f0, f0 + 256)), 'ld_w3_%d' % bsel)
                S.dma('pool', w2b[bsel], w2[e][f0:f0 + 256, :].rearrange('(c p) d -> p c d', p=128), 'ld_w2_%d' % bsel)
                first = (k == 0)
                k += 1
                for (t0, tn) in ntiles:
                    hd = hid[kq % 2]
                    for fc in range(2):
                        p1 = S.psum(0 + fc, [128, tn])
                        p3 = S.psum(2 + fc, [128, tn])
                        S.mm(p1, [(w1b[bsel][:, c, fc * 128:(fc + 1) * 128], h2T[:, c, t0:t0 + tn]) for c in range(16)])
                        S.mm(p3, [(w3b[bsel][:, c, fc * 128:(fc + 1) * 128], h2T[:, c, t0:t0 + tn]) for c in range(16)])
                        st = s1[fc][:, 0:tn]
                        S.act(st, p1, AF.Silu)
                        S.tt('dve', hd[:, fc, 0:tn], st, p3, ALU.mult)
                    kq += 1
                    for ts_ in range(tn // 128):
                        sub = t0 // 128 + ts_
                        for dc in range(4):
                            yp = S.psum(4 + (ts_ * 4 + dc) % 4, [128, 512])
                            S.mm(yp, [(hd[:, fc, ts_ * 128:(ts_ + 1) * 128], w2b[bsel][:, fc, dc * 512:(dc + 1) * 512]) for fc in range(2)])
                            ya = yacc[:, sub, dc * 512:(dc + 1) * 512]
                            if first:
                                S.ts('dve', ya, yp, comb[:, sub, e:e + 1], ALU.mult)
                            else:
                                S.stt(ya, yp, comb[:, sub, e:e + 1], ya, ALU.mult, ALU.add)
        S.release(m1)
        m1 = S.mark()
        g2l = S.sb([128, D], F32)
        g2c = S.sb([128, D], F32)
        load_bc(S, g2l, modv_l, 0, 5, 'ld_bc')
        if any(t < CTX // 128 for t in tiles):
            load_bc(S, g2c, modv_l, 1, 5, 'ld_bc')
        xts = [S.sb([128, D], F32) for _ in range(2)]
        if final_out is not None:
            fg = S.sb([128, D], F32)
            S.dma('sp', fg, I['final_norm_g'].unsqueeze(0).to_broadcast([128, D]), 'ld_bc')
            junk = S.sb([128, D], F32)
            sm = S.sb([128, 4], F32)
        for n, ti in enumerate(tiles):
            xt = xts[n % 2]
            S.dma('sp', xt, XR[ti * 128:(ti + 1) * 128, :], 'ld_x%d' % (n % 2))
            g2 = g2c if ti < CTX // 128 else g2l
            S.tt('dve', yacc[:, n, :], yacc[:, n, :], g2, ALU.mult)
            S.tt('pool', xt, xt, yacc[:, n, :], ALU.add)
            if final_out is None:
                S.dma('sp', XR[ti * 128:(ti + 1) * 128, :], xt, 'st_x%d' % (n % 2))
            else:
                ssq = sm[:, 0:1]
                S.act(junk, xt, AF.Square)
                S.op('dve', lambda e, ssq=ssq, junk=junk: e.tensor_reduce(out=ssq, in_=junk, axis=mybir.AxisListType.X, op=ALU.add), [junk], [ssq])
                S.ts('dve', ssq, ssq, 1.0 / D, ALU.mult, EPS, ALU.add)
                S.act(ssq, ssq, AF.Sqrt)
                S.recip(ssq, ssq)
                S.stt(xt, xt, ssq, fg, ALU.mult, ALU.mult)
                r0 = ti * 128 - CTX
                S.dma('sp', final_out[r0:r0 + 128, :], xt, 'st_x%d' % (n % 2))
        S.release(m1)
        S.release(m)


def build(dbg=(), stop_after=None, sbuf_kb=192):
    P = Prog(dbg)
    nc = P.nc
    I = {}
    I['xin'] = P.din('xin', [T, D])
    I['cT'] = P.din('cT', [128, 16, 2])
    I['mod_w'] = P.din('mod_w', [2, D, 6 * D])
    I['mod_b'] = P.din('mod_b', [2, 6 * D])
    I['norm_attn_g'] = P.din('norm_attn_g', [2, D])
    I['norm_ffn_g'] = P.din('norm_ffn_g', [2, D])
    I['final_norm_g'] = P.din('final_norm_g', [D])
    I['ab_w_in'] = P.din('ab_w_in', [D, 5952])
    I['ab_w_out'] = P.din('ab_w_out', [D, D])
    I['mla_w_uq'] = P.din('mla_w_uq', [512, 1536])
    I['mla_w_ukv'] = P.din('mla_w_ukv', [256, 2048])
    I['mla_q_gT'] = P.din('mla_q_gT', [128, 4])
    I['mla_kv_gT'] = P.din('mla_kv_gT', [128, 2])
    I['hgrn_gT'] = P.din('hgrn_gT', [128, 1])
    I['lbT'] = P.din('lbT', [128, 2, 3, 8])
    I['cosB'] = P.din('cosB', [64, LAT])
    I['sinB'] = P.din('sinB', [64, LAT])
    I['cd_w_in'] = P.din('cd_w_in', [D, 4640])
    I['cd_w_out'] = P.din('cd_w_out', [D, D])
    I['gqa_q_gT'] = P.din('gqa_q_gT', [128, 1])
    I['gqa_k_gT'] = P.din('gqa_k_gT', [128, 1])
    I['gla_w_a2'] = P.din('gla_w_a2', [2, 16, 512])
    I['gla_bT'] = P.din('gla_bT', [128, 2, 4])
    I['gla_gT'] = P.din('gla_gT', [128, 2])
    I['cosC'] = P.din('cosC', [128, LAT])
    I['sinC'] = P.din('sinC', [128, LAT])
    I['router_w'] = P.din('router_w', [D, 16])
    I['router_b'] = P.din('router_b', [1, 16])
    I['moe_w1'] = P.din('moe_w1', [2, NE, D, EDIM])
    I['moe_w3'] = P.din('moe_w3', [2, NE, D, EDIM])
    I['moe_w2'] = P.din('moe_w2', [2, NE, EDIM, D])
    I['consts'] = P.din('consts', [128, 1024])
    out = P.dout('out', [HALF, D])
    modv = [P.dscr('modv%d' % l, [2, 6 * D]) for l in range(2)]
    PF0 = P.dscr('PF0', [5952, T])
    PV0 = P.dscr('PV0', [T, 1024], BF16)
    OF0 = P.dscr('OF0', [1024, T])
    XR = P.dscr('XR', [T, D])
    PF1 = P.dscr('PF1', [4640, T])
    PV1 = P.dscr('PV1', [T, 1280], BF16)
    OF1 = P.dscr('OF1', [1024, T])

    with contextlib.ExitStack() as es:
        S = Sch(nc, es, sbuf_bytes=sbuf_kb * 1024)
        cst = S.sb([128, 1024], F32)
        S.dma('sp', cst, I['consts'], 'ld_small')
        C = {}
        C['ident'] = cst[:, 0:128]
        C['ident_bf'] = S.sb([128, 128], BF16)
        S.cp('dve', C['ident_bf'], cst[:, 0:128])
        C['perm128_bf'] = S.sb([128, 128], BF16)
        S.cp('dve', C['perm128_bf'], cst[:, 128:256])
        C['perm64_bf'] = S.sb([64, 64], BF16)
        S.cp('dve', C['perm64_bf'], cst[0:64, 256:320])
        C['maskf'] = cst[0:64, 320:384]
        C['maskb'] = cst[0:64, 384:448]
        C['rmask'] = cst[:, 512:1024]
        C['ones_bf'] = S.sb([128, 128], BF16)
        S.memset('dve', C['ones_bf'], 1.0)

        def dump(name, src_ap, shape, dt=F32):
            if name in P.dbg:
                d = P.dout(name, shape, dt)
                S.dma('sp', d, src_ap, 'st_small')

        def done():
            return stop_after is not None and stop_after in done.passed
        done.passed = set()

        def layer0():
            stage_mod(P, S, I, modv)
            m0 = S.mark()
            hT = S.sb([128, 16, T], BF16)
            stage_A(P, S, I, I['xin'], modv[0], 0, 1, hT, C['ident_bf'], [(i, i * 128) for i in range(NT)])
            segs = [(0, 1024, 'fm', AF.Silu, 0), (1024, 3072, 'fm', AF.Copy, 1024), (3072, 4096, 'tm', None, 0),
                    (4096, 5120, 'fm', AF.Sigmoid, 4096), (5120, 5952, 'fm', AF.Copy, 5120)]
            stage_inproj(P, S, I['ab_w_in'], 5952, hT, segs, PF0, PV0)
            S.release(m0)
            if stop_after == 'inproj0':
                return
            mixT = S.sb([128, 16, T], BF16)
            if 'skip_mla' not in P.dbg:
                stage_mla(P, S, I, PF0, mixT, C)
            if stop_after == 'mla':
                dump('mixT', mixT.rearrange('p c t -> p (c t)'), [128, 16 * T], BF16)
                return
            cfg = dict(H=8, DVH=1, kind='hgrn', qscale=128 ** -0.5, qbase=0, gbase=4096, mixbase=0, gcol=I['hgrn_gT'])
            stage_scan(P, S, I, cfg, PF0, PV0, OF0, mixT, C)
            dump('mixT', mixT.rearrange('p c t -> p (c t)'), [128, 16 * T], BF16)
            if stop_after == 'scan0':
                return
            stage_outproj(P, S, I, I['ab_w_out'], mixT, modv[0], I['xin'], XR, list(range(NT)))
            S.release(m0)
            dumpXR('XR_a')
            if stop_after == 'out0':
                return
            stage_moe(P, S, I, 0, modv[0], XR, C, [list(range(0, 9)), list(range(9, 18))])

        def dumpXR(name):
            if name in P.dbg:
                d = P.dout(name, [T, D])
                for ti in range(NT):
                    S.dma('sp', d[ti * 128:(ti + 1) * 128, :], XR[ti * 128:(ti + 1) * 128, :], 'st_small')

        def layer1():
            m0 = S.mark()
            hT = S.sb([128, 16, T], BF16)
            stage_A(P, S, I, XR, modv[1], 0, 1, hT, C['ident_bf'], [(i, i * 128) for i in range(NT)])
            segs = [(0, 1024, 'fm', AF.Copy, 0), (1024, 1280, 'fm', AF.Copy, 1024), (1280, 1536, 'tm', None, 0),
                    (1536, 2560, 'fm', AF.Copy, 1536), (2560, 3584, 'tm', None, 256),
                    (3584, 4608, 'fm', AF.Silu, 3584), (4608, 4640, 'fm', AF.Copy, 4608)]
            stage_inproj(P, S, I['cd_w_in'], 4640, hT, segs, PF1, PV1)
            S.release(m0)
            if stop_after == 'inproj1':
                return
            mixT = S.sb([128, 16, T], BF16)
            stage_gqa(P, S, I, PF1, PV1, mixT, C)
            if stop_after == 'gqa':
                return
            cfg = dict(H=4, DVH=2, kind='gla', qscale=128 ** -0.5, qbase=1536, kbase=2048, abase=4608, gbase=3584,
                       mixbase=8, vbase=256, gcol=I['gla_gT'])
            stage_scan(P, S, I, cfg, PF1, PV1, OF1, mixT, C)
            lat_tiles = list(range(CTX // 128, CTX // 128 + HALF // 128))
            if stop_after == 'scan1':
                return
            stage_outproj(P, S, I, I['cd_w_out'], mixT, modv[1], XR, XR, lat_tiles)
            S.release(m0)
            dumpXR('XR_c')
            if stop_after == 'out1':
                return
            stage_moe(P, S, I, 1, modv[1], XR, C, [lat_tiles], final_out=out)

        layer0()
        if stop_after is None or stop_after in ('inproj1', 'gqa', 'scan1', 'out1'):
            dumpXR('XR_b')
            layer1()
        if stop_after is not None:
            z = S.sb([128, D], F32)
            S.memset('dve', z, 0.0)
            S.dma('sp', out[0:128, :], z, 'st_small')
        S.finish()
        print("instructions:", S.n_inst, "sbuf top", S.sb_top, "dma sems", len(S.dma_sems), sorted(S.dma_sems))
    return P


def _rope_tables(d_rope):
    quarter = d_rope // 4
    half = d_rope // 2
    freqs = (np.float32(10000.0) ** (-np.arange(quarter, dtype=np.float32) / np.float32(quarter))).astype(np.float32)
    rows = LAT // 64
    row = np.repeat(np.arange(rows, dtype=np.float32), 64)
    col = np.tile(np.arange(64, dtype=np.float32), rows)
    ang = np.concatenate([row[:, None] * freqs, col[:, None] * freqs], axis=-1).astype(np.float32)
    c = np.cos(ang).astype(np.float32).T
    s_ = np.sin(ang).astype(np.float32).T
    return np.ascontiguousarray(np.concatenate([c, c], 0)), np.ascontiguousarray(np.concatenate([-s_, s_], 0))


def _consts():
    c = np.zeros((128, 1024), np.float32)
    c[:, 0:128] = np.eye(128, dtype=np.float32)
    k = np.arange(128)
    c[(k + 64) % 128, 128 + k] = 1.0
    k = np.arange(64)
    c[(k + 32) % 64, 256 + k] = 1.0
    c[0:64, 320:384] = np.triu(np.ones((64, 64), np.float32))
    c[0:64, 384:448] = np.tril(np.ones((64, 64), np.float32))
    c[:, 512:1024] = 1.0
    c[:, 512:1024:64] = 0.0
    return c


def host_inputs(inp, b, rev=False):
    f = lambda a: np.ascontiguousarray(np.asarray(a, np.float32))
    m = {}
    if rev:
        m['xin'] = f(np.concatenate([inp['ctx'][b][::-1], inp['x'][b][::-1]], 0))
    else:
        m['xin'] = f(np.concatenate([inp['ctx'][b], inp['x'][b]], 0))
    cc = np.stack([inp['c'][b], inp['c_ctx']], 1)
    m['cT'] = f(cc.reshape(16, 128, 2).transpose(1, 0, 2))
    for k_ in ['mod_w', 'mod_b', 'norm_attn_g', 'norm_ffn_g', 'final_norm_g', 'router_w', 'moe_w1', 'moe_w3', 'moe_w2']:
        m[k_] = f(inp[k_])
    m['ab_w_in'] = f(inp['ab_w_in'][0])
    m['ab_w_out'] = f(inp['ab_w_out'][0])
    m['mla_w_uq'] = f(inp['mla_w_uq'][0])
    m['mla_w_ukv'] = f(inp['mla_w_ukv'][0])
    m['mla_q_gT'] = f(inp['mla_q_norm_g'][0].reshape(4, 128).T)
    m['mla_kv_gT'] = f(inp['mla_kv_norm_g'][0].reshape(2, 128).T)
    m['hgrn_gT'] = f(inp['hgrn_norm_g'][0].reshape(1, 128).T)
    m['lbT'] = f(inp['hgrn_lb_logits'].reshape(2, 3, 8, 128).transpose(3, 0, 1, 2))
    m['cosB'], m['sinB'] = _rope_tables(64)
    m['cd_w_in'] = f(inp['cd_w_in'][0])
    m['cd_w_out'] = f(inp['cd_w_out'][0])
    m['gqa_q_gT'] = f(inp['gqa_q_norm_g'][0].reshape(1, 128).T)
    m['gqa_k_gT'] = f(inp['gqa_k_norm_g'][0].reshape(1, 128).T)
    m['gla_w_a2'] = f(inp['gla_w_a2'][0])
    m['gla_bT'] = f(inp['gla_b_a'][0].reshape(2, 4, 128).transpose(2, 0, 1))
    m['gla_gT'] = f(inp['gla_norm_g'][0].reshape(2, 128).T)
    m['cosC'], m['sinC'] = _rope_tables(128)
    m['router_b'] = f(inp['router_b'].reshape(1, 16))
    m['consts'] = _consts()
    if rev:
        w = m['ab_w_in'].copy()
        w[:, 1024:2048] = m['ab_w_in'][:, 2048:3072]
        w[:, 2048:3072] = m['ab_w_in'][:, 1024:2048]
        m['ab_w_in'] = w
        m['lbT'] = f(m['lbT'][:, ::-1])
        w = m['cd_w_in'].copy()
        w[:, 4608:4624] = m['cd_w_in'][:, 4624:4640]
        w[:, 4624:4640] = m['cd_w_in'][:, 4608:4624]
        m['cd_w_in'] = w
        m['gla_w_a2'] = f(m['gla_w_a2'][::-1])
        m['gla_bT'] = f(m['gla_bT'][:, ::-1])
        for k_ in ['cosB', 'sinB', 'cosC', 'sinC']:
            m[k_] = f(m[k_][:, ::-1])
    return m


_PROG = None
NCORES = 8


def kernel(**inputs):
    global _PROG
    if _PROG is None:
        _PROG = build()
    P = _PROG
    inp = {k: np.asarray(v) for k, v in inputs.items()}
    in_maps = []
    for c in range(NCORES):
        m = host_inputs(inp, c % 4, rev=(c >= 4))
        in_maps.append({k: v for k, v in m.items() if k in P.inputs})
    res = run_bass_kernel_spmd(P.nc, in_maps, core_ids=list(range(NCORES)))
    full = np.empty((4, LAT, D), np.float32)
    for b in range(4):
        full[b, 0:HALF] = np.asarray(res.results[b]['out'], np.float32)
        full[b, HALF:LAT] = np.asarray(res.results[b + 4]['out'], np.float32)[::-1]
    return full
```

```python
import contextlib
import numpy as np
import concourse.bass as bass
import concourse.mybir as mybir
from concourse.bass_utils import run_bass_kernel_spmd

F32 = mybir.dt.float32
BF16 = mybir.dt.bfloat16
AF = mybir.ActivationFunctionType
ALU = mybir.AluOpType
DT_SIZE = {F32: 4, BF16: 2}

D = 2048
CTX = 256
LAT = 2048
T = CTX + LAT
NT = T // 128
EPS = 1e-6
NE = 16
EDIM = 512
HALF = LAT // 2


def _box(ap):
    esz = DT_SIZE[ap.dtype]
    pat = ap.ap
    off = ap.offset
    name = ap.tensor.name
    if str(ap.space) == 'DRAM':
        hi = off + sum((c - 1) * s for s, c in pat) + 1
        return (name, 0, 1, off * esz, hi * esz)
    pstep, pcnt = pat[0]
    if pstep == 0:
        p0 = 0
        f0 = off
        pcnt = 128
    else:
        p0 = off // pstep
        f0 = off - p0 * pstep
    f1 = f0 + sum((c - 1) * s for s, c in pat[1:]) + 1
    return (name, p0, p0 + pcnt, f0 * esz, f1 * esz)


def _ov(a, b):
    return a[1] < b[2] and b[1] < a[2] and a[3] < b[4] and b[3] < a[4]


class Sch:
    ENG = ('pe', 'act', 'dve', 'pool', 'sp')

    def __init__(s, nc, es, sbuf_bytes=192 * 1024):
        s.nc = nc
        s.es = es
        s.eng = {'pe': nc.tensor, 'act': nc.scalar, 'dve': nc.vector, 'pool': nc.gpsimd, 'sp': nc.sync}
        s.sem = {e: es.enter_context(nc.semaphore('sem_' + e)) for e in s.ENG}
        s.cnt = {e: 0 for e in s.ENG}
        s.waited = {}
        s.regions = {}
        s.dma_sems = {}
        s.big = es.enter_context(nc.sbuf_tensor('SB', [128, sbuf_bytes // 4], F32))
        s.sb_top = 0
        s.sb_cap = sbuf_bytes
        s.ps = es.enter_context(nc.psum_tensor('PS', [128, 4096], F32))
        s.n_inst = 0
        s.dom_map = {}
        import os
        s.npool = int(os.environ.get('NPOOL', '1000'))
        s.waitall = os.environ.get('WAITALL', '1') == '1'
        s.serial = os.environ.get('SERIAL', '0') == '1'

    def mark(s):
        return s.sb_top

    def release(s, m):
        s.sb_top = m

    @staticmethod
    def _shape(v, shape):
        if len(shape) > 2:
            names = ' '.join('d%d' % i for i in range(1, len(shape)))
            v = v.rearrange('p (%s) -> p %s' % (names, names), **{'d%d' % i: shape[i] for i in range(2, len(shape))})
        return v

    def sb(s, shape, dt):
        n = int(np.prod(shape[1:]))
        nb = (n * DT_SIZE[dt] + 63) // 64 * 64
        assert s.sb_top + nb <= s.sb_cap, ('SBUF OOM', s.sb_top, nb)
        v = s.big[0:shape[0], s.sb_top // 4:(s.sb_top + nb) // 4]
        s.sb_top += nb
        if dt != F32:
            v = v.bitcast(dt)
        return s._shape(v[:, 0:n], shape)

    def psum(s, bank, shape, dt=F32, off=0):
        n = int(np.prod(shape[1:]))
        nb = n * DT_SIZE[dt]
        assert off % 4 == 0 and off + nb <= 2048 * (8 - bank)
        v = s.ps[0:shape[0], bank * 512 + off // 4: bank * 512 + (off + nb + 3) // 4]
        if dt != F32:
            v = v.bitcast(dt)
        return s._shape(v[:, 0:n], shape)

    def _deps(s, reads, writes, me):
        deps = {}
        rb = [_box(a) for a in reads]
        wb = [_box(a) for a in writes]
        for b in rb:
            for r in s.regions.get(b[0], ()):
                if _ov(r[0], b):
                    for dom, val in r[1].items():
                        if deps.get(dom, 0) < val:
                            deps[dom] = val
        for b in wb:
            for r in s.regions.get(b[0], ()):
                if _ov(r[0], b):
                    for dom, val in r[1].items():
                        if deps.get(dom, 0) < val:
                            deps[dom] = val
                    for dom, val in r[2].items():
                        if dom != me and deps.get(dom, 0) < val:
                            deps[dom] = val
        return deps, rb, wb

    def _record(s, rb, wb, dom, val):
        for b in rb:
            lst = s.regions.setdefault(b[0], [])
            found = None
            for r in lst:
                if r[0] == b:
                    found = r
                elif _ov(r[0], b) and not r[0][0].endswith('#old'):
                    r[2][dom] = val
            if found is None:
                w = {}
                for r in lst:
                    if _ov(r[0], b):
                        for d, v in r[1].items():
                            if w.get(d, 0) < v:
                                w[d] = v
                found = [b, w, {}]
                lst.append(found)
            found[2][dom] = val
        for b in wb:
            lst = s.regions.setdefault(b[0], [])
            found = None
            for r in lst:
                if r[0] == b:
                    found = r
                elif _ov(r[0], b):
                    q = r[0]
                    if b[1] <= q[1] and q[2] <= b[2] and b[3] <= q[3] and q[4] <= b[4]:
                        r[1] = {dom: val}
                        r[2] = {}
                    elif not q[0].endswith('#old'):
                        r[1][dom] = val
            if found is None:
                found = [b, {}, {}]
                lst.append(found)
            found[1] = {dom: val}
            found[2] = {}
        for b in rb + wb:
            lst = s.regions[b[0]]
            if len(lst) > 400:
                s._compact(b[0])

    def _compact(s, name):
        lst = s.regions[name]
        half = len(lst) // 2
        old = lst[:half]
        p0 = min(r[0][1] for r in old)
        p1 = max(r[0][2] for r in old)
        f0 = min(r[0][3] for r in old)
        f1 = max(r[0][4] for r in old)
        w = {}
        rd = {}
        for r in old:
            for d, v in r[1].items():
                if w.get(d, 0) < v:
                    w[d] = v
            for d, v in r[2].items():
                if rd.get(d, 0) < v:
                    rd[d] = v
        s.regions[name] = [[(name + '#old', p0, p1, f0, f1), w, rd]] + lst[half:]

    def _emit_waits(s, e, deps):
        eng = s.eng[e]
        if s.serial:
            deps = {d: v for d, v in s.cnt.items() if v > 0}
        for dom, val in deps.items():
            if dom == e and e == 'pe':
                continue
            if dom in s.dma_sems and s.waitall:
                val = s.cnt[dom]
            if s.waited.get((e, dom), 0) >= val:
                continue
            s.waited[(e, dom)] = val
            sem = s.sem[dom] if dom in s.sem else s.dma_sems[dom]
            eng.wait_ge(sem, val)
            s.n_inst += 1

    def op(s, e, fn, reads, writes):
        deps, rb, wb = s._deps(reads, writes, e)
        s._emit_waits(e, deps)
        ins = fn(s.eng[e])
        s.n_inst += 1
        s.cnt[e] += 1
        ins.then_inc(s.sem[e], 1)
        s._record(rb, wb, e, s.cnt[e])
        return ins

    def dma(s, e, out, in_, dom):
        if dom not in s.dom_map:
            s.dom_map[dom] = 'dq%d' % (len(s.dom_map) % s.npool)
        dom = s.dom_map[dom]
        if dom not in s.dma_sems:
            s.dma_sems[dom] = s.es.enter_context(s.nc.semaphore(dom))
            s.cnt[dom] = 0
        deps, rb, wb = s._deps([in_], [out], dom)
        if s.cnt[dom] > 0:
            deps[dom] = s.cnt[dom]
        s._emit_waits(e, deps)
        s.cnt[dom] += 16
        s.eng[e].dma_start(out=out, in_=in_).then_inc(s.dma_sems[dom], 16)
        s.n_inst += 1
        s._record(rb, wb, dom, s.cnt[dom])

    def finish(s):
        for dom in s.dma_sems:
            if s.cnt[dom] > 0:
                s.eng['sp'].wait_ge(s.dma_sems[dom], s.cnt[dom])

    def act(s, out, in_, func, bias=None, scale=None, accum_out=None, eng='act'):
        kw = {}
        rd = [in_]
        wr = [out]
        if bias is not None:
            kw['bias'] = bias
            if not isinstance(bias, (int, float)):
                rd.append(bias)
        if scale is not None:
            kw['scale'] = scale
            if not isinstance(scale, (int, float)):
                rd.append(scale)
        if accum_out is not None:
            kw['accum_out'] = accum_out
            wr.append(accum_out)
        return s.op('act', lambda e: e.activation(out=out, in_=in_, func=func, **kw), rd, wr)

    def tt(s, eng, out, in0, in1, op):
        return s.op(eng, lambda e: e.tensor_tensor(out=out, in0=in0, in1=in1, op=op), [in0, in1], [out])

    def ts(s, eng, out, in0, s1, op0, s2=None, op1=None):
        rd = [in0] + [x for x in (s1, s2) if x is not None and not isinstance(x, (int, float))]
        if op1 is None:
            return s.op(eng, lambda e: e.tensor_scalar(out=out, in0=in0, scalar1=s1, scalar2=None, op0=op0), rd, [out])
        return s.op(eng, lambda e: e.tensor_scalar(out=out, in0=in0, scalar1=s1, scalar2=s2, op0=op0, op1=op1), rd, [out])

    def stt(s, out, in0, sc, in1, op0, op1):
        rd = [in0, in1] + ([] if isinstance(sc, (int, float)) else [sc])
        return s.op('dve', lambda e: e.scalar_tensor_tensor(out=out, in0=in0, scalar=sc, in1=in1, op0=op0, op1=op1), rd, [out])

    def cp(s, eng, out, in_):
        if eng == 'act':
            return s.op('act', lambda e: e.copy(out=out, in_=in_), [in_], [out])
        return s.op(eng, lambda e: e.tensor_copy(out=out, in_=in_), [in_], [out])

    def recip(s, out, in_):
        return s.op('dve', lambda e: e.reciprocal(out=out, in_=in_), [in_], [out])

    def memset(s, eng, out, val):
        return s.op(eng, lambda e: e.memset(out, val), [], [out])

    def mm(s, out, pairs):
        n = len(pairs)

        def fn(e):
            ins = None
            for i, (l, r) in enumerate(pairs):
                ins = e.matmul(out, l, r, start=(i == 0), stop=(i == n - 1))
            return ins
        rd = []
        for l, r in pairs:
            rd.append(l)
            rd.append(r)
        s.n_inst += n - 1
        return s.op('pe', fn, rd, [out])

    def mm1(s, out, l, r, start, stop):
        return s.op('pe', lambda e: e.matmul(out, l, r, start=start, stop=stop), [l, r], [out])

    def tr(s, out, in_, ident):
        return s.op('pe', lambda e: e.transpose(out, in_, ident), [in_, ident], [out])


class Prog:
    def __init__(p, dbg=()):
        p.dbg = set(dbg)
        p.nc = bass.Bass("TRN2", target_bir_lowering=False)
        p.inputs = {}
        p.outputs = {}

    def din(p, name, shape, dt=F32):
        a = p.nc.dram_tensor(name, list(shape), dt, kind="ExternalInput").ap()
        p.inputs[name] = a
        return a

    def dscr(p, name, shape, dt=F32):
        kind = "ExternalOutput" if name in p.dbg else "Internal"
        a = p.nc.dram_tensor(name, list(shape), dt, kind=kind).ap()
        if name in p.dbg:
            p.outputs[name] = a
        return a

    def dout(p, name, shape, dt=F32):
        a = p.nc.dram_tensor(name, list(shape), dt, kind="ExternalOutput").ap()
        p.outputs[name] = a
        return a


def kc_view(w, cols=None):
    v = w.rearrange("(c p) f -> p c f", p=128)
    if cols is not None:
        v = v[:, :, cols[0]:cols[1]]
    return v


def stage_mod(P, S, I, modv):
    m = S.mark()
    cs32 = S.sb([128, 16, 2], F32)
    cs = S.sb([128, 16, 2], BF16)
    S.dma('sp', cs32, I['cT'], 'ld_small')
    S.act(cs, cs32, AF.Silu)
    msb = S.sb([2, 6 * D], F32)
    mb = S.sb([2, 6 * D], F32)
    gA = S.sb([2, D], F32)
    gF = S.sb([2, D], F32)
    wb = [S.sb([128, 16, 512], BF16) for _ in range(3)]
    k = 0
    for l in range(2):
        S.dma('sp', mb, I['mod_b'][l:l + 1, :].to_broadcast([2, 6 * D]), 'ld_small')
        S.dma('sp', gA, I['norm_attn_g'][l:l + 1, :].to_broadcast([2, D]), 'ld_small')
        S.dma('sp', gF, I['norm_ffn_g'][l:l + 1, :].to_broadcast([2, D]), 'ld_small')
        for j in range(24):
            w = wb[k % 3]
            S.dma('pool', w, kc_view(I['mod_w'][l], (j * 512, (j + 1) * 512)), 'ld_w%d' % (k % 3))
            ps = S.psum(k % 2, [2, 512])
            S.mm(ps, [(cs[:, c, :], w[:, c, :]) for c in range(16)])
            S.tt('dve', msb[:, j * 512:(j + 1) * 512], ps, mb[:, j * 512:(j + 1) * 512], ALU.add)
            k += 1
        S.stt(msb[:, D:2 * D], msb[:, D:2 * D], 1.0, gA, ALU.add, ALU.mult)
        S.stt(msb[:, 4 * D:5 * D], msb[:, 4 * D:5 * D], 1.0, gF, ALU.add, ALU.mult)
        S.dma('sp', modv[l], msb, 'st_small')
    S.release(m)


def load_bc(S, dst, modv_l, row, k, dom):
    S.dma('sp', dst, modv_l[row:row + 1, k * D:(k + 1) * D].to_broadcast([128, D]), dom)


def norm_mod_transpose(S, xt, G, Sf, hT_dst, ident_bf, junk, hb, small, ps_banks):
    ssq = small[:, 0:1]
    S.act(junk, xt, AF.Square)
    S.op('dve', lambda e, ssq=ssq, junk=junk: e.tensor_reduce(out=ssq, in_=junk, axis=mybir.AxisListType.X, op=ALU.add), [junk], [ssq])
    S.ts('dve', ssq, ssq, 1.0 / D, ALU.mult, EPS, ALU.add)
    S.act(ssq, ssq, AF.Sqrt)
    S.recip(ssq, ssq)
    S.stt(junk, xt, ssq, G, ALU.mult, ALU.mult)
    S.tt('pool', hb, junk, Sf, ALU.add)
    for half in range(2):
        ps = S.psum(ps_banks[half], [128, 8, 128], BF16)
        for c in range(8):
            S.tr(ps[:, c, :], hb[:, (half * 8 + c) * 128:(half * 8 + c + 1) * 128], ident_bf)
        if half == 0:
            S.cp('act', hT_dst[:, 0:8, :], ps)
        else:
            S.cp('dve', hT_dst[:, 8:16, :], ps)


def stage_A(P, S, I, xres, modv_l, kS, kG, hT, ident_bf, tiles, ps_banks=(6, 7)):
    m = S.mark()
    Gl = S.sb([128, D], F32)
    Sl = S.sb([128, D], F32)
    Gc = S.sb([128, D], F32)
    Sc = S.sb([128, D], F32)
    load_bc(S, Gl, modv_l, 0, kG, 'ld_bc')
    load_bc(S, Sl, modv_l, 0, kS, 'ld_bc')
    load_bc(S, Gc, modv_l, 1, kG, 'ld_bc')
    load_bc(S, Sc, modv_l, 1, kS, 'ld_bc')
    xts = [S.sb([128, D], F32) for _ in range(2)]
    junk = S.sb([128, D], F32)
    hb = S.sb([128, D], BF16)
    small = S.sb([128, 4], F32)
    for n, (ti, dc) in enumerate(tiles):
        xt = xts[n % 2]
        S.dma('sp', xt, xres[ti * 128:(ti + 1) * 128, :], 'ld_x%d' % (n % 2))
        isctx = ti < CTX // 128
        norm_mod_transpose(S, xt, Gc if isctx else Gl, Sc if isctx else Sl, hT[:, :, dc:dc + 128], ident_bf,
                           junk, hb, small, ps_banks)
    S.release(m)


def stage_inproj(P, S, w_in, F, hT, segs, PF, PV, wdom='ld_w'):
    m = S.mark()
    wb = [S.sb([128, 16, 512], BF16) for _ in range(3)]
    stg = [S.sb([128, 512], F32) for _ in range(3)]
    stgb = [S.sb([128, 512], BF16) for _ in range(2)]
    nblk = (F + 511) // 512
    k = 0
    kk = 0
    ttiles = [(t0, min(512, T - t0)) for t0 in range(0, T, 512)]
    for bi in range(nblk):
        c0 = bi * 512
        c1 = min(F, c0 + 512)
        w = wb[bi % 3]
        S.dma('pool', w[:, :, 0:c1 - c0], kc_view(w_in, (c0, c1)), '%s%d' % (wdom, bi % 3))
        for (lo, hi, kind, func, base) in segs:
            a = max(lo, c0)
            b = min(hi, c1)
            if a >= b:
                continue
            if kind == 'fm':
                for cc in range(a, b, 128):
                    ncol = min(128, b - cc)
                    for (t0, tn) in ttiles:
                        ps = S.psum(k % 4, [ncol, tn])
                        S.mm(ps, [(w[:, c, cc - c0:cc - c0 + ncol], hT[:, c, t0:t0 + tn]) for c in range(16)])
                        st = stg[k % 3]
                        if k % 2 == 0 or func != AF.Copy:
                            S.act(st[0:ncol, 0:tn], ps, func)
                        else:
                            S.cp('dve', st[0:ncol, 0:tn], ps)
                        S.dma('sp', PF[base + cc - lo: base + cc - lo + ncol, t0:t0 + tn], st[0:ncol, 0:tn], 'st_pf%d' % (k % 3))
                        k += 1
            else:
                wd = b - a
                for ti in range(NT):
                    ps = S.psum(4 + kk % 2, [128, wd])
                    S.mm(ps, [(hT[:, c, ti * 128:(ti + 1) * 128], w[:, c, a - c0:b - c0]) for c in range(16)])
                    st = stgb[kk % 2][:, 0:wd]
                    if kk % 2 == 0:
                        S.cp('act', st, ps)
                    else:
                        S.cp('dve', st, ps)
                    S.dma('sp', PV[ti * 128:(ti + 1) * 128, base + a - lo: base + b - lo], st, 'st_pv%d' % (kk % 2))
                    kk += 1
    S.release(m)


LT_TILES = [(0, 256)] + [(256 + 512 * i, 512) for i in range(4)]


def bc_mid(ap, n):
    return ap.to_broadcast([ap.shape[0], ap.shape[1], n])


def ssq_rstd(S, chunks, n, dim, ones_bf, sqb, rstd_out, bank):
    ps = S.psum(bank, [128, n])
    for i, ch in enumerate(chunks):
        R = ch.shape[0]
        sq = sqb[i % len(sqb)][0:R, 0:n]
        S.act(sq, ch, AF.Square)
        S.mm1(ps, ones_bf[0:R, :], sq, i == 0, i == len(chunks) - 1)
    S.ts('dve', rstd_out, ps, 1.0 / dim, ALU.mult, EPS, ALU.add)
    S.act(rstd_out, rstd_out, AF.Sqrt)
    S.recip(rstd_out, rstd_out)


def rope_fm(S, x32, R, n, cos, sin, perm_bf, out_bf, xb, tb, bank):
    S.cp('pool', xb[0:R, 0:n], x32)
    ps = S.psum(bank, [R, n])
    S.mm(ps, [(perm_bf[0:R, 0:R], xb[0:R, 0:n])])
    S.tt('pool', tb[0:R, 0:n], x32, cos, ALU.mult)
    S.tt('dve', x32, ps, sin, ALU.mult)
    S.tt('dve', out_bf, x32, tb[0:R, 0:n], ALU.add)


def attn_core(S, qparts, kparts, v, q0, nq, stiles, scale, dst, ones_bf, PTs, rden, par, cnt):
    oT = S.psum(2 + 2 * par, [128, nq])
    den = S.psum(3 + 2 * par, [128, nq])
    ns = len(stiles)
    for i, si in enumerate(stiles):
        sc = S.psum(cnt[0] % 2, [128, nq])
        S.mm(sc, [(kp[:, si * 128:(si + 1) * 128], qp[:, q0:q0 + nq]) for kp, qp in zip(kparts, qparts)])
        pt = PTs[cnt[0] % 2][:, 0:nq]
        cnt[0] += 1
        S.act(pt, sc, AF.Exp, scale=scale)
        S.mm1(oT, v[:, si, :], pt, i == 0, i == ns - 1)
        S.mm1(den, ones_bf, pt, i == 0, i == ns - 1)
    S.recip(rden[:, 0:nq], den)
    S.tt('dve', dst, oT, rden[:, 0:nq], ALU.mult)


def stage_mla(P, S, I, PF, mixT, C, need_ctx=True):
    import os
    ROT = 0 if os.environ.get('MLA_NOROPE') == '1' else CTX
    ROT = ROT if ROT else 10 ** 9
    m = S.mark()
    ones_bf = C['ones_bf']
    wuq = S.sb([128, 4, 1536], BF16)
    wukv = S.sb([128, 2, 2048], BF16)
    S.dma('pool', wuq, kc_view(I['mla_w_uq']), 'ld_w0')
    S.dma('pool', wukv, kc_view(I['mla_w_ukv']), 'ld_w1')
    gq = S.sb([128, 4], F32)
    gkv = S.sb([128, 2], F32)
    S.dma('sp', gq, I['mla_q_gT'], 'ld_small')
    S.dma('sp', gkv, I['mla_kv_gT'], 'ld_small')
    cosB = S.sb([64, LAT], F32)
    sinB = S.sb([64, LAT], F32)
    S.dma('sp', cosB, I['cosB'], 'ld_small')
    S.dma('sp', sinB, I['sinB'], 'ld_small')
    nqT = S.sb([128, 4, T], BF16)
    nkvT = S.sb([128, 2, T], BF16)
    krT = S.sb([64, T], BF16)
    sqb = [S.sb([128, 512], BF16) for _ in range(2)]
    rstd = S.sb([128, 512], F32)
    xb = S.sb([128, 512], BF16)
    tb = S.sb([128, 512], F32)
    x32 = S.sb([64, 512], F32)
    m1 = S.mark()
    p5 = S.sb([128, 4, 512], F32)
    p6 = S.sb([128, 2, 512], F32)
    p7 = S.sb([64, 512], F32)
    for (t0, n) in LT_TILES:
        S.dma('sp', p5[:, :, 0:n], PF[5120:5632, t0:t0 + n].rearrange('(c p) t -> p c t', p=128), 'ld_p5')
        S.dma('sp', p6[:, :, 0:n], PF[5632:5888, t0:t0 + n].rearrange('(c p) t -> p c t', p=128), 'ld_p6')
        S.dma('sp', p7[:, 0:n], PF[5888:5952, t0:t0 + n], 'ld_p7')
        ssq_rstd(S, [p5[:, c, 0:n] for c in range(4)], n, 512, ones_bf, sqb, rstd[:, 0:n], 6)
        for c in range(4):
            S.stt(nqT[:, c, t0:t0 + n], p5[:, c, 0:n], gq[:, c:c + 1], rstd[:, 0:n], ALU.mult, ALU.mult)
        ssq_rstd(S, [p6[:, c, 0:n] for c in range(2)], n, 256, ones_bf, sqb, rstd[:, 0:n], 7)
        for c in range(2):
            S.stt(nkvT[:, c, t0:t0 + n], p6[:, c, 0:n], gkv[:, c:c + 1], rstd[:, 0:n], ALU.mult, ALU.mult)
        if t0 >= ROT:
            l0 = t0 - CTX
            rope_fm(S, p7[:, 0:n], 64, n, cosB[:, l0:l0 + n], sinB[:, l0:l0 + n], C['perm64_bf'], krT[:, t0:t0 + n], xb, tb, 6)
        else:
            S.cp('pool', krT[:, t0:t0 + n], p7[:, 0:n])
    S.release(m1)
    hb = []
    for _ in range(1):
        hb.append((S.sb([128, T], BF16), S.sb([64, T], BF16), S.sb([128, T], BF16), S.sb([128, NT, 128], BF16)))
    PTs = [S.sb([128, 512], BF16) for _ in range(2)]
    rden = S.sb([128, 512], F32)
    cnt = [0]
    scale = float((128 + 64) ** -0.5)
    par = 0
    for h in range(8):
        qn, qr, kn, vh = hb[0]
        for (t0, n) in LT_TILES:
            ps = S.psum(6, [128, n])
            S.mm(ps, [(wuq[:, c, h * 192:h * 192 + 128], nqT[:, c, t0:t0 + n]) for c in range(4)])
            S.cp('act', qn[:, t0:t0 + n], ps)
            ps2 = S.psum(7, [64, n])
            S.mm(ps2, [(wuq[:, c, h * 192 + 128:h * 192 + 192], nqT[:, c, t0:t0 + n]) for c in range(4)])
            if t0 >= ROT:
                l0 = t0 - CTX
                S.cp('act', x32[:, 0:n], ps2)
                rope_fm(S, x32[:, 0:n], 64, n, cosB[:, l0:l0 + n], sinB[:, l0:l0 + n], C['perm64_bf'], qr[:, t0:t0 + n], xb, tb, 7)
            else:
                S.cp('act', qr[:, t0:t0 + n], ps2)
            ps = S.psum(6, [128, n])
            S.mm(ps, [(wukv[:, c, h * 256:h * 256 + 128], nkvT[:, c, t0:t0 + n]) for c in range(2)])
            S.cp('dve', kn[:, t0:t0 + n], ps)
            nb = n // 128
            psv = S.psum(7, [128, nb, 128])
            for j in range(nb):
                tk = t0 + j * 128
                S.mm(psv[:, j, :], [(nkvT[:, c, tk:tk + 128], wukv[:, c, h * 256 + 128:(h + 1) * 256]) for c in range(2)])
            S.cp('dve', vh[:, t0 // 128:t0 // 128 + nb, :], psv)
        if need_ctx:
            attn_core(S, [qn, qr], [kn, krT], vh, 0, 256, [0, 1], scale, mixT[:, 8 + h, 0:256], ones_bf, PTs, rden, par, cnt)
            par ^= 1
        for qi in range(4):
            q0 = CTX + 512 * qi
            attn_core(S, [qn, qr], [kn, krT], vh, q0, 512, list(range(NT)), scale, mixT[:, 8 + h, q0:q0 + 512], ones_bf, PTs, rden, par, cnt)
            par ^= 1
    S.release(m)


def stage_gqa(P, S, I, PF, PV, mixT, C):
    m = S.mark()
    ones_bf = C['ones_bf']
    gq = S.sb([128, 1], F32)
    gk = S.sb([128, 1], F32)
    S.dma('sp', gq, I['gqa_q_gT'], 'ld_small')
    S.dma('sp', gk, I['gqa_k_gT'], 'ld_small')
    cosC = S.sb([128, LAT], F32)
    sinC = S.sb([128, LAT], F32)
    S.dma('sp', cosC, I['cosC'], 'ld_small')
    S.dma('sp', sinC, I['sinC'], 'ld_small')
    kT = S.sb([128, T], BF16)
    qT = S.sb([128, T], BF16)
    vh = S.sb([128, NT, 128], BF16)
    sqb = [S.sb([128, 512], BF16) for _ in range(2)]
    rstd = S.sb([128, 512], F32)
    xb = S.sb([128, 512], BF16)
    tb = S.sb([128, 512], F32)
    x32 = S.sb([128, 512], F32)
    p = S.sb([128, 512], F32)
    PTs = [S.sb([128, 512], BF16) for _ in range(2)]
    rden = S.sb([128, 512], F32)
    cnt = [0]
    par = 0
    scale = float(128 ** -0.5)

    def prep(row0, gcol, dst, tiles):
        for (t0, n) in tiles:
            S.dma('sp', p[:, 0:n], PF[row0:row0 + 128, t0:t0 + n], 'ld_p5')
            ssq_rstd(S, [p[:, 0:n]], n, 128, ones_bf, sqb, rstd[:, 0:n], 6)
            if t0 >= CTX:
                l0 = t0 - CTX
                S.stt(x32[:, 0:n], p[:, 0:n], gcol, rstd[:, 0:n], ALU.mult, ALU.mult)
                rope_fm(S, x32[:, 0:n], 128, n, cosC[:, l0:l0 + n], sinC[:, l0:l0 + n], C['perm128_bf'], dst[:, t0:t0 + n], xb, tb, 7)
            else:
                S.stt(dst[:, t0:t0 + n], p[:, 0:n], gcol, rstd[:, 0:n], ALU.mult, ALU.mult)

    for g in range(2):
        prep(1024 + g * 128, gk[:, 0:1], kT, LT_TILES)
        S.dma('sp', vh, PV[:, g * 128:(g + 1) * 128].rearrange('(j p) v -> p j v', p=128), 'ld_v0')
        for i in range(4):
            h = g * 4 + i
            prep(h * 128, gq[:, 0:1], qT, LT_TILES[1:3])
            for qi in range(2):
                q0 = CTX + 512 * qi
                attn_core(S, [qT], [kT], vh, q0, 512, list(range(NT)), scale, mixT[:, h, q0:q0 + 512], ones_bf, PTs, rden, par, cnt)
                par ^= 1
    S.release(m)


def stage_scan(P, S, I, cfg, PF, PV, OF, mixT, C):
    H = cfg['H']
    DVH = cfg['DVH']
    dv = 128 * DVH
    X = H * DVH
    kind = cfg['kind']
    vbase = cfg.get('vbase', 0)
    m = S.mark()
    ident_bf = C['ident_bf']
    ones_bf = C['ones_bf']
    NB = 1
    qt = [S.sb([128, H, 512], BF16) for _ in range(NB)]
    kt = [S.sb([128, H, 512], BF16) for _ in range(NB)]
    kh = [S.sb([64, H, 8, 128], BF16) for _ in range(NB)]
    vv = [S.sb([64, H, 8, dv], BF16) for _ in range(NB)]
    dec = [S.sb([128, H, 8], F32) for _ in range(NB)]
    tq = [S.sb([128, 512], F32) for _ in range(2)]
    tk = [S.sb([128, 512], F32) for _ in range(2)]
    tg = [S.sb([128, 512], F32) for _ in range(2)]
    tc_ = [S.sb([128, 512], F32) for _ in range(2)]
    ta = [S.sb([128, 512], F32) for _ in range(2)]
    te = [S.sb([128, 512], F32) for _ in range(2)]
    kht = [S.sb([128, 512], BF16) for _ in range(2)]
    Sst = S.sb([128, H, dv], F32)
    Sbf = S.sb([128, H, dv], BF16)
    otile = S.sb([128, X, 512], F32)
    scmb = [S.sb([64, H, 64], BF16) for _ in range(2)]
    gcol = S.sb([128, DVH], F32)
    S.dma('sp', gcol, cfg['gcol'], 'ld_small')
    if kind == 'hgrn':
        lbt = S.sb([128, 2, 3, 8], F32)
        S.dma('sp', lbt, I['lbT'], 'ld_small')
        S.act(lbt, lbt, AF.Exp)
        lsum = S.sb([128, 2, 8], F32)
        S.tt('dve', lsum, lbt[:, :, 0, :], lbt[:, :, 1, :], ALU.add)
        S.tt('dve', lsum, lsum, lbt[:, :, 2, :], ALU.add)
        S.recip(lsum, lsum)
        lb = S.sb([128, 2, 8], F32)
        oml = S.sb([128, 2, 8], F32)
        S.tt('dve', lb, lbt[:, :, 0, :], lsum, ALU.mult)
        S.ts('dve', oml, lb, -1.0, ALU.mult, 1.0, ALU.add)
    else:
        wa2 = S.sb([16, 2, 512], BF16)
        S.dma('pool', wa2, I['gla_w_a2'].rearrange('d r f -> r d f'), 'ld_w0')
        negb = S.sb([128, 2, 4], F32)
        S.dma('sp', negb, I['gla_bT'], 'ld_small')
        S.ts('dve', negb, negb, -1.0, ALU.mult)
        a32 = S.sb([16, 512], F32)
        abf = S.sb([16, 512], BF16)
    import os
    _dirs = [int(x) for x in os.environ.get('SCAN_DIRS', '0,1').split(',')]
    _nt = int(os.environ.get('SCAN_TILES', '5'))
    _noch = os.environ.get('SCAN_NOCHUNK') == '1'
    _nopost = os.environ.get('SCAN_NOPOST') == '1'
    k = 0
    for d in _dirs:
        order = LT_TILES if d == 0 else [LT_TILES[0]] + LT_TILES[:0:-1]
        mask = C['maskf'] if d == 0 else C['maskb']
        mask_b = mask.unsqueeze(1).to_broadcast([64, H, 64])
        S.memset('dve', Sst, 0.0)
        S.memset('pool', Sbf, 0.0)
        for ti_, (t0, n) in enumerate(order[:_nt]):
            nch = n // 64
            b = ti_ % NB
            if kind == 'gla':
                S.dma('sp', a32[:, 0:n], PF[cfg['abase'] + 16 * d: cfg['abase'] + 16 * d + 16, t0:t0 + n], 'ld_a')
                S.cp('act', abf[:, 0:n], a32[:, 0:n])
            for h in range(H):
                r = k % 2
                k += 1
                q = tq[r][:, 0:n]
                kk = tk[r][:, 0:n]
                g = tg[r][:, 0:n]
                c = tc_[r][:, 0:n]
                a = ta[r][:, 0:n]
                e_ = te[r][:, 0:n]
                S.dma('sp', q, PF[cfg['qbase'] + h * 128: cfg['qbase'] + (h + 1) * 128, t0:t0 + n], 'ld_q%d' % r)
                if kind == 'hgrn':
                    lo = 1024 * (1 + d) + h * 128
                    S.dma('sp', kk, PF[lo:lo + 128, t0:t0 + n], 'ld_k%d' % r)
                    S.act(kk, kk, AF.Sigmoid)
                    S.ts('dve', kk, kk, oml[:, d, h:h + 1], ALU.mult, lb[:, d, h:h + 1], ALU.add)
                    S.act(g, kk, AF.Ln)
                    S.ts('pool', kk, kk, -1.0, ALU.mult, 1.0, ALU.add)
                else:
                    S.dma('sp', kk, PF[cfg['kbase'] + h * 128: cfg['kbase'] + (h + 1) * 128, t0:t0 + n], 'ld_k%d' % r)
                    zp = S.psum(7, [128, n])
                    S.mm(zp, [(wa2[:, d, h * 128:(h + 1) * 128], abf[:, 0:n])])
                    S.act(e_, zp, AF.Exp, scale=-1.0, bias=negb[:, d, h:h + 1])
                    S.act(g, e_, AF.Ln, bias=1.0)
                    S.ts('dve', g, g, -1.0 / 16.0, ALU.mult)
                S.op('dve', lambda e, c=c, g=g, n=n: e.tensor_tensor_scan(out=c, data0=C['rmask'][:, 0:n], data1=g, initial=0.0,
                                                                            op0=ALU.mult, op1=ALU.add), [C['rmask'][:, 0:n], g], [c])
                c3 = c.rearrange('p (j s) -> p j s', s=64)
                tot = c3[:, :, 63:64]
                if d == 1:
                    S.tt('dve', a, g, c, ALU.subtract)
                    S.tt('dve', a.rearrange('p (j s) -> p j s', s=64), a.rearrange('p (j s) -> p j s', s=64), bc_mid(tot, 64), ALU.add)
                else:
                    a = c
                S.act(e_, a, AF.Exp)
                S.stt(qt[b][:, h, 0:n], q, float(cfg['qscale']), e_, ALU.mult, ALU.mult)
                S.act(e_, a, AF.Exp, scale=-1.0)
                S.tt('dve', kt[b][:, h, 0:n], kk, e_, ALU.mult)
                S.tt('dve', g.rearrange('p (j s) -> p j s', s=64), bc_mid(tot, 64), a.rearrange('p (j s) -> p j s', s=64), ALU.subtract)
                S.act(g, g, AF.Exp)
                S.tt('pool', kht[r][:, 0:n], kk, g, ALU.mult)
                S.act(dec[b][:, h, 0:nch], c3[:, :, 63], AF.Exp)
                psT = S.psum(6, [64, 8, 128], BF16)
                for j in range(nch):
                    S.tr(psT[:, j, :], kht[r][:, j * 64:(j + 1) * 64], ident_bf)
                S.cp('act', kh[b][:, h, 0:nch, :], psT[:, 0:nch, :])
                S.dma('sp', vv[b][:, h, 0:nch, :], PV[t0:t0 + n, vbase + h * dv:vbase + (h + 1) * dv].rearrange('(j p) v -> p j v', p=64), 'ld_v%d' % b)
            chs = list(range(nch)) if d == 0 else list(range(nch - 1, -1, -1))
            if _noch:
                chs = []
            for ji, j in enumerate(chs):
                sc = S.psum(ji % 2, [64, H, 64])
                for h in range(H):
                    S.mm(sc[:, h, :], [(kt[b][:, h, j * 64:(j + 1) * 64], qt[b][:, h, j * 64:(j + 1) * 64])])
                scm = scmb[ji % 2]
                S.tt('dve', scm, sc, mask_b, ALU.mult)
                ops = S.psum(2 + ji % 2, [128, X, 64])
                kv = S.psum(4, [128, H, dv])
                for h in range(H):
                    for hf in range(DVH):
                        S.mm(ops[:, h * DVH + hf, :], [(vv[b][:, h, j, hf * 128:(hf + 1) * 128], scm[:, h, :]),
                                                       (Sbf[:, h, hf * 128:(hf + 1) * 128], qt[b][:, h, j * 64:(j + 1) * 64])])
                    S.mm(kv[:, h, :], [(kh[b][:, h, j, :], vv[b][:, h, j, :])])
                    S.stt(Sst[:, h, :], Sst[:, h, :], dec[b][:, h, j:j + 1], kv[:, h, :], ALU.mult, ALU.add)
                    S.cp('act', Sbf[:, h, :], Sst[:, h, :])
                S.cp('act', otile[:, :, j * 64:(j + 1) * 64], ops)
            OFv = OF.rearrange('(x p) t -> p x t', p=128)[:, :, t0:t0 + n]
            if _nopost:
                continue
            if d == 0:
                S.dma('sp', OFv, otile[:, :, 0:n], 'st_of')
            else:
                m2 = S.mark()
                sqb = [kht[0], kht[1]]
                for h in range(H):
                    r = k % 2
                    k += 1
                    for hf in range(DVH):
                        x = h * DVH + hf
                        oft = tc_[(r + hf) % 2][:, 0:n]
                        S.dma('sp', oft, OF[x * 128:(x + 1) * 128, t0:t0 + n], 'ld_of%d' % ((r + hf) % 2))
                        S.tt('pool', otile[:, x, 0:n], otile[:, x, 0:n], oft, ALU.add)
                    rstd = tq[r][:, 0:n]
                    ssq_rstd(S, [otile[:, h * DVH + hf, 0:n] for hf in range(DVH)], n, dv, ones_bf, sqb, rstd, 7)
                    for hf in range(DVH):
                        x = h * DVH + hf
                        gt = tk[(r + hf) % 2][:, 0:n]
                        gl = cfg['gbase'] + x * 128
                        S.dma('sp', gt, PF[gl:gl + 128, t0:t0 + n], 'ld_g%d' % ((r + hf) % 2))
                        S.stt(otile[:, x, 0:n], otile[:, x, 0:n], gcol[:, hf:hf + 1], rstd, ALU.mult, ALU.mult)
                        S.tt('dve', mixT[:, cfg['mixbase'] + x, t0:t0 + n], otile[:, x, 0:n], gt, ALU.mult)
                S.release(m2)
    S.release(m)


def stage_outproj(P, S, I, w_out, mixT, modv_l, xsrc, XR, tiles):
    m = S.mark()
    wb = [S.sb([128, 16, 512], BF16) for _ in range(2)]
    gl = [S.sb([128, 512], F32) for _ in range(2)]
    gc = [S.sb([128, 512], F32) for _ in range(2)]
    xt = [S.sb([128, 512], F32) for _ in range(3)]
    yt = [S.sb([128, 512], F32) for _ in range(3)]
    k = 0
    for cb in range(4):
        w = wb[cb % 2]
        S.dma('pool', w, kc_view(w_out, (cb * 512, (cb + 1) * 512)), 'ld_w%d' % (cb % 2))
        S.dma('sp', gl[cb % 2], modv_l[0:1, 2 * D + cb * 512: 2 * D + (cb + 1) * 512].to_broadcast([128, 512]), 'ld_bc')
        S.dma('sp', gc[cb % 2], modv_l[1:2, 2 * D + cb * 512: 2 * D + (cb + 1) * 512].to_broadcast([128, 512]), 'ld_bc')
        for ti in tiles:
            ps = S.psum(k % 4, [128, 512])
            S.mm(ps, [(mixT[:, c, ti * 128:(ti + 1) * 128], w[:, c, :]) for c in range(16)])
            x = xt[k % 3]
            y = yt[k % 3]
            S.dma('sp', x, xsrc[ti * 128:(ti + 1) * 128, cb * 512:(cb + 1) * 512], 'ld_xo%d' % (k % 3))
            gate = gc[cb % 2] if ti < CTX // 128 else gl[cb % 2]
            S.tt('dve', y, ps, gate, ALU.mult)
            S.tt('pool', y, y, x, ALU.add)
            S.dma('sp', XR[ti * 128:(ti + 1) * 128, cb * 512:(cb + 1) * 512], y, 'st_xo%d' % (k % 3))
            k += 1
    S.release(m)


def stage_moe(P, S, I, l, modv_l, XR, C, supers, final_out=None):
    ident = C['ident']
    ident_bf = C['ident_bf']
    w1 = I['moe_w1'][l]
    w3 = I['moe_w3'][l]
    w2 = I['moe_w2'][l]
    for tiles in supers:
        nsub = len(tiles)
        TS = nsub * 128
        m = S.mark()
        h2T = S.sb([128, 16, TS], BF16)
        yacc = S.sb([128, nsub, D], F32)
        comb = S.sb([128, nsub, 16], F32)
        m1 = S.mark()
        Gl = S.sb([128, D], F32)
        Sl = S.sb([128, D], F32)
        Gc = S.sb([128, D], F32)
        Sc = S.sb([128, D], F32)
        load_bc(S, Gl, modv_l, 0, 4, 'ld_bc')
        load_bc(S, Sl, modv_l, 0, 3, 'ld_bc')
        if any(t < CTX // 128 for t in tiles):
            load_bc(S, Gc, modv_l, 1, 4, 'ld_bc')
            load_bc(S, Sc, modv_l, 1, 3, 'ld_bc')
        xts = [S.sb([128, D], F32) for _ in range(2)]
        junk = S.sb([128, D], F32)
        hf = S.sb([128, D], F32)
        hb = S.sb([128, D], BF16)
        h32T = S.sb([128, 16, 128], F32)
        rw = S.sb([128, 16, 16], F32)
        S.dma('sp', rw, kc_view(I['router_w']), 'ld_small')
        rb = S.sb([128, 16], F32)
        S.dma('sp', rb, I['router_b'].to_broadcast([128, 16]), 'ld_small')
        sm = S.sb([128, 4], F32)
        sc_ = S.sb([128, 16], F32)
        sel = S.sb([128, 16], F32)
        t4 = [S.sb([128, 4], F32) for _ in range(8)]
        em = S.sb([128, 16], F32)
        for n, ti in enumerate(tiles):
            xt = xts[n % 2]
            S.dma('sp', xt, XR[ti * 128:(ti + 1) * 128, :], 'ld_x%d' % (n % 2))
            isctx = ti < CTX // 128
            G = Gc if isctx else Gl
            Sf = Sc if isctx else Sl
            ssq = sm[:, 0:1]
            S.act(junk, xt, AF.Square)
            S.op('dve', lambda e, ssq=ssq, junk=junk: e.tensor_reduce(out=ssq, in_=junk, axis=mybir.AxisListType.X, op=ALU.add), [junk], [ssq])
            S.ts('dve', ssq, ssq, 1.0 / D, ALU.mult, EPS, ALU.add)
            S.act(ssq, ssq, AF.Sqrt)
            S.recip(ssq, ssq)
            S.stt(junk, xt, ssq, G, ALU.mult, ALU.mult)
            S.tt('pool', hf, junk, Sf, ALU.add)
            S.cp('act', hb, hf)
            for half in range(2):
                ps = S.psum(6 + half, [128, 8, 128], BF16)
                for c in range(8):
                    S.tr(ps[:, c, :], hb[:, (half * 8 + c) * 128:(half * 8 + c + 1) * 128], ident_bf)
                S.cp('act' if half == 0 else 'dve', h2T[:, half * 8:half * 8 + 8, n * 128:(n + 1) * 128], ps)
            for q4 in range(4):
                ps = S.psum(q4 % 2, [128, 4, 128])
                for c in range(4):
                    cc = q4 * 4 + c
                    S.tr(ps[:, c, :], hf[:, cc * 128:(cc + 1) * 128], ident)
                S.cp('act' if q4 % 2 == 0 else 'dve', h32T[:, q4 * 4:q4 * 4 + 4, :], ps)
            lg = S.psum(2, [128, 16])
            S.mm(lg, [(h32T[:, c, :], rw[:, c, :]) for c in range(16)])
            S.act(sc_, lg, AF.Sigmoid)
            S.tt('dve', sel, sc_, rb, ALU.add)
            s4 = sel.rearrange('p (g e) -> p g e', e=4)
            a_, b_, c_, d_ = s4[:, :, 0], s4[:, :, 1], s4[:, :, 2], s4[:, :, 3]
            m1_, n1_, m2_, n2_, top1, xx, yy, sec = t4
            S.tt('dve', m1_, a_, b_, ALU.max)
            S.tt('dve', n1_, a_, b_, ALU.min)
            S.tt('dve', m2_, c_, d_, ALU.max)
            S.tt('dve', n2_, c_, d_, ALU.min)
            S.tt('dve', top1, m1_, m2_, ALU.max)
            S.tt('dve', xx, m1_, m2_, ALU.min)
            S.tt('dve', yy, n1_, n2_, ALU.max)
            S.tt('dve', sec, xx, yy, ALU.max)
            S.tt('dve', top1, top1, sec, ALU.add)
            gmax = sm[:, 1:2]
            S.op('dve', lambda e, gmax=gmax, top1=top1: e.tensor_reduce(out=gmax, in_=top1, axis=mybir.AxisListType.X, op=ALU.max), [top1], [gmax])
            S.ts('dve', xx, top1, gmax, ALU.is_ge)
            e4 = em.rearrange('p (g e) -> p g e', e=4)
            S.tt('dve', e4, s4, bc_mid(sec.unsqueeze(2), 4), ALU.is_ge)
            S.tt('dve', e4, e4, bc_mid(xx.unsqueeze(2), 4), ALU.mult)
            S.tt('dve', em, em, sc_, ALU.mult)
            den = sm[:, 2:3]
            S.op('dve', lambda e, den=den: e.tensor_reduce(out=den, in_=em, axis=mybir.AxisListType.X, op=ALU.add), [em], [den])
            S.recip(den, den)
            S.ts('dve', comb[:, n, :], em, den, ALU.mult)
        S.release(m1)
        m1 = S.mark()
        NBUF = 2
        w1b = [S.sb([128, 16, 256], BF16) for _ in range(NBUF)]
        w3b = [S.sb([128, 16, 256], BF16) for _ in range(NBUF)]
        w2b = [S.sb([128, 2, D], BF16) for _ in range(NBUF)]
        s1 = [S.sb([128, 512], F32) for _ in range(2)]
        hid = [S.sb([128, 2, 512], BF16) for _ in range(2)]
        ntiles = [(t0, min(512, TS - t0)) for t0 in range(0, TS, 512)]
        k = 0
        kq = 0
        for e in range(NE):
            for hfx in range(2):
                bsel = k % NBUF
                f0 = hfx * 256
                S.dma('pool', w1b[bsel], kc_view(w1[e], (f0, f0 + 256)), 'ld_w1_%d' % bsel)
                S.dma('pool', w3b[bsel], kc_view(w3[e], (f0, f0 + 256)), 'ld_w3_%d' % bsel)
                S.dma('pool', w2b[bsel], w2[e][f0:f0 + 256, :].rearrange('(c p) d -> p c d', p=128), 'ld_w2_%d' % bsel)
                first = (k == 0)
                k += 1
                for (t0, tn) in ntiles:
                    hd = hid[kq % 2]
                    for fc in range(2):
                        p1 = S.psum(0 + fc, [128, tn])
                        p3 = S.psum(2 + fc, [128, tn])
                        S.mm(p1, [(w1b[bsel][:, c, fc * 128:(fc + 1) * 128], h2T[:, c, t0:t0 + tn]) for c in range(16)])
                        S.mm(p3, [(w3b[bsel][:, c, fc * 128:(fc + 1) * 128], h2T[:, c, t0:t0 + tn]) for c in range(16)])
                        st = s1[fc][:, 0:tn]
                        S.act(st, p1, AF.Silu)
                        S.tt('dve', hd[:, fc, 0:tn], st, p3, ALU.mult)
                    kq += 1
                    for ts_ in range(tn // 128):
                        sub = t0 // 128 + ts_
                        for dc in range(4):
                            yp = S.psum(4 + (ts_ * 4 + dc) % 4, [128, 512])
                            S.mm(yp, [(hd[:, fc, ts_ * 128:(ts_ + 1) * 128], w2b[bsel][:, fc, dc * 512:(dc + 1) * 512]) for fc in range(2)])
                            ya = yacc[:, sub, dc * 512:(dc + 1) * 512]
                            if first:
                                S.ts('dve', ya, yp, comb[:, sub, e:e + 1], ALU.mult)
                            else:
                                S.stt(ya, yp, comb[:, sub, e:e + 1], ya, ALU.mult, ALU.add)
        S.release(m1)
        m1 = S.mark()
        g2l = S.sb([128, D], F32)
        g2c = S.sb([128, D], F32)
        load_bc(S, g2l, modv_l, 0, 5, 'ld_bc')
        if any(t < CTX // 128 for t in tiles):
            load_bc(S, g2c, modv_l, 1, 5, 'ld_bc')
        xts = [S.sb([128, D], F32) for _ in range(2)]
        if final_out is not None:
            fg = S.sb([128, D], F32)
            S.dma('sp', fg, I['final_norm_g'].unsqueeze(0).to_broadcast([128, D]), 'ld_bc')
            junk = S.sb([128, D], F32)
            sm = S.sb([128, 4], F32)
        for n, ti in enumerate(tiles):
            xt = xts[n % 2]
            S.dma('sp', xt, XR[ti * 128:(ti + 1) * 128, :], 'ld_x%d' % (n % 2))
            g2 = g2c if ti < CTX // 128 else g2l
            S.tt('dve', yacc[:, n, :], yacc[:, n, :], g2, ALU.mult)
            S.tt('pool', xt, xt, yacc[:, n, :], ALU.add)
            if final_out is None:
                S.dma('sp', XR[ti * 128:(ti + 1) * 128, :], xt, 'st_x%d' % (n % 2))
            else:
                ssq = sm[:, 0:1]
                S.act(junk, xt, AF.Square)
                S.op('dve', lambda e, ssq=ssq, junk=junk: e.tensor_reduce(out=ssq, in_=junk, axis=mybir.AxisListType.X, op=ALU.add), [junk], [ssq])
                S.ts('dve', ssq, ssq, 1.0 / D, ALU.mult, EPS, ALU.add)
                S.act(ssq, ssq, AF.Sqrt)
                S.recip(ssq, ssq)
                S.stt(xt, xt, ssq, fg, ALU.mult, ALU.mult)
                r0 = ti * 128 - CTX
                S.dma('sp', final_out[r0:r0 + 128, :], xt, 'st_x%d' % (n % 2))
        S.release(m1)
        S.release(m)


def build(dbg=(), stop_after=None, sbuf_kb=192):
    P = Prog(dbg)
    nc = P.nc
    I = {}
    I['xin'] = P.din('xin', [T, D])
    I['cT'] = P.din('cT', [128, 16, 2])
    I['mod_w'] = P.din('mod_w', [2, D, 6 * D])
    I['mod_b'] = P.din('mod_b', [2, 6 * D])
    I['norm_attn_g'] = P.din('norm_attn_g', [2, D])
    I['norm_ffn_g'] = P.din('norm_ffn_g', [2, D])
    I['final_norm_g'] = P.din('final_norm_g', [D])
    I['ab_w_in'] = P.din('ab_w_in', [D, 5952])
    I['ab_w_out'] = P.din('ab_w_out', [D, D])
    I['mla_w_uq'] = P.din('mla_w_uq', [512, 1536])
    I['mla_w_ukv'] = P.din('mla_w_ukv', [256, 2048])
    I['mla_q_gT'] = P.din('mla_q_gT', [128, 4])
    I['mla_kv_gT'] = P.din('mla_kv_gT', [128, 2])
    I['hgrn_gT'] = P.din('hgrn_gT', [128, 1])
    I['lbT'] = P.din('lbT', [128, 2, 3, 8])
    I['cosB'] = P.din('cosB', [64, LAT])
    I['sinB'] = P.din('sinB', [64, LAT])
    I['cd_w_in'] = P.din('cd_w_in', [D, 4640])
    I['cd_w_out'] = P.din('cd_w_out', [D, D])
    I['gqa_q_gT'] = P.din('gqa_q_gT', [128, 1])
    I['gqa_k_gT'] = P.din('gqa_k_gT', [128, 1])
    I['gla_w_a2'] = P.din('gla_w_a2', [2, 16, 512])
    I['gla_bT'] = P.din('gla_bT', [128, 2, 4])
    I['gla_gT'] = P.din('gla_gT', [128, 2])
    I['cosC'] = P.din('cosC', [128, LAT])
    I['sinC'] = P.din('sinC', [128, LAT])
    I['router_w'] = P.din('router_w', [D, 16])
    I['router_b'] = P.din('router_b', [1, 16])
    I['moe_w1'] = P.din('moe_w1', [2, NE, D, EDIM])
    I['moe_w3'] = P.din('moe_w3', [2, NE, D, EDIM])
    I['moe_w2'] = P.din('moe_w2', [2, NE, EDIM, D])
    I['consts'] = P.din('consts', [128, 1024])
    out = P.dout('out', [HALF, D])
    modv = [P.dscr('modv%d' % l, [2, 6 * D]) for l in range(2)]
    PF0 = P.dscr('PF0', [5952, T])
    PV0 = P.dscr('PV0', [T, 1024], BF16)
    OF0 = P.dscr('OF0', [1024, T])
    XR = P.dscr('XR', [T, D])
    PF1 = P.dscr('PF1', [4640, T])
    PV1 = P.dscr('PV1', [T, 1280], BF16)
    OF1 = P.dscr('OF1', [1024, T])

    with contextlib.ExitStack() as es:
        S = Sch(nc, es, sbuf_bytes=sbuf_kb * 1024)
        cst = S.sb([128, 1024], F32)
        S.dma('sp', cst, I['consts'], 'ld_small')
        C = {}
        C['ident'] = cst[:, 0:128]
        C['ident_bf'] = S.sb([128, 128], BF16)
        S.cp('dve', C['ident_bf'], cst[:, 0:128])
        C['perm128_bf'] = S.sb([128, 128], BF16)
        S.cp('dve', C['perm128_bf'], cst[:, 128:256])
        C['perm64_bf'] = S.sb([64, 64], BF16)
        S.cp('dve', C['perm64_bf'], cst[0:64, 256:320])
        C['maskf'] = cst[0:64, 320:384]
        C['maskb'] = cst[0:64, 384:448]
        C['rmask'] = cst[:, 512:1024]
        C['ones_bf'] = S.sb([128, 128], BF16)
        S.memset('dve', C['ones_bf'], 1.0)

        def dump(name, src_ap, shape, dt=F32):
            if name in P.dbg:
                d = P.dout(name, shape, dt)
                S.dma('sp', d, src_ap, 'st_small')

        def done():
            return stop_after is not None and stop_after in done.passed
        done.passed = set()

        def layer0():
            stage_mod(P, S, I, modv)
            m0 = S.mark()
            hT = S.sb([128, 16, T], BF16)
            stage_A(P, S, I, I['xin'], modv[0], 0, 1, hT, C['ident_bf'], [(i, i * 128) for i in range(NT)])
            segs = [(0, 1024, 'fm', AF.Silu, 0), (1024, 3072, 'fm', AF.Copy, 1024), (3072, 4096, 'tm', None, 0),
                    (4096, 5120, 'fm', AF.Sigmoid, 4096), (5120, 5952, 'fm', AF.Copy, 5120)]
            stage_inproj(P, S, I['ab_w_in'], 5952, hT, segs, PF0, PV0)
            S.release(m0)
            if stop_after == 'inproj0':
                return
            mixT = S.sb([128, 16, T], BF16)
            if 'skip_mla' not in P.dbg:
                stage_mla(P, S, I, PF0, mixT, C)
            if stop_after == 'mla':
                dump('mixT', mixT.rearrange('p c t -> p (c t)'), [128, 16 * T], BF16)
                return
            cfg = dict(H=8, DVH=1, kind='hgrn', qscale=128 ** -0.5, qbase=0, gbase=4096, mixbase=0, gcol=I['hgrn_gT'])
            stage_scan(P, S, I, cfg, PF0, PV0, OF0, mixT, C)
            dump('mixT', mixT.rearrange('p c t -> p (c t)'), [128, 16 * T], BF16)
            if stop_after == 'scan0':
                return
            stage_outproj(P, S, I, I['ab_w_out'], mixT, modv[0], I['xin'], XR, list(range(NT)))
            S.release(m0)
            dumpXR('XR_a')
            if stop_after == 'out0':
                return
            stage_moe(P, S, I, 0, modv[0], XR, C, [list(range(0, 9)), list(range(9, 18))])

        def dumpXR(name):
            if name in P.dbg:
                d = P.dout(name, [T, D])
                for ti in range(NT):
                    S.dma('sp', d[ti * 128:(ti + 1) * 128, :], XR[ti * 128:(ti + 1) * 128, :], 'st_small')

        def layer1():
            m0 = S.mark()
            hT = S.sb([128, 16, T], BF16)
            stage_A(P, S, I, XR, modv[1], 0, 1, hT, C['ident_bf'], [(i, i * 128) for i in range(NT)])
            segs = [(0, 1024, 'fm', AF.Copy, 0), (1024, 1280, 'fm', AF.Copy, 1024), (1280, 1536, 'tm', None, 0),
                    (1536, 2560, 'fm', AF.Copy, 1536), (2560, 3584, 'tm', None, 256),
                    (3584, 4608, 'fm', AF.Silu, 3584), (4608, 4640, 'fm', AF.Copy, 4608)]
            stage_inproj(P, S, I['cd_w_in'], 4640, hT, segs, PF1, PV1)
            S.release(m0)
            if stop_after == 'inproj1':
                return
            mixT = S.sb([128, 16, T], BF16)
            stage_gqa(P, S, I, PF1, PV1, mixT, C)
            if stop_after == 'gqa':
                return
            cfg = dict(H=4, DVH=2, kind='gla', qscale=128 ** -0.5, qbase=1536, kbase=2048, abase=4608, gbase=3584,
                       mixbase=8, vbase=256, gcol=I['gla_gT'])
            stage_scan(P, S, I, cfg, PF1, PV1, OF1, mixT, C)
            lat_tiles = list(range(CTX // 128, CTX // 128 + HALF // 128))
            if stop_after == 'scan1':
                return
            stage_outproj(P, S, I, I['cd_w_out'], mixT, modv[1], XR, XR, lat_tiles)
            S.release(m0)
            dumpXR('XR_c')
            if stop_after == 'out1':
                return
            stage_moe(P, S, I, 1, modv[1], XR, C, [lat_tiles], final_out=out)

        layer0()
        if stop_after is None or stop_after in ('inproj1', 'gqa', 'scan1', 'out1'):
            dumpXR('XR_b')
            layer1()
        if stop_after is not None:
            z = S.sb([128, D], F32)
            S.memset('dve', z, 0.0)
            S.dma('sp', out[0:128, :], z, 'st_small')
        S.finish()
        print("instructions:", S.n_inst, "sbuf top", S.sb_top, "dma sems", len(S.dma_sems), sorted(S.dma_sems))
    return P


def _rope_tables(d_rope):
    quarter = d_rope // 4
    half = d_rope // 2
    freqs = (np.float32(10000.0) ** (-np.arange(quarter, dtype=np.float32) / np.float32(quarter))).astype(np.float32)
    rows = LAT // 64
    row = np.repeat(np.arange(rows, dtype=np.float32), 64)
    col = np.tile(np.arange(64, dtype=np.float32), rows)
    ang = np.concatenate([row[:, None] * freqs, col[:, None] * freqs], axis=-1).astype(np.float32)
    c = np.cos(ang).astype(np.float32).T
    s_ = np.sin(ang).astype(np.float32).T
    return np.ascontiguousarray(np.concatenate([c, c], 0)), np.ascontiguousarray(np.concatenate([-s_, s_], 0))


def _consts():
    c = np.zeros((128, 1024), np.float32)
    c[:, 0:128] = np.eye(128, dtype=np.float32)
    k = np.arange(128)
    c[(k + 64) % 128, 128 + k] = 1.0
    k = np.arange(64)
    c[(k + 32) % 64, 256 + k] = 1.0
    c[0:64, 320:384] = np.triu(np.ones((64, 64), np.float32))
    c[0:64, 384:448] = np.tril(np.ones((64, 64), np.float32))
    c[:, 512:1024] = 1.0
    c[:, 512:1024:64] = 0.0
    return c


def host_inputs(inp, b, rev=False):
    f = lambda a: np.ascontiguousarray(np.asarray(a, np.float32))
    m = {}
    if rev:
        m['xin'] = f(np.concatenate([inp['ctx'][b][::-1], inp['x'][b][::-1]], 0))
    else:
        m['xin'] = f(np.concatenate([inp['ctx'][b], inp['x'][b]], 0))
    cc = np.stack([inp['c'][b], inp['c_ctx']], 1)
    m['cT'] = f(cc.reshape(16, 128, 2).transpose(1, 0, 2))
    for k_ in ['mod_w', 'mod_b', 'norm_attn_g', 'norm_ffn_g', 'final_norm_g', 'router_w', 'moe_w1', 'moe_w3', 'moe_w2']:
        m[k_] = f(inp[k_])
    m['ab_w_in'] = f(inp['ab_w_in'][0])
    m['ab_w_out'] = f(inp['ab_w_out'][0])
    m['mla_w_uq'] = f(inp['mla_w_uq'][0])
    m['mla_w_ukv'] = f(inp['mla_w_ukv'][0])
    m['mla_q_gT'] = f(inp['mla_q_norm_g'][0].reshape(4, 128).T)
    m['mla_kv_gT'] = f(inp['mla_kv_norm_g'][0].reshape(2, 128).T)
    m['hgrn_gT'] = f(inp['hgrn_norm_g'][0].reshape(1, 128).T)
    m['lbT'] = f(inp['hgrn_lb_logits'].reshape(2, 3, 8, 128).transpose(3, 0, 1, 2))
    m['cosB'], m['sinB'] = _rope_tables(64)
    m['cd_w_in'] = f(inp['cd_w_in'][0])
    m['cd_w_out'] = f(inp['cd_w_out'][0])
    m['gqa_q_gT'] = f(inp['gqa_q_norm_g'][0].reshape(1, 128).T)
    m['gqa_k_gT'] = f(inp['gqa_k_norm_g'][0].reshape(1, 128).T)
    m['gla_w_a2'] = f(inp['gla_w_a2'][0])
    m['gla_bT'] = f(inp['gla_b_a'][0].reshape(2, 4, 128).transpose(2, 0, 1))
    m['gla_gT'] = f(inp['gla_norm_g'][0].reshape(2, 128).T)
    m['cosC'], m['sinC'] = _rope_tables(128)
    m['router_b'] = f(inp['router_b'].reshape(1, 16))
    m['consts'] = _consts()
    if rev:
        w = m['ab_w_in'].copy()
        w[:, 1024:2048] = m['ab_w_in'][:, 2048:3072]
        w[:, 2048:3072] = m['ab_w_in'][:, 1024:2048]
        m['ab_w_in'] = w
        m['lbT'] = f(m['lbT'][:, ::-1])
        w = m['cd_w_in'].copy()
        w[:, 4608:4624] = m['cd_w_in'][:, 4624:4640]
        w[:, 4624:4640] = m['cd_w_in'][:, 4608:4624]
        m['cd_w_in'] = w
        m['gla_w_a2'] = f(m['gla_w_a2'][::-1])
        m['gla_bT'] = f(m['gla_bT'][:, ::-1])
        for k_ in ['cosB', 'sinB', 'cosC', 'sinC']:
            m[k_] = f(m[k_][:, ::-1])
    return m


_PROG = None
NCORES = 8


def kernel(**inputs):
    global _PROG
    if _PROG is None:
        _PROG = build()
    P = _PROG
    inp = {k: np.asarray(v) for k, v in inputs.items()}
    in_maps = []
    for c in range(NCORES):
        m = host_inputs(inp, c % 4, rev=(c >= 4))
        in_maps.append({k: v for k, v in m.items() if k in P.inputs})
    res = run_bass_kernel_spmd(P.nc, in_maps, core_ids=list(range(NCORES)))
    full = np.empty((4, LAT, D), np.float32)
    for b in range(4):
        full[b, 0:HALF] = np.asarray(res.results[b]['out'], np.float32)
        full[b, HALF:LAT] = np.asarray(res.results[b + 4]['out'], np.float32)[::-1]
    return full
```
